# Optimizing a Trainium2 kernel written in Bass

```python
import math, functools
import jax, jax.numpy as jnp
from jax import lax
import numpy as np

D_MODEL = 1024
BATCH = 8
SEQ = 2048
DEPTH = 2

CHUNK = 64
Q_BLOCK = 2 * CHUNK
HEAD_DIM = 64
N_HEADS = D_MODEL // HEAD_DIM
D_PLE = 256
D_FF = 256 * ((8 * D_MODEL // 3 + 255) // 256)
N_EXPERTS = 8
TOP_K = 2
D_FF_EXPERT = 7 * D_MODEL // 2
EXPERT_BLOCK = 256
RMS_EPS = 1e-6

kernel_name = 'yoco_fox_stickbreaking_moe_trunk'


def rmsnorm(x, g):
    xf = x.astype(jnp.float32)
    y = xf * lax.rsqrt(jnp.mean(xf * xf, axis=-1, keepdims=True) + RMS_EPS)
    return (y * g.astype(jnp.float32)).astype(x.dtype)


def split_heads(t):
    b, s, _ = t.shape
    return t.reshape(b, s, N_HEADS, HEAD_DIM).transpose(0, 2, 1, 3)


def to_query_blocks(t):
    b, h, s = t.shape[:3]
    nq = s // Q_BLOCK
    t = t.reshape((b, h, nq, Q_BLOCK) + t.shape[3:])
    return jnp.moveaxis(t, 2, 0)


def merge_query_blocks(o):
    nq, b, h, qb, d = o.shape
    return o.transpose(1, 0, 3, 2, 4).reshape(b, nq * qb, h * d)


def forgetting_attention(a, w_in, b_f, w_o):
    b, s, d_model = a.shape
    proj = a @ w_in
    q, k, v, f = jnp.split(proj, [d_model, 2 * d_model, 3 * d_model], axis=-1)
    log_f = jax.nn.log_sigmoid((f + b_f).astype(jnp.float32))
    c = jnp.cumsum(log_f, axis=1).transpose(0, 2, 1)
    q = split_heads(q) * (1.0 / math.sqrt(HEAD_DIM))
    k = split_heads(k)
    v = split_heads(v)
    kpos = jnp.arange(s)
    qpos = kpos.reshape(s // Q_BLOCK, Q_BLOCK)

    def block(args):
        qb, cb, qp = args
        logits = jnp.einsum('bhqd,bhkd->bhqk', qb, k).astype(jnp.float32)
        logits = logits + cb[..., :, None] - c[:, :, None, :]
        logits = jnp.where(kpos[None, :] <= qp[:, None], logits, -jnp.inf)
        probs = jax.nn.softmax(logits, axis=-1).astype(v.dtype)
        return jnp.einsum('bhqk,bhkd->bhqd', probs, v)

    o = lax.map(block, (to_query_blocks(q), to_query_blocks(c), qpos))
    return merge_query_blocks(o) @ w_o


def stick_breaking_attention(a, w_q, k, v, w_o):
    s = a.shape[1]
    q = split_heads(a @ w_q) * (1.0 / math.sqrt(HEAD_DIM))
    kpos = jnp.arange(s)
    qpos = kpos.reshape(s // Q_BLOCK, Q_BLOCK)

    def block(args):
        qb, qp = args
        z = jnp.einsum('bhqd,bhkd->bhqk', qb, k).astype(jnp.float32)
        mask = kpos[None, :] < qp[:, None]
        log_1mb = jnp.where(mask, jax.nn.log_sigmoid(-z), 0.0)
        later = lax.cumsum(log_1mb, axis=3, reverse=True) - log_1mb
        weights = jnp.where(mask, jnp.exp(jax.nn.log_sigmoid(z) + later), 0.0)
        return jnp.einsum('bhqk,bhkd->bhqd', weights.astype(v.dtype), v)

    o = lax.map(block, (to_query_blocks(q), qpos))
    return merge_query_blocks(o) @ w_o


def swiglu(h, w_gu, w_down):
    g, u = jnp.split(h @ w_gu, 2, axis=-1)
    return (jax.nn.silu(g) * u) @ w_down


def moe_swiglu(h, router_w, router_b, w_gu, w_down):
    b, s, d = h.shape
    t = b * s
    xt = h.reshape(t, d)
    logits = (xt @ router_w).astype(jnp.float32) + router_b.astype(jnp.float32)
    top_v, top_i = lax.top_k(logits, TOP_K)
    gates = jax.nn.softmax(top_v, axis=-1)
    n_assign = t * TOP_K
    flat_e = top_i.reshape(-1)
    flat_g = gates.reshape(-1)
    flat_tok = jnp.arange(n_assign) // TOP_K
    order = jnp.argsort(flat_e)
    sorted_e = flat_e[order]
    counts = jnp.bincount(flat_e, length=N_EXPERTS)
    padded = (counts + EXPERT_BLOCK - 1) // EXPERT_BLOCK * EXPERT_BLOCK
    start = jnp.cumsum(counts) - counts
    pstart = jnp.cumsum(padded) - padded
    dest = pstart[sorted_e] + jnp.arange(n_assign) - start[sorted_e]
    n_blocks = -(-n_assign // EXPERT_BLOCK) + N_EXPERTS
    n_rows = n_blocks * EXPERT_BLOCK
    row_tok = jnp.zeros((n_rows,), jnp.int32).at[dest].set(flat_tok[order])
    row_gate = jnp.zeros((n_rows,), jnp.float32).at[dest].set(flat_g[order])
    block_e = jnp.minimum(
        jnp.searchsorted(jnp.cumsum(padded), jnp.arange(n_blocks) * EXPERT_BLOCK, side='right'),
        N_EXPERTS - 1)
    xb = xt[row_tok].reshape(n_blocks, EXPERT_BLOCK, d)

    def expert_block(args):
        xe, e = args
        return swiglu(xe, w_gu[e], w_down[e])

    yb = lax.map(expert_block, (xb, block_e)).reshape(n_rows, d)
    out = jnp.zeros((t, d), h.dtype).at[row_tok].add(yb * row_gate[:, None].astype(yb.dtype))
    return out.reshape(b, s, d)


def setup_inputs(seed: int = 0) -> dict:
    key = jax.random.key(seed)
    ks = jax.random.split(key, 24)
    n_a = DEPTH // 2
    n_b = DEPTH - n_a
    n_dense = (DEPTH + 1) // 2
    n_moe = DEPTH // 2
    D, H, F, FE = D_MODEL, N_HEADS, D_FF, D_FF_EXPERT

    def nrm(k, shape, scale):
        return jax.random.normal(k, shape, jnp.float32) * scale

    return {
        'x': nrm(ks[0], (BATCH, SEQ, D), 1.0),
        'p': nrm(ks[1], (DEPTH, BATCH, SEQ, D_PLE), 1.0),
        'attn_norm': 1.0 + nrm(ks[2], (DEPTH, D), 0.02),
        'ffn_norm': 1.0 + nrm(ks[3], (DEPTH, D), 0.02),
        'w_in_a': nrm(ks[4], (n_a, D, 3 * D + H), D ** -0.5),
        'b_f': jax.random.uniform(ks[5], (n_a, H), jnp.float32, 2.0, 5.0),
        'w_o_a': nrm(ks[6], (n_a, D, D), D ** -0.5),
        'kv_norm': 1.0 + nrm(ks[7], (D,), 0.02),
        'w_kv': nrm(ks[8], (D, 2 * D), D ** -0.5),
        'w_q_b': nrm(ks[9], (n_b, D, D), D ** -0.5),
        'w_o_b': nrm(ks[10], (n_b, D, D), D ** -0.5),
        'w_gu_dense': nrm(ks[11], (n_dense, D, 2 * F), D ** -0.5),
        'w_down_dense': nrm(ks[12], (n_dense, F, D), F ** -0.5),
        'router_w': nrm(ks[13], (n_moe, D, N_EXPERTS), D ** -0.5),
        'router_b': nrm(ks[14], (n_moe, N_EXPERTS), 0.01),
        'w_gu_moe': nrm(ks[15], (n_moe, N_EXPERTS, D, 2 * FE), D ** -0.5),
        'w_down_moe': nrm(ks[16], (n_moe, N_EXPERTS, FE, D), FE ** -0.5),
        'w_ple_proj': nrm(ks[17], (DEPTH, D_PLE, D), D_PLE ** -0.5),
        'w_ple_gate': nrm(ks[18], (DEPTH, D, D), D ** -0.5),
        'final_norm': 1.0 + nrm(ks[19], (D,), 0.02),
    }


def reference(x, p, attn_norm, ffn_norm, w_in_a, b_f, w_o_a, kv_norm, w_kv, w_q_b, w_o_b,
              w_gu_dense, w_down_dense, router_w, router_b, w_gu_moe, w_down_moe,
              w_ple_proj, w_ple_gate, final_norm):
    n_a = DEPTH // 2
    h = x
    k_shared = None
    v_shared = None
    for i in range(DEPTH):
        a = rmsnorm(h, attn_norm[i])
        if i < n_a:
            h = h + forgetting_attention(a, w_in_a[i], b_f[i], w_o_a[i])
        else:
            j = i - n_a
            h = h + stick_breaking_attention(a, w_q_b[j], k_shared, v_shared, w_o_b[j])
        f = rmsnorm(h, ffn_norm[i])
        if i % 2 == 0:
            h = h + swiglu(f, w_gu_dense[i // 2], w_down_dense[i // 2])
        else:
            h = h + moe_swiglu(f, router_w[i // 2], router_b[i // 2], w_gu_moe[i // 2], w_down_moe[i // 2])
        h = h + (p[i] @ w_ple_proj[i]) * jax.nn.sigmoid(h @ w_ple_gate[i])
        if i == n_a - 1:
            kv = rmsnorm(h, kv_norm) @ w_kv
            k_s, v_s = jnp.split(kv, 2, axis=-1)
            k_shared = split_heads(k_s)
            v_shared = split_heads(v_s)
    return rmsnorm(h, final_norm)
```

```python
import contextlib
import numpy as np
import concourse.bass as bass
import concourse.mybir as mybir
from concourse.bass_utils import run_bass_kernel_spmd

F32 = mybir.dt.float32
BF16 = mybir.dt.bfloat16
I32 = mybir.dt.int32
AF = mybir.ActivationFunctionType
ALU = mybir.AluOpType

S = 2048
D = 1024
NH = 16
FF = 2816
FE = 3584
NE = 8
NG = 4
NCH = 8
EPS = 1e-6
NEGBIG = -30000.0

DEBUG_STOP = None


class Region:
    __slots__ = ("w", "r")

    def __init__(self):
        self.w = None
        self.r = []


class FW:
    def __init__(self, nc, es):
        self.nc = nc
        self.es = es
        self.eng = {"pe": nc.tensor, "act": nc.scalar, "dve": nc.vector, "pool": nc.gpsimd, "sp": nc.sync}
        self.sem = {k: es.enter_context(nc.semaphore("s_" + k)) for k in self.eng}
        self.cnt = {k: 0 for k in self.eng}
        self.waited = {k: {} for k in self.eng}
        self.dsems = []

    def dsem(self, name):
        h = {"sem": self.es.enter_context(self.nc.semaphore(f"{name}_{len(self.dsems)}")), "val": 0}
        self.dsems.append(h)
        return h

    def _wait(self, e, tok):
        sem, val, owner = tok
        if owner is not None:
            if owner == "pe" and e == "pe":
                return
            assert val <= self.cnt[owner], f"wait on future token {owner}:{val} > {self.cnt[owner]}"
        key = id(sem)
        if self.waited[e].get(key, 0) >= val:
            return
        self.waited[e][key] = val
        self.eng[e].wait_ge(sem, val)

    def _deps(self, e, reads, writes):
        for r in reads:
            if r.w is not None:
                self._wait(e, r.w)
        for w in writes:
            if w.w is not None:
                self._wait(e, w.w)
            for t in w.r:
                self._wait(e, t)

    def _record(self, tok, reads, writes):
        for r in reads:
            r.r.append(tok)
            if len(r.r) > 64:
                best = {}
                for t in r.r:
                    k = id(t[0])
                    if k not in best or best[k][1] < t[1]:
                        best[k] = t
                r.r = list(best.values())
        for w in writes:
            w.w = tok
            w.r = []

    def op(self, e, fn, reads=(), writes=(), track=True):
        self._deps(e, reads, writes)
        inst = fn(self.eng[e])
        if track:
            self.cnt[e] += 1
            inst.then_inc(self.sem[e], 1)
            tok = (self.sem[e], self.cnt[e], e)
        else:
            tok = (self.sem[e], self.cnt[e] + 1, e)
        self._record(tok, reads, writes)
        return inst

    def dma(self, q, out, in_, ds, reads=(), writes=(), **kw):
        self._deps(q, reads, writes)
        inst = self.eng[q].dma_start(out=out, in_=in_, **kw)
        ds["val"] += 16
        inst.then_inc(ds["sem"], 16)
        tok = (ds["sem"], ds["val"], None)
        self._record(tok, reads, writes)
        return tok

    def barrier(self):
        for e in self.eng:
            for o in self.eng:
                if self.cnt[o] > 0:
                    self._wait(e, (self.sem[o], self.cnt[o], None))
            for ds in self.dsems:
                if ds["val"] > 0:
                    self._wait(e, (ds["sem"], ds["val"], None))


def build(stop=None):
    nc = bass.Bass("TRN2", target_bir_lowering=False)

    def din(name, shape, dt=F32):
        return nc.dram_tensor(name, list(shape), dt, kind="ExternalInput").ap()

    x = din("x", [S, D])
    p = din("p", [2, S, 256])
    gam_d = din("gam", [128, 48])
    bf_d = din("bf", [16, 1])
    rb_d = din("rb", [128, 8])
    w_in = din("w_in", [D, 3 * D + NH])
    w_o_a = din("w_o_a", [D, D])
    w_kv = din("w_kv", [D, 2 * D])
    w_q_b = din("w_q_b", [D, D])
    w_o_b = din("w_o_b", [D, D])
    w_gu_d = din("w_gu_d", [D, 2 * FF])
    w_dn_d = din("w_dn_d", [FF, D])
    r_w = din("r_w", [D, NE])
    lite = stop is not None and stop not in ("moe", "ple1")
    w_gu_m = din("w_gu_m", [NE, D, 2 * FE] if not lite else [1, 1, 2])
    w_dn_m = din("w_dn_m", [NE, FE, D] if not lite else [1, 1, 2])
    w_pp = din("w_pp", [2, 256, D])
    w_pg = din("w_pg", [2, D, D])
    out = nc.dram_tensor("out", [S, D], F32, kind="ExternalOutput").ap()
    cq_d = nc.dram_tensor("cq_d", [NH, 3, S], BF16, kind="Internal").ap()
    ck_d = nc.dram_tensor("ck_d", [NH, 3, S], BF16, kind="Internal").ap()
    dbg = None
    if stop is not None:
        dbg = nc.dram_tensor("dbg", [128, NCH, S], F32, kind="ExternalOutput").ap()

    def kview(w2d):
        return w2d.rearrange("(kc p) n -> p kc n", p=128)

    with contextlib.ExitStack() as es:
        fw = FW(nc, es)
        op = fw.op

        uid = [0]

        def sb(st, name, shape, dt):
            uid[0] += 1
            return st.enter_context(nc.sbuf_tensor(f"{name}_{uid[0]}", list(shape), dt))

        T = {}
        ps = es.enter_context(nc.psum_tensor("ps", [128, 8, 512], F32))
        psr = [Region() for _ in range(8)]

        hT = sb(es, "hT", [128, NCH, S], F32)
        R_h = [[Region() for _ in range(NG)] for _ in range(NCH)]
        def alloc_aT(st):
            T["aT"] = sb(st, "aT", [128, NCH, S], BF16)

        R_a = [[Region() for _ in range(NG)] for _ in range(NCH)]
        all_a = [R_a[m][g] for m in range(NCH) for g in range(NG)]

        ident_f = sb(es, "ident_f", [128, 128], F32)
        negI_b = sb(es, "negI_b", [128, 128], BF16)
        negbigI_b = sb(es, "negbigI_b", [128, 128], BF16)
        negtri_b = sb(es, "negtri_b", [128, 128], BF16)
        ones_b = sb(es, "ones_b", [128, 128], BF16)
        gam = sb(es, "gam", [128, 48], F32)
        nbf = sb(es, "nbf", [16, 1], F32)
        rb = sb(es, "rb", [128, 8], F32)
        R_const = Region()
        ds_c = fw.dsem("ds_c")
        with contextlib.ExitStack() as ph:
            io = sb(ph, "io", [128, 512], I32)
            R_io = Region()
            op("pool", lambda e: e.iota(io[:, 0:128], [[1, 128]], base=0, channel_multiplier=-1), writes=[R_io])
            op("dve", lambda e: e.tensor_scalar(ident_f[:], io[:, 0:128], 0.0, None, ALU.is_equal), reads=[R_io], writes=[R_const])
            op("dve", lambda e: e.tensor_scalar(negI_b[:], io[:, 0:128], 0.0, -1.0, ALU.is_equal, ALU.mult), reads=[R_io], writes=[R_const])
            op("dve", lambda e: e.tensor_scalar(negbigI_b[:], io[:, 0:128], 0.0, NEGBIG, ALU.is_equal, ALU.mult), reads=[R_io], writes=[R_const])
            op("dve", lambda e: e.tensor_scalar(negtri_b[:], io[:, 0:128], 0.0, -1.0, ALU.is_le, ALU.mult), reads=[R_io], writes=[R_const])
            op("dve", lambda e: e.memset(ones_b[:], 1.0), writes=[R_const])
            fw.dma("sp", gam[:], gam_d[:, :], ds_c, writes=[R_const])
            fw.dma("sp", nbf[:], bf_d[:, :], ds_c, writes=[R_const])
            fw.dma("sp", rb[:], rb_d[:, :], ds_c, writes=[R_const])
            op("dve", lambda e: e.tensor_scalar(nbf[:], nbf[:], -1.0, None, ALU.mult), reads=[R_const], writes=[R_const])
            fw.barrier()

        def gc(g):
            return slice(g * 512, (g + 1) * 512)

        def make_mask(st, name, cmp_op):
            mk = sb(st, name, [128, 4, 512], BF16)
            with contextlib.ExitStack() as tmpst:
                io_ = sb(tmpst, name + "_io", [128, 512], I32)
                R_io_ = Region()
                for v in range(4):
                    op("pool", lambda e: e.iota(io_[:, :], [[1, 512]], base=-128 * v, channel_multiplier=-1), writes=[R_io_])
                    op("dve", lambda e: e.tensor_scalar(mk[:, v, :], io_[:, :], 0.0, None, cmp_op), reads=[R_io_], writes=[R_const])
                fw.barrier()
            return mk

        def mm(out_ap, lhsT, rhs, start, stop, reads, writes, track=None):
            if track is None:
                track = stop
            return op("pe", lambda e: e.matmul(out_ap, lhsT, rhs, start=start, stop=stop), reads=reads, writes=writes, track=track)

        class Stream:
            def __init__(self, st, name, shape, dt, nbuf):
                self.t = [sb(st, f"{name}{i}", shape, dt) for i in range(nbuf)]
                self.r = [Region() for _ in range(nbuf)]
                self.ds = [fw.dsem(f"ds_{name}{i}") for i in range(nbuf)]
                self.i = 0
                self.n = nbuf

            def next(self):
                i = self.i
                self.i = (i + 1) % self.n
                return self.t[i], self.r[i], self.ds[i]

        def wload(stream, dst_fn, src, q="pool", **kw):
            t, r, ds = stream.next()
            fw.dma(q, dst_fn(t), src, ds, writes=[r], **kw)
            return t, r

        def dump(name):
            if stop == name:
                ds = fw.dsem("ds_dbg")
                tok = fw.dma("sp", dbg[:, :, :], hT[:], ds, reads=[R_h[m][g] for m in range(NCH) for g in range(NG)])
                fw._wait("sp", tok)
                return True
            return False

        def rmsnorm(gi, ph, out_fn=None, post=None):
            sq = [sb(ph, f"sq{gi}_{i}", [128, NCH, 512], BF16) for i in range(2)]
            R_sq = [Region(), Region()]
            lnt = sb(ph, f"lnt{gi}", [128, 512], F32)
            R_ln = Region()
            rstd = [sb(ph, f"rstd{gi}_{i}", [128, 512], F32) for i in range(2)]
            R_rs = [Region(), Region()]
            for g in range(NG):
                b = g % 2
                for m in range(NCH):
                    op("act", lambda e: e.activation(out=sq[b][:, m, :], in_=hT[:, m, gc(g)], func=AF.Square),
                       reads=[R_h[m][g]], writes=[R_sq[b]])
                bank = 6 + b
                for m in range(NCH):
                    mm(ps[:, bank, :], ones_b[:], sq[b][:, m, :], m == 0, m == NCH - 1, [R_sq[b]], [psr[bank]])
                op("act", lambda e: e.activation(out=lnt[:], in_=ps[:, bank, :], func=AF.Ln, bias=EPS, scale=1.0 / D),
                   reads=[psr[bank]], writes=[R_ln])
                op("act", lambda e: e.activation(out=rstd[b][:], in_=lnt[:], func=AF.Exp, scale=-0.5),
                   reads=[R_ln], writes=[R_rs[b]])
                for m in range(NCH):
                    if out_fn is None:
                        op("dve", lambda e: e.scalar_tensor_tensor(out=T["aT"][:, m, gc(g)], in0=hT[:, m, gc(g)],
                                                                   scalar=gam[:, gi * 8 + m:gi * 8 + m + 1], in1=rstd[b][:],
                                                                   op0=ALU.mult, op1=ALU.mult),
                           reads=[R_h[m][g], R_rs[b]], writes=[R_a[m][g]])
                    else:
                        out_fn(g, m, rstd[b], R_rs[b])
                if post is not None:
                    post(g)

        with contextlib.ExitStack() as ph:
            xs = Stream(ph, "xs", [128, 4, D], F32, 2)
            k = 0
            for g in range(NG):
                xt, xr = wload(xs, lambda t: t[:], x[g * 512:(g + 1) * 512, :].rearrange("(t q) d -> q t d", q=128), q="sp")
                for m in range(NCH):
                    bank = k % 4
                    k += 1
                    for t in range(4):
                        op("pe", lambda e: e.transpose(ps[:, bank, t * 128:(t + 1) * 128], xt[:, t, m * 128:(m + 1) * 128], ident_f[:]),
                           reads=[xr, R_const], writes=[psr[bank]], track=(t == 3))
                    if m % 2 == 0:
                        op("act", lambda e: e.activation(out=hT[:, m, gc(g)], in_=ps[:, bank, :], func=AF.Copy),
                           reads=[psr[bank]], writes=[R_h[m][g]])
                    else:
                        op("dve", lambda e: e.tensor_copy(hT[:, m, gc(g)], ps[:, bank, :]), reads=[psr[bank]], writes=[R_h[m][g]])
            fw.barrier()
        if dump("x"):
            return nc

        def out_proj_pair(wo, wor, OTp, R_ot, banks):
            k = 0
            for m in range(NCH):
                for g in range(NG):
                    bank = banks[k % len(banks)]
                    k += 1
                    mm(ps[:, bank, :], wo[:, m * 128:(m + 1) * 128], OTp[:, gc(g)], True, True, [wor, R_ot[g]], [psr[bank]])
                    op("dve", lambda e: e.tensor_tensor(out=hT[:, m, gc(g)], in0=ps[:, bank, :], in1=hT[:, m, gc(g)], op=ALU.add),
                       reads=[psr[bank], R_h[m][g]], writes=[R_h[m][g]])

        def vslice(h):
            if h % 2 == 0:
                return slice(h + 1, 18, 16 - h)
            return slice(0, h + 2, h + 1)

        R_v = [Region() for _ in range(16)]

        def v_proj(ph, wsrc, col0, fill):
            Vt = T["Vt"]
            wv = Stream(ph, "wv", [128, NCH, 512], BF16, 2)
            k = 0
            for half in range(2):
                wt, wr = wload(wv, lambda t: t[:], kview(wsrc)[:, :, col0 + half * 512: col0 + (half + 1) * 512])
                for tt in range(16):
                    bank = k % 4
                    k += 1
                    g = tt // 4
                    for kc in range(NCH):
                        mm(ps[:, bank, :], T["aT"][:, kc, tt * 128:(tt + 1) * 128], wt[:, kc, :], kc == 0, kc == NCH - 1,
                           [R_a[kc][g], wr], [psr[bank]])
                    dst = Vt[:, tt, half * 512:(half + 1) * 512]
                    src = ps[:, bank, :]
                    if tt % 2 == 0:
                        op("act", lambda e: e.activation(out=dst, in_=src, func=AF.Copy), reads=[psr[bank]], writes=[R_v[tt]])
                    else:
                        op("dve", lambda e: e.tensor_copy(dst, src), reads=[psr[bank]], writes=[R_v[tt]])

        phA = contextlib.ExitStack()
        Vt = T["Vt"] = sb(phA, "VtA", [128, 16, D], BF16)
        maskF = make_mask(phA, "maskF", ALU.is_lt)
        alloc_aT(phA)
        with contextlib.ExitStack() as ph:
            rmsnorm(0, ph)
            fw.barrier()
        with contextlib.ExitStack() as ph:
            wf = sb(ph, "wf", [128, NCH, NH], BF16)
            R_wf = Region()
            ds_wf = fw.dsem("ds_wf")
            fw.dma("pool", wf[:], kview(w_in)[:, :, 3 * D:3 * D + NH], ds_wf, writes=[R_wf])
            t1 = sb(ph, "t1", [NH, S], F32)
            cc = sb(ph, "cc", [NH, S], F32)
            one16 = sb(ph, "one16", [NH, S], F32)
            SQ = sb(ph, "SQ", [NH, 3, S], BF16)
            SK = sb(ph, "SK", [NH, 3, S], BF16)
            R_t = Region()
            op("pool", lambda e: e.memset(one16[:], 1.0), writes=[R_t])
            for g in range(NG):
                for kc in range(NCH):
                    mm(ps[0:NH, g, :], wf[:, kc, :], T["aT"][:, kc, gc(g)], kc == 0, kc == NCH - 1, [R_wf, R_a[kc][g]], [psr[g]])
                op("act", lambda e: e.activation(out=t1[:, gc(g)], in_=ps[0:NH, g, :], func=AF.Exp, bias=nbf[:, 0:1], scale=-1.0),
                   reads=[psr[g], R_const], writes=[R_t])
            op("act", lambda e: e.activation(out=t1[:], in_=t1[:], func=AF.Ln, bias=1.0, scale=1.0), reads=[R_t], writes=[R_t])
            op("dve", lambda e: e.tensor_tensor_scan(cc[:], one16[:], t1[:], 0.0, ALU.mult, ALU.subtract), reads=[R_t], writes=[R_t])
            op("dve", lambda e: e.tensor_copy(SQ[:, 0, :], cc[:]), reads=[R_t], writes=[R_t])
            op("dve", lambda e: e.tensor_tensor(out=cc[:], in0=cc[:], in1=SQ[:, 0, :], op=ALU.subtract), reads=[R_t], writes=[R_t])
            op("dve", lambda e: e.tensor_copy(SQ[:, 1, :], cc[:]), reads=[R_t], writes=[R_t])
            op("dve", lambda e: e.tensor_tensor(out=cc[:], in0=cc[:], in1=SQ[:, 1, :], op=ALU.subtract), reads=[R_t], writes=[R_t])
            op("dve", lambda e: e.tensor_copy(SQ[:, 2, :], cc[:]), reads=[R_t], writes=[R_t])
            op("dve", lambda e: e.tensor_scalar(SK[:], SQ[:], -1.0, None, ALU.mult), reads=[R_t], writes=[R_t])
            ds_cq = fw.dsem("ds_cq")
            fw.dma("sp", cq_d[:, :, :], SQ[:], ds_cq, reads=[R_t])
            fw.dma("sp", ck_d[:, :, :], SK[:], ds_cq, reads=[R_t])
            fw.barrier()
        with contextlib.ExitStack() as ph:
            v_proj(ph, w_in, 2 * D, 1.0)
            fw.barrier()
        with contextlib.ExitStack() as ph:
            wqs = Stream(ph, "wq", [128, NCH, 128], BF16, 2)
            wks = Stream(ph, "wk", [128, NCH, 128], BF16, 2)
            wos = Stream(ph, "wo", [128, D], BF16, 2)
            qa = [[sb(ph, f"qa{b}{hh}", [128, S], BF16) for hh in range(2)] for b in range(2)]
            ka = [[sb(ph, f"ka{b}{hh}", [128, S], BF16) for hh in range(2)] for b in range(2)]
            R_qa = [[[Region() for _ in range(NG)] for hh in range(2)] for b in range(2)]
            R_ka = [[[Region() for _ in range(NG)] for hh in range(2)] for b in range(2)]
            R_qaug = [[Region() for hh in range(2)] for b in range(2)]
            R_kaug = [[Region() for hh in range(2)] for b in range(2)]
            ds_aug = [[[fw.dsem(f"ds_aug{b}{hh}{z}") for z in range(2)] for hh in range(2)] for b in range(2)]
            PT = [sb(ph, f"PT{i}", [128, 512], BF16) for i in range(3)]
            R_pt = [Region() for _ in range(3)]
            OTp = [sb(ph, f"OTp{i}", [128, S], BF16) for i in range(2)]
            R_ot = [[Region() for _ in range(NG)] for _ in range(2)]
            rc = [sb(ph, f"rc{i}", [128, 512], F32) for i in range(2)]
            R_rc = [Region(), Region()]
            for b in range(2):
                for hh in range(2):
                    op("pool", lambda e: e.memset(qa[b][hh][64:128, :], 0.0), writes=[R_qaug[b][hh]])
                    op("pool", lambda e: e.memset(qa[b][hh][96:99, :], 1.0), writes=[R_qaug[b][hh]])
                    op("pool", lambda e: e.memset(ka[b][hh][64:128, :], 0.0), writes=[R_kaug[b][hh]])
                    op("pool", lambda e: e.memset(ka[b][hh][64:67, :], 1.0), writes=[R_kaug[b][hh]])

            def proj_pair(pr):
                b = pr % 2
                wq, wqr = wload(wqs, lambda t: t[:], kview(w_in)[:, :, pr * 128:(pr + 1) * 128])
                wk, wkr = wload(wks, lambda t: t[:], kview(w_in)[:, :, D + pr * 128: D + (pr + 1) * 128])
                for hh in range(2):
                    h = 2 * pr + hh
                    fw.dma("sp", qa[b][hh][64:67, :], cq_d[h, :, :], ds_aug[b][hh][0], writes=[R_qaug[b][hh]])
                    fw.dma("sp", ka[b][hh][96:99, :], ck_d[h, :, :], ds_aug[b][hh][1], writes=[R_kaug[b][hh]])
                k = 0
                for (wt, wr, dst, R_d, scl) in ((wq, wqr, qa[b], R_qa[b], 0.125), (wk, wkr, ka[b], R_ka[b], 1.0)):
                    for g in range(NG):
                        bank = 7
                        k += 1
                        for kc in range(NCH):
                            mm(ps[:, bank, :], wt[:, kc, :], T["aT"][:, kc, gc(g)], kc == 0, kc == NCH - 1, [wr, R_a[kc][g]], [psr[bank]])
                        for hh in range(2):
                            op("dve", lambda e: e.tensor_scalar(dst[hh][0:64, gc(g)], ps[hh * 64:(hh + 1) * 64, bank, :], scl, None, ALU.mult),
                               reads=[psr[bank]], writes=[R_d[hh][g]])

            sk = 0
            ak = 0
            proj_pair(0)
            for pr in range(NH // 2):
                b = pr % 2
                wo, wor = wload(wos, lambda t: t[:], w_o_a[pr * 128:(pr + 1) * 128, :])
                for hh in range(2):
                    h = 2 * pr + hh
                    for g in range(NG):
                        accb = 3 + (ak % 2)
                        denb = 5 + (ak % 2)
                        ak += 1
                        nj = 4 * g + 4
                        hp = slice(hh * 64, (hh + 1) * 64)

                        def s_mm(j, sbk):
                            diag = j >= 4 * g
                            mm(ps[:, sbk, :], ka[b][hh][0:99, j * 128:(j + 1) * 128], qa[b][hh][0:99, gc(g)], True, not diag,
                               [R_ka[b][hh][j // 4], R_kaug[b][hh], R_qa[b][hh][g], R_qaug[b][hh]], [psr[sbk]])
                            if diag:
                                mm(ps[:, sbk, :], negbigI_b[:], maskF[:, j - 4 * g, :], False, True, [R_const], [psr[sbk]])

                        sk0 = sk
                        sk += nj
                        s_mm(0, sk0 % 3)
                        if nj > 1:
                            s_mm(1, (sk0 + 1) % 3)

                        def pv_den(jj):
                            cb = (sk0 + jj) % 3
                            mm(ps[:, accb, :], Vt[:, jj, pr * 128:(pr + 1) * 128], PT[cb][:], jj == 0, jj == nj - 1,
                               [R_v[jj], R_pt[cb]], [psr[accb]], track=(jj == nj - 1))
                            mm(ps[:, denb, :], ones_b[:], PT[cb][:], jj == 0, jj == nj - 1,
                               [R_const, R_pt[cb]], [psr[denb]], track=True)

                        for j in range(nj):
                            cur = (sk0 + j) % 3
                            if j + 2 < nj:
                                s_mm(j + 2, (sk0 + j + 2) % 3)
                            op("act", lambda e: e.activation(out=PT[cur][:], in_=ps[:, cur, :], func=AF.Exp),
                               reads=[psr[cur]], writes=[R_pt[cur]])
                            if j >= 1:
                                pv_den(j - 1)
                        pv_den(nj - 1)
                        r2 = ak % 2
                        op("act", lambda e: e.activation(out=rc[r2][hp, :], in_=ps[hp, denb, :], func=AF.Ln), reads=[psr[denb]], writes=[R_rc[r2]])
                        op("act", lambda e: e.activation(out=rc[r2][hp, :], in_=rc[r2][hp, :], func=AF.Exp, scale=-1.0), reads=[R_rc[r2]], writes=[R_rc[r2]])
                        op("dve", lambda e: e.tensor_tensor(out=OTp[b][hp, gc(g)], in0=ps[hp, accb, :], in1=rc[r2][hp, :], op=ALU.mult),
                           reads=[psr[accb], R_rc[r2]], writes=[R_ot[b][g]])
                    if hh == 0 and pr + 1 < NH // 2:
                        proj_pair(pr + 1)
                out_proj_pair(wo, wor, OTp[b], R_ot[b], [7])
            fw.barrier()
        phA.close()
        if dump("attn0"):
            return nc

        def swiglu(ph, tag, w_gu2d, w_dn2d, ff, halves, gate_fn=None):
            nmb = ff // 256
            nkc = ff // 128
            wgs = Stream(ph, f"wg{tag}", [128, NCH, 256], BF16, 2)
            wus = Stream(ph, f"wu{tag}", [128, NCH, 256], BF16, 2)
            wds = Stream(ph, f"wd{tag}", [128, nkc, 128], BF16, 2)
            actT = sb(ph, f"actT{tag}", [128, nkc, 1024], BF16)
            R_act = [[Region() for _ in range(2)] for _ in range(nkc)]
            sgt = [sb(ph, f"sg{tag}{i}", [128, 512], BF16) for i in range(2)]
            R_sg = [Region(), Region()]
            tmp = [sb(ph, f"tmp{tag}{i}", [128, 512], F32) for i in range(2)]
            R_tmp = [Region(), Region()]
            return dict(nmb=nmb, nkc=nkc, wgs=wgs, wus=wus, wds=wds, actT=actT, R_act=R_act, sgt=sgt, R_sg=R_sg, tmp=tmp, R_tmp=R_tmp)

        def swiglu_run(st, w_gu2d, w_dn2d, ff, half, gate=None):
            nmb, nkc = st["nmb"], st["nkc"]
            actT, R_act = st["actT"], st["R_act"]
            k = 0
            sgk = 0
            wg_v = kview(w_gu2d)
            wd_v = kview(w_dn2d)
            nxt = (wload(st["wgs"], lambda t: t[:], wg_v[:, :, 0:256]), wload(st["wus"], lambda t: t[:], wg_v[:, :, ff:ff + 256]))
            for mb in range(nmb):
                (wg, wgr), (wu, wur) = nxt
                if mb + 1 < nmb:
                    c0 = (mb + 1) * 256
                    nxt = (wload(st["wgs"], lambda t: t[:], wg_v[:, :, c0:c0 + 256]),
                           wload(st["wus"], lambda t: t[:], wg_v[:, :, ff + c0:ff + c0 + 256]))
                for mi in range(2):
                    m = mb * 2 + mi
                    for gg in range(2):
                        g = half * 2 + gg
                        gb = (k % 2) * 2
                        ub = gb + 1
                        k += 1
                        for kc in range(NCH):
                            mm(ps[:, gb, :], wg[:, kc, mi * 128:(mi + 1) * 128], T["aT"][:, kc, gc(g)], kc == 0, kc == NCH - 1,
                               [wgr, R_a[kc][g]], [psr[gb]])
                        for kc in range(NCH):
                            mm(ps[:, ub, :], wu[:, kc, mi * 128:(mi + 1) * 128], T["aT"][:, kc, gc(g)], kc == 0, kc == NCH - 1,
                               [wur, R_a[kc][g]], [psr[ub]])
                        s = sgk % 2
                        sgk += 1
                        op("act", lambda e: e.activation(out=st["sgt"][s][:], in_=ps[:, gb, :], func=AF.Silu),
                           reads=[psr[gb]], writes=[st["R_sg"][s]])
                        op("dve", lambda e: e.tensor_tensor(out=actT[:, m, gg * 512:(gg + 1) * 512], in0=ps[:, ub, :], in1=st["sgt"][s][:], op=ALU.mult),
                           reads=[psr[ub], st["R_sg"][s]], writes=[R_act[m][gg]])
            nxt = wload(st["wds"], lambda t: t[:], wd_v[:, :, 0:128])
            k = 0
            for o in range(NCH):
                wd, wdr = nxt
                if o + 1 < NCH:
                    nxt = wload(st["wds"], lambda t: t[:], wd_v[:, :, (o + 1) * 128:(o + 2) * 128])
                for gg in range(2):
                    g = half * 2 + gg
                    bank = 4 + (k % 4)
                    k += 1
                    for kc in range(nkc):
                        mm(ps[:, bank, :], wd[:, kc, :], actT[:, kc, gg * 512:(gg + 1) * 512], kc == 0, kc == nkc - 1,
                           [wdr, R_act[kc][gg]], [psr[bank]])
                    if gate is None:
                        op("dve", lambda e: e.tensor_tensor(out=hT[:, o, gc(g)], in0=ps[:, bank, :], in1=hT[:, o, gc(g)], op=ALU.add),
                           reads=[psr[bank], R_h[o][g]], writes=[R_h[o][g]])
                    else:
                        gt, gr = gate
                        s = k % 2
                        op("dve", lambda e: e.tensor_tensor(out=st["tmp"][s][:], in0=ps[:, bank, :], in1=gt[:, gg * 512:(gg + 1) * 512], op=ALU.mult),
                           reads=[psr[bank], gr], writes=[st["R_tmp"][s]])
                        op("pool", lambda e: e.tensor_tensor(out=hT[:, o, gc(g)], in0=st["tmp"][s][:], in1=hT[:, o, gc(g)], op=ALU.add),
                           reads=[st["R_tmp"][s], R_h[o][g]], writes=[R_h[o][g]])

        phF = contextlib.ExitStack()
        alloc_aT(phF)
        with contextlib.ExitStack() as ph:
            rmsnorm(1, ph)
            fw.barrier()
        with contextlib.ExitStack() as ph:
            st = swiglu(ph, "d", w_gu_d, w_dn_d, FF, 2)
            for half in range(2):
                swiglu_run(st, w_gu_d, w_dn_d, FF, half)
            fw.barrier()
        phF.close()
        if dump("ffn0"):
            return nc

        def ple(i):
            with contextlib.ExitStack() as ph:
                alloc_aT(ph)
                for m in range(NCH):
                    for g in range(NG):
                        if (m + g) % 2 == 0:
                            op("act", lambda e: e.activation(out=T["aT"][:, m, gc(g)], in_=hT[:, m, gc(g)], func=AF.Copy),
                               reads=[R_h[m][g]], writes=[R_a[m][g]])
                        else:
                            op("pool", lambda e: e.tensor_copy(T["aT"][:, m, gc(g)], hT[:, m, gc(g)]), reads=[R_h[m][g]], writes=[R_a[m][g]])
                pt = sb(ph, f"pt{i}", [128, 16, 256], F32)
                R_p = Region()
                ds_p = fw.dsem(f"ds_p{i}")
                fw.dma("sp", pt[:], p[i, :, :].rearrange("(t q) f -> q t f", q=128), ds_p, writes=[R_p])
                pT = sb(ph, f"pT{i}", [128, 2, S], BF16)
                R_pT = [Region() for _ in range(NG)]
                wpp = sb(ph, f"wpp{i}", [128, 2, D], BF16)
                R_wpp = Region()
                ds_wpp = fw.dsem(f"ds_wpp{i}")
                fw.dma("pool", wpp[:], w_pp[i, :, :].rearrange("(kc q) n -> q kc n", q=128), ds_wpp, writes=[R_wpp])
                k = 0
                for g in range(NG):
                    for pc in range(2):
                        bank = k % 2
                        k += 1
                        for t in range(4):
                            op("pe", lambda e: e.transpose(ps[:, bank, t * 128:(t + 1) * 128], pt[:, 4 * g + t, pc * 128:(pc + 1) * 128], ident_f[:]),
                               reads=[R_p, R_const], writes=[psr[bank]], track=(t == 3))
                        op("dve", lambda e: e.tensor_copy(pT[:, pc, gc(g)], ps[:, bank, :]), reads=[psr[bank]], writes=[R_pT[g]])
                wpgs = Stream(ph, f"wpg{i}", [128, NCH, 128], BF16, 2)
                sgt = [sb(ph, f"psg{i}{j}", [128, 512], F32) for j in range(2)]
                R_sg = [Region(), Region()]
                tmp = [sb(ph, f"ptmp{i}{j}", [128, 512], F32) for j in range(2)]
                R_tmp = [Region(), Region()]
                wpg_v = kview(w_pg[i, :, :])
                nxt = wload(wpgs, lambda t: t[:], wpg_v[:, :, 0:128])
                k = 0
                for o in range(NCH):
                    wg, wgr = nxt
                    if o + 1 < NCH:
                        nxt = wload(wpgs, lambda t: t[:], wpg_v[:, :, (o + 1) * 128:(o + 2) * 128])
                    for g in range(NG):
                        gb = 2 + (k % 2) * 2
                        pb = gb + 1
                        s = k % 2
                        k += 1
                        for kc in range(NCH):
                            mm(ps[:, gb, :], wg[:, kc, :], T["aT"][:, kc, gc(g)], kc == 0, kc == NCH - 1, [wgr, R_a[kc][g]], [psr[gb]])
                        for kc in range(2):
                            mm(ps[:, pb, :], wpp[:, kc, o * 128:(o + 1) * 128], pT[:, kc, gc(g)], kc == 0, kc == 1, [R_wpp, R_pT[g]], [psr[pb]])
                        op("act", lambda e: e.activation(out=sgt[s][:], in_=ps[:, gb, :], func=AF.Sigmoid), reads=[psr[gb]], writes=[R_sg[s]])
                        op("dve", lambda e: e.tensor_tensor(out=tmp[s][:], in0=ps[:, pb, :], in1=sgt[s][:], op=ALU.mult),
                           reads=[psr[pb], R_sg[s]], writes=[R_tmp[s]])
                        op("pool", lambda e: e.tensor_tensor(out=hT[:, o, gc(g)], in0=tmp[s][:], in1=hT[:, o, gc(g)], op=ALU.add),
                           reads=[R_tmp[s], R_h[o][g]], writes=[R_h[o][g]])
                fw.barrier()

        ple(0)
        if dump("ple0"):
            return nc

        phB = contextlib.ExitStack()
        KT = sb(phB, "KT", [128, NCH, S], BF16)
        Vt = T["Vt"] = sb(phB, "VtB", [128, 16, D], BF16)
        maskS = make_mask(phB, "maskS", ALU.is_le)
        alloc_aT(phB)
        R_k = [[Region() for _ in range(NG)] for _ in range(NCH)]
        with contextlib.ExitStack() as ph:
            rmsnorm(2, ph)
            fw.barrier()
        with contextlib.ExitStack() as ph:
            wks = Stream(ph, "wkk", [128, NCH, 128], BF16, 2)
            wkv_v = kview(w_kv)
            k = 0
            for pr in range(NCH):
                wk, wkr = wload(wks, lambda t: t[:], wkv_v[:, :, pr * 128:(pr + 1) * 128])
                for g in range(NG):
                    bank = 4 + k % 4
                    k += 1
                    for kc in range(NCH):
                        mm(ps[:, bank, :], wk[:, kc, :], T["aT"][:, kc, gc(g)], kc == 0, kc == NCH - 1, [wkr, R_a[kc][g]], [psr[bank]])
                    if k % 2 == 0:
                        op("act", lambda e: e.activation(out=KT[:, pr, gc(g)], in_=ps[:, bank, :], func=AF.Copy), reads=[psr[bank]], writes=[R_k[pr][g]])
                    else:
                        op("dve", lambda e: e.tensor_copy(KT[:, pr, gc(g)], ps[:, bank, :]), reads=[psr[bank]], writes=[R_k[pr][g]])
            v_proj(ph, w_kv, D, 0.0)
            fw.barrier()

        with contextlib.ExitStack() as ph:
            rmsnorm(3, ph)
            fw.barrier()
        with contextlib.ExitStack() as ph:
            wqs = Stream(ph, "wq1", [128, NCH, 128], BF16, 2)
            wos = Stream(ph, "wo1", [128, D], BF16, 2)
            qz = [[sb(ph, f"qz{b}{hh}", [128, S], BF16) for hh in range(2)] for b in range(2)]
            R_q = [[[Region() for _ in range(NG)] for hh in range(2)] for _ in range(2)]
            R_qz = Region()
            for b_ in range(2):
                for hh_ in range(2):
                    op("pool", lambda e: e.memset(qz[b_][hh_][:], 0.0), writes=[R_qz])
            et = [sb(ph, f"et{i}", [128, 512], F32) for i in range(2)]
            R_e = [Region(), Region()]
            SPt = [sb(ph, f"SPt{i}", [128, 512], BF16) for i in range(3)]
            R_sp = [Region() for _ in range(3)]
            Wt = [sb(ph, f"Wt{i}", [128, 512], BF16) for i in range(2)]
            R_w = [Region(), Region()]
            Rsb = [sb(ph, f"Rsb{i}", [128, 512], BF16) for i in range(2)]
            R_r = [Region(), Region()]
            _ot1 = sb(ph, "OT1p", [128, S], BF16)
            OTp = [_ot1, _ot1]
            _rot1 = [Region() for _ in range(NG)]
            R_ot = [_rot1, _rot1]
            wq_v = kview(w_q_b)

            def qproj(pr):
                b = pr % 2
                wq, wqr = wload(wqs, lambda t: t[:], wq_v[:, :, pr * 128:(pr + 1) * 128])
                for g in range(NG):
                    bank = 7
                    for kc in range(NCH):
                        mm(ps[:, bank, :], wq[:, kc, :], T["aT"][:, kc, gc(g)], kc == 0, kc == NCH - 1, [wqr, R_a[kc][g]], [psr[bank]])
                    for hh_ in range(2):
                        hp_ = slice(hh_ * 64, (hh_ + 1) * 64)
                        op("dve", lambda e: e.tensor_scalar(qz[b][hh_][hp_, gc(g)], ps[hp_, bank, :], 0.125, None, ALU.mult),
                           reads=[psr[bank], R_qz], writes=[R_q[b][hh_][g]])

            cn = dict(a=0, b=0, e=0, sp=0, w=0, r=0)
            qproj(0)
            for pr in range(NH // 2):
                b = pr % 2
                wo, wor = wload(wos, lambda t: t[:], w_o_b[pr * 128:(pr + 1) * 128, :])
                for g in range(NG):
                    accb = 4 + (g % 2)
                    for hh in range(2):
                        h = 2 * pr + hh
                        hp = slice(hh * 64, (hh + 1) * 64)
                        nj = 4 * g + 4

                        def z_mm(bank, j, stop, track):
                            diag = j >= 4 * g
                            jc = slice(j * 128, (j + 1) * 128)
                            mm(ps[:, bank, :], KT[:, pr, jc], qz[b][hh][:, gc(g)], True, stop and not diag,
                               [R_k[pr][j // 4], R_q[b][hh][g]], [psr[bank]], track=(track and not diag))
                            if diag:
                                mm(ps[:, bank, :], negbigI_b[:], maskS[:, j - 4 * g, :], False, stop, [R_const], [psr[bank]], track=track)

                        def stageA(j):
                            A = cn["a"] % 2
                            cn["a"] += 1
                            z_mm(A, j, True, True)
                            e_i = cn["e"] % 2
                            cn["e"] += 1
                            op("act", lambda e: e.activation(out=et[e_i][:], in_=ps[:, A, :], func=AF.Exp), reads=[psr[A]], writes=[R_e[e_i]])
                            s_i = cn["sp"] % 3
                            cn["sp"] += 1
                            op("act", lambda e: e.activation(out=SPt[s_i][:], in_=et[e_i][:], func=AF.Ln, bias=1.0, scale=1.0),
                               reads=[R_e[e_i]], writes=[R_sp[s_i]])
                            return s_i

                        js = list(range(nj - 1, -1, -1))
                        nt = len(js)
                        sp_of = {0: stageA(js[0])}
                        if nt > 1:
                            sp_of[1] = stageA(js[1])
                        pend = None

                        def emit_pv(pv):
                            pj, pw, pfirst = pv
                            mm(ps[:, 4 + hh, :], Vt[:, pj, pr * 128:(pr + 1) * 128], Wt[pw][:], pfirst, pj == 0,
                               [R_v[pj], R_w[pw]], [psr[4 + hh]], track=True)

                        for idx, j in enumerate(js):
                            first = idx == 0
                            if idx + 2 < nt:
                                sp_of[idx + 2] = stageA(js[idx + 2])
                            s_i = sp_of[idx]
                            r_prev = (cn["r"] - 1) % 2
                            if j > 0:
                                mm(ps[:, 6, :], ones_b[:], SPt[s_i][:], True, True, [R_const, R_sp[s_i]], [psr[6]], track=True)
                                r_i = cn["r"] % 2
                                cn["r"] += 1
                                if first:
                                    op("dve", lambda e: e.tensor_copy(Rsb[r_i][:], ps[:, 6, :]), reads=[psr[6]], writes=[R_r[r_i]])
                                else:
                                    op("dve", lambda e: e.tensor_tensor(out=Rsb[r_i][:], in0=ps[:, 6, :], in1=Rsb[r_prev][:], op=ALU.add),
                                       reads=[psr[6], R_r[r_prev]], writes=[R_r[r_i]])
                            B = 2 + (cn["b"] % 2)
                            cn["b"] += 1
                            z_mm(B, j, False, False)
                            mm(ps[:, B, :], negtri_b[:], SPt[s_i][:], False, first, [R_const, R_sp[s_i]], [psr[B]], track=first)
                            if not first:
                                mm(ps[:, B, :], negI_b[:], Rsb[r_prev][:], False, True, [R_const, R_r[r_prev]], [psr[B]])
                            w_i = cn["w"] % 2
                            cn["w"] += 1
                            op("act", lambda e: e.activation(out=Wt[w_i][:], in_=ps[:, B, :], func=AF.Exp), reads=[psr[B]], writes=[R_w[w_i]])
                            if pend is not None:
                                emit_pv(pend)
                            pend = (j, w_i, first)
                        emit_pv(pend)
                        op("dve", lambda e: e.tensor_copy(OTp[b][hp, gc(g)], ps[hp, 4 + hh, :]), reads=[psr[4 + hh]], writes=[R_ot[b][g]])
                    if g == 1 and pr + 1 < NH // 2:
                        qproj(pr + 1)
                out_proj_pair(wo, wor, OTp[b], R_ot[b], [7])
            fw.barrier()
        phB.close()
        if dump("attn1"):
            return nc

        CAP = 384
        NSC = CAP // 128
        with contextlib.ExitStack() as ph:
            G = sb(ph, "G", [128, 16, NE], F32)
            R_G = Region()
            GT = sb(ph, "GT", [NE, S], BF16)
            R_GT = Region()
            sel = sb(ph, "sel", [NE, NE, 128], BF16)
            R_sel = Region()
            f_tok = sb(ph, "f_tok", [128, 16, D], BF16)
            R_ft = [Region() for _ in range(16)]
            posT = sb(ph, "posT", [NE, 2, S], BF16)
            R_posT = Region()
            pos = sb(ph, "pos", [128, 16, NE], F32)
            Mf = sb(ph, "Mf", [128, 16, NE], F32)
            R_pos = Region()
            iotaC = sb(ph, "iotaC", [128, CAP], F32)
            slotid = sb(ph, "slotid", [128, NSC], F32)
            R_io = Region()
            with contextlib.ExitStack() as ph2:
                alloc_aT(ph2)
                aT = T["aT"]
                wr_f = sb(ph2, "wr_f", [128, NCH, NE], F32)
                wr_hi = sb(ph2, "wr_hi", [128, NCH, NE], BF16)
                wr_lo = sb(ph2, "wr_lo", [128, NCH, NE], BF16)
                R_wr = Region()
                ds_wr = fw.dsem("ds_wr")
                fw.dma("sp", wr_f[:], kview(r_w), ds_wr, writes=[R_wr])
                op("dve", lambda e: e.tensor_copy(wr_hi[:], wr_f[:]), reads=[R_wr], writes=[R_wr])
                op("dve", lambda e: e.tensor_tensor(out=wr_f[:], in0=wr_f[:], in1=wr_hi[:], op=ALU.subtract), reads=[R_wr], writes=[R_wr])
                op("dve", lambda e: e.tensor_copy(wr_lo[:], wr_f[:]), reads=[R_wr], writes=[R_wr])
                fT32 = sb(ph2, "fT32", [128, 512], F32)
                R_f32 = Region()
                loT = [sb(ph2, f"loT{i}", [128, NCH, 512], BF16) for i in range(2)]
                R_lo = [Region(), Region()]
                lg = sb(ph2, "lg", [128, 16, NE], F32)
                R_lg = Region()
                mx = sb(ph2, "mx", [128, 8], F32)
                nv1 = sb(ph2, "nv1", [128, 1], F32)
                msk = sb(ph2, "msk", [128, NE], F32)
                eg = sb(ph2, "eg", [128, NE], F32)
                den = sb(ph2, "den", [128, 1], F32)
                R_s = Region()
                io2 = sb(ph2, "io2", [NE, NE, 128], I32)
                op("pool", lambda e: e.iota(io2[:], [[-1, NE], [0, 128]], base=0, channel_multiplier=1), writes=[R_sel])
                op("dve", lambda e: e.tensor_scalar(sel[:], io2[:], 0.0, None, ALU.is_equal), reads=[R_sel], writes=[R_sel])
                io3 = sb(ph2, "io3", [128, CAP], I32)
                op("pool", lambda e: e.iota(io3[:], [[1, CAP]], base=0, channel_multiplier=0), writes=[R_io])
                op("dve", lambda e: e.tensor_copy(iotaC[:], io3[:]), reads=[R_io], writes=[R_io])
                op("pool", lambda e: e.iota(io3[:, 0:NSC], [[128, NSC]], base=0, channel_multiplier=1), reads=[R_io], writes=[R_io])
                op("dve", lambda e: e.tensor_copy(slotid[:], io3[:, 0:NSC]), reads=[R_io], writes=[R_io])

                def norm_out(g, m, rstd_t, R_rs):
                    b = g % 2
                    op("dve", lambda e: e.scalar_tensor_tensor(out=fT32[:], in0=hT[:, m, gc(g)], scalar=gam[:, 4 * 8 + m:4 * 8 + m + 1],
                                                               in1=rstd_t[:], op0=ALU.mult, op1=ALU.mult),
                       reads=[R_h[m][g], R_rs], writes=[R_f32])
                    op("dve", lambda e: e.tensor_copy(aT[:, m, gc(g)], fT32[:]), reads=[R_f32], writes=[R_a[m][g]])
                    op("dve", lambda e: e.tensor_tensor(out=loT[b][:, m, :], in0=fT32[:], in1=aT[:, m, gc(g)], op=ALU.subtract),
                       reads=[R_f32, R_a[m][g]], writes=[R_lo[b]])

                def route(g):
                    b = g % 2
                    for t in range(4):
                        tt = 4 * g + t
                        tc_ = slice(t * 128, (t + 1) * 128)
                        bank = 4 + (tt % 2)
                        n = 0
                        for kc in range(NCH):
                            for (l, r_, rr) in ((aT[:, kc, tt * 128:(tt + 1) * 128], wr_hi, R_a[kc][g]),
                                                (loT[b][:, kc, tc_], wr_hi, R_lo[b]),
                                                (aT[:, kc, tt * 128:(tt + 1) * 128], wr_lo, R_a[kc][g])):
                                mm(ps[:, bank, 0:NE], l, r_[:, kc, :], n == 0, n == 3 * NCH - 1, [rr, R_wr], [psr[bank]])
                                n += 1
                        op("dve", lambda e: e.tensor_tensor(out=lg[:, tt, :], in0=ps[:, bank, 0:NE], in1=rb[:], op=ALU.add),
                           reads=[psr[bank], R_const], writes=[R_lg])
                        op("dve", lambda e: e.max(mx[:], lg[:, tt, :]), reads=[R_lg], writes=[R_s])
                        op("dve", lambda e: e.tensor_scalar(nv1[:], mx[:, 0:1], -1.0, None, ALU.mult), reads=[R_s], writes=[R_s])
                        op("dve", lambda e: e.tensor_scalar(msk[:], lg[:, tt, :], mx[:, 1:2], None, ALU.is_ge), reads=[R_lg, R_s], writes=[R_s])
                        op("dve", lambda e: e.tensor_copy(Mf[:, tt, :], msk[:]), reads=[R_s], writes=[R_pos])
                        op("act", lambda e: e.activation(out=eg[:], in_=lg[:, tt, :], func=AF.Exp, bias=nv1[:, 0:1], scale=1.0),
                           reads=[R_lg, R_s], writes=[R_s])
                        op("dve", lambda e: e.tensor_tensor(out=eg[:], in0=eg[:], in1=msk[:], op=ALU.mult), reads=[R_s], writes=[R_s])
                        op("dve", lambda e: e.tensor_reduce(out=den[:], in_=eg[:], axis=mybir.AxisListType.X, op=ALU.add), reads=[R_s], writes=[R_s])
                        op("dve", lambda e: e.reciprocal(out=den[:], in_=den[:]), reads=[R_s], writes=[R_s])
                        op("dve", lambda e: e.tensor_scalar(G[:, tt, :], eg[:], den[:, 0:1], None, ALU.mult), reads=[R_s], writes=[R_G])
                        for mq in range(2):
                            fb = 2 * (tt % 2) + mq
                            for mi in range(4):
                                m = mq * 4 + mi
                                mm(ps[:, fb, mi * 128:(mi + 1) * 128], aT[:, m, tt * 128:(tt + 1) * 128], negI_b[:], True, True,
                                   [R_a[m][g], R_const], [psr[fb]], track=(mi == 3))
                            op("dve", lambda e: e.tensor_scalar(f_tok[:, tt, mq * 512:(mq + 1) * 512], ps[:, fb, :], -1.0, None, ALU.mult),
                               reads=[psr[fb]], writes=[R_ft[tt]])

                rmsnorm(4, ph2, out_fn=norm_out, post=route)
                for g in range(NG):
                    bank = g % 2
                    for t in range(4):
                        op("pe", lambda e: e.transpose(ps[0:NE, bank, t * 128:(t + 1) * 128], G[:, 4 * g + t, :], ident_f[:]),
                           reads=[R_G, R_const], writes=[psr[bank]], track=(t == 3))
                    op("dve", lambda e: e.tensor_copy(GT[:, gc(g)], ps[0:NE, bank, :]), reads=[psr[bank]], writes=[R_GT])
                Mk = sb(ph2, "Mk", [128, 16 * NE], BF16)
                tot = sb(ph2, "tot", [128, 16, NE], F32)
                offs = sb(ph2, "offs", [128, 16, NE], F32)
                stri_b = sb(ph2, "stri_b", [128, 128], BF16)
                io4 = sb(ph2, "io4", [128, 128], I32)
                op("pool", lambda e: e.iota(io4[:], [[1, 128]], base=0, channel_multiplier=-1), writes=[R_io])
                op("dve", lambda e: e.tensor_scalar(stri_b[:], io4[:], 0.0, None, ALU.is_gt), reads=[R_io], writes=[R_io])
                op("dve", lambda e: e.tensor_copy(Mk[:], Mf[:].rearrange("p a b -> p (a b)")), reads=[R_pos], writes=[R_pos])
                mm(ps[:, 4, 0:128], stri_b[:], Mk[:], True, True, [R_io, R_pos], [psr[4]])
                mm(ps[:, 5, 0:128], ones_b[:], Mk[:], True, True, [R_const, R_pos], [psr[5]])
                op("dve", lambda e: e.tensor_copy(tot[:].rearrange("p a b -> p (a b)"), ps[:, 5, 0:128]), reads=[psr[5]], writes=[R_pos])
                op("dve", lambda e: e.memset(offs[:], 0.0), writes=[R_pos])
                for k_ in list(range(1, 8)) + list(range(9, 16)):
                    op("dve", lambda e: e.tensor_tensor(out=offs[:, k_, :], in0=offs[:, k_ - 1, :], in1=tot[:, k_ - 1, :], op=ALU.add),
                       reads=[R_pos], writes=[R_pos])
                op("dve", lambda e: e.tensor_tensor(out=pos[:].rearrange("p a b -> p (a b)"), in0=ps[:, 4, 0:128],
                                                    in1=offs[:].rearrange("p a b -> p (a b)"), op=ALU.add),
                   reads=[psr[4], R_pos], writes=[R_pos])
                for g in range(NG):
                    bank = 6 + g % 2
                    for t in range(4):
                        op("pe", lambda e: e.transpose(ps[0:NE, bank, t * 128:(t + 1) * 128], pos[:, 4 * g + t, :], ident_f[:]),
                           reads=[R_pos, R_const], writes=[psr[bank]], track=(t == 3))
                    op("dve", lambda e: e.tensor_copy(posT[:, 0, gc(g)], ps[0:NE, bank, :]), reads=[psr[bank]], writes=[R_posT])
                    op("dve", lambda e: e.tensor_tensor(out=posT[:, 1, gc(g)], in0=ps[0:NE, bank, :], in1=posT[:, 0, gc(g)], op=ALU.subtract),
                       reads=[psr[bank], R_posT], writes=[R_posT])
                fw.barrier()
            with contextlib.ExitStack() as ph2:
                NKF = FE // 128
                wgs = Stream(ph2, "wgm", [128, NCH, 256], BF16, 3)
                wus = Stream(ph2, "wum", [128, NCH, 256], BF16, 3)
                wds = Stream(ph2, "wdm", [128, 4, 512], BF16, 3)
                Se = sb(ph2, "Se", [128, 8, CAP], BF16)
                R_se = [Region() for _ in range(8)]
                Xg = sb(ph2, "Xg", [128, NCH, CAP], BF16)
                R_xg = [Region() for _ in range(NCH)]
                actT = sb(ph2, "actTm", [128, NKF, CAP], BF16)
                R_act = [Region() for _ in range(NKF)]
                Ysm = [sb(ph2, f"Ysm{i}", [128, NSC, 512], BF16) for i in range(2)]
                R_y = [Region(), Region()]
                GS = sb(ph2, "GS", [128, NSC, 1024], BF16)
                R_gs = [Region(), Region()]
                Gb = sb(ph2, "Gb", [128, 1024], BF16)
                R_gb = [Region(), Region()]
                sgt = [sb(ph2, f"sgm{i}", [128, CAP], BF16) for i in range(2)]
                R_sg = [Region(), Region()]
                kk = dict(b=0, s=0, d=0, y=0, e=0)
                eqt = [sb(ph2, f"eqt{i}", [128, 512], BF16) for i in range(2)]
                R_eq = [Region(), Region()]
                items = [(half, ex) for half in range(2) for ex in range(NE)]
                pre = {}

                def prefetch_gu(it):
                    half, ex = it
                    wg_v = kview(w_gu_m[ex, :, :])
                    pre[it] = (wload(wgs, lambda t: t[:], wg_v[:, :, 0:256]), wload(wus, lambda t: t[:], wg_v[:, :, FE:FE + 256]))

                def build_Se(it):
                    half, ex = it
                    for ti in range(8):
                        tt = half * 8 + ti
                        op("dve", lambda e: e.tensor_scalar(Se[:, ti, :], iotaC[:], pos[:, tt, ex:ex + 1], Mf[:, tt, ex:ex + 1],
                                                            ALU.is_equal, ALU.mult),
                           reads=[R_io, R_pos], writes=[R_se[ti]])

                def gather(it):
                    half, ex = it
                    for m in range(NCH):
                        bank = m % 4
                        for ti in range(8):
                            tt = half * 8 + ti
                            mm(ps[:, bank, 0:CAP], f_tok[:, tt, m * 128:(m + 1) * 128], Se[:, ti, :], ti == 0, ti == 7,
                               [R_ft[tt], R_se[ti]], [psr[bank]])
                        if m % 2 == 0:
                            op("act", lambda e: e.activation(out=Xg[:, m, :], in_=ps[:, bank, 0:CAP], func=AF.Copy), reads=[psr[bank]], writes=[R_xg[m]])
                        else:
                            op("dve", lambda e: e.tensor_copy(Xg[:, m, :], ps[:, bank, 0:CAP]), reads=[psr[bank]], writes=[R_xg[m]])

                def build_GS(it):
                    half, ex = it
                    for gg in range(2):
                        g = half * 2 + gg
                        mm(ps[:, 4 + gg, :], sel[:, ex, :], posT[:, 0, gc(g)], True, False, [R_sel, R_posT], [psr[4 + gg]], track=False)
                        mm(ps[:, 4 + gg, :], sel[:, ex, :], posT[:, 1, gc(g)], False, True, [R_sel, R_posT], [psr[4 + gg]])
                        mm(ps[:, 6 + gg, :], sel[:, ex, :], GT[:, gc(g)], True, True, [R_sel, R_GT], [psr[6 + gg]])
                        op("act", lambda e: e.activation(out=Gb[:, gg * 512:(gg + 1) * 512], in_=ps[:, 6 + gg, :], func=AF.Copy),
                           reads=[psr[6 + gg]], writes=[R_gb[gg]])
                        for sc in range(NSC):
                            e_i = kk["e"] % 2
                            kk["e"] += 1
                            op("dve", lambda e: e.tensor_scalar(eqt[e_i][:], ps[:, 4 + gg, :], slotid[:, sc:sc + 1], None, ALU.is_equal),
                               reads=[psr[4 + gg], R_io], writes=[R_eq[e_i]])
                            op("dve", lambda e: e.tensor_tensor(out=GS[:, sc, gg * 512:(gg + 1) * 512], in0=eqt[e_i][:],
                                                                 in1=Gb[:, gg * 512:(gg + 1) * 512], op=ALU.mult),
                               reads=[R_eq[e_i], R_gb[gg]], writes=[R_gs[gg]])

                def gu(it):
                    half, ex = it
                    wg_v = kview(w_gu_m[ex, :, :])
                    nxt = pre.pop(it)
                    for mb in range(FE // 256):
                        (wg, wgr), (wu, wur) = nxt
                        if mb + 1 < FE // 256:
                            c0 = (mb + 1) * 256
                            nxt = (wload(wgs, lambda t: t[:], wg_v[:, :, c0:c0 + 256]),
                                   wload(wus, lambda t: t[:], wg_v[:, :, FE + c0:FE + c0 + 256]))
                        for mi in range(2):
                            m = mb * 2 + mi
                            gb = (kk["b"] % 2) * 2
                            ub = gb + 1
                            kk["b"] += 1
                            for kc in range(NCH):
                                mm(ps[:, gb, 0:CAP], wg[:, kc, mi * 128:(mi + 1) * 128], Xg[:, kc, :], kc == 0, kc == NCH - 1,
                                   [wgr, R_xg[kc]], [psr[gb]])
                            for kc in range(NCH):
                                mm(ps[:, ub, 0:CAP], wu[:, kc, mi * 128:(mi + 1) * 128], Xg[:, kc, :], kc == 0, kc == NCH - 1,
                                   [wur, R_xg[kc]], [psr[ub]])
                            s_ = kk["s"] % 2
                            kk["s"] += 1
                            op("act", lambda e: e.activation(out=sgt[s_][:], in_=ps[:, gb, 0:CAP], func=AF.Silu), reads=[psr[gb]], writes=[R_sg[s_]])
                            op("dve", lambda e: e.tensor_tensor(out=actT[:, m, :], in0=ps[:, ub, 0:CAP], in1=sgt[s_][:], op=ALU.mult),
                               reads=[psr[ub], R_sg[s_]], writes=[R_act[m]])

                def down(it, oh, banks):
                    half, ex = it
                    wd_v = kview(w_dn_m[ex, :, :])
                    for k4 in range(NKF // 4):
                        wd, wdr = wload(wds, lambda t: t[:], wd_v[:, 4 * k4:4 * k4 + 4, oh * 512:(oh + 1) * 512])
                        for kq in range(4):
                            kc = 4 * k4 + kq
                            for sc in range(NSC):
                                mm(ps[:, banks[sc], :], actT[:, kc, sc * 128:(sc + 1) * 128], wd[:, kq, :], kc == 0, kc == NKF - 1,
                                   [R_act[kc], wdr], [psr[banks[sc]]], track=(kc == NKF - 1 or (kq == 3 and sc == NSC - 1)))

                def evac_y(oh, banks):
                    for sc in range(NSC):
                        if sc % 2 == 0:
                            op("act", lambda e: e.activation(out=Ysm[oh][:, sc, :], in_=ps[:, banks[sc], :], func=AF.Copy), reads=[psr[banks[sc]]], writes=[R_y[oh]])
                        else:
                            op("dve", lambda e: e.tensor_copy(Ysm[oh][:, sc, :], ps[:, banks[sc], :]), reads=[psr[banks[sc]]], writes=[R_y[oh]])

                def scatter(it, oh, banks):
                    half, ex = it
                    for o4 in range(4):
                        o = oh * 4 + o4
                        for gg in range(2):
                            g = half * 2 + gg
                            bank = banks[kk["d"] % len(banks)]
                            kk["d"] += 1
                            for sc in range(NSC):
                                mm(ps[:, bank, :], Ysm[oh][:, sc, o4 * 128:(o4 + 1) * 128], GS[:, sc, gg * 512:(gg + 1) * 512],
                                   sc == 0, sc == NSC - 1, [R_y[oh], R_gs[gg]], [psr[bank]])
                            op("dve", lambda e: e.tensor_tensor(out=hT[:, o, gc(g)], in0=ps[:, bank, :], in1=hT[:, o, gc(g)], op=ALU.add),
                               reads=[psr[bank], R_h[o][g]], writes=[R_h[o][g]])

                prefetch_gu(items[0])
                build_Se(items[0])
                gather(items[0])
                for i, it in enumerate(items):
                    nx = items[i + 1] if i + 1 < len(items) else None
                    if nx is not None:
                        build_Se(nx)
                    gu(it)
                    if nx is not None:
                        prefetch_gu(nx)
                    build_GS(it)
                    down(it, 0, [4, 5, 6])
                    evac_y(0, [4, 5, 6])
                    if nx is not None:
                        gather(nx)
                    down(it, 1, [7, 0, 1])
                    scatter(it, 0, [2, 3])
                    evac_y(1, [7, 0, 1])
                    scatter(it, 1, [4, 5])
                fw.barrier()
        if dump("moe"):
            return nc

        ple(1)
        if dump("ple1"):
            return nc

        with contextlib.ExitStack() as ph:
            nrm = [sb(ph, f"nrm{i}", [128, NCH, 512], F32) for i in range(2)]
            R_n = [Region(), Region()]
            ot = [sb(ph, f"ot{i}", [128, D], F32) for i in range(2)]
            R_o = [Region(), Region()]
            ds_o = [fw.dsem("ds_o0"), fw.dsem("ds_o1")]
            cnt = [0]
            last_tok = []

            def norm_out(g, m, rstd_t, R_rs):
                b = g % 2
                op("dve", lambda e: e.scalar_tensor_tensor(out=nrm[b][:, m, :], in0=hT[:, m, gc(g)], scalar=gam[:, 5 * 8 + m:5 * 8 + m + 1],
                                                           in1=rstd_t[:], op0=ALU.mult, op1=ALU.mult),
                   reads=[R_h[m][g], R_rs], writes=[R_n[b]])

            def store(g):
                b = g % 2
                for t in range(4):
                    o_i = cnt[0] % 2
                    cnt[0] += 1
                    for hf in range(2):
                        bank = 2 * o_i + hf
                        for mi in range(4):
                            m = hf * 4 + mi
                            op("pe", lambda e: e.transpose(ps[:, bank, mi * 128:(mi + 1) * 128], nrm[b][:, m, t * 128:(t + 1) * 128], ident_f[:]),
                               reads=[R_n[b], R_const], writes=[psr[bank]], track=(mi == 3))
                        if hf == 0:
                            op("act", lambda e: e.activation(out=ot[o_i][:, 0:512], in_=ps[:, bank, :], func=AF.Copy), reads=[psr[bank]], writes=[R_o[o_i]])
                        else:
                            op("dve", lambda e: e.tensor_copy(ot[o_i][:, 512:1024], ps[:, bank, :]), reads=[psr[bank]], writes=[R_o[o_i]])
                    tok = fw.dma("sp", out[g * 512 + t * 128: g * 512 + (t + 1) * 128, :], ot[o_i][:], ds_o[o_i], reads=[R_o[o_i]])
                    last_tok.append(tok)

            rmsnorm(5, ph, out_fn=norm_out, post=store)
            for tok in last_tok[-2:]:
                fw._wait("sp", tok)
            fw.barrier()
    return nc


_W_KEYS = [("w_in", "w_in_a", 0), ("w_o_a", "w_o_a", 0), ("w_kv", "w_kv", None), ("w_q_b", "w_q_b", 0), ("w_o_b", "w_o_b", 0),
           ("w_gu_d", "w_gu_dense", 0), ("w_dn_d", "w_down_dense", 0), ("r_w", "router_w", 0), ("w_gu_m", "w_gu_moe", 0),
           ("w_dn_m", "w_down_moe", 0), ("w_pp", "w_ple_proj", None), ("w_pg", "w_ple_gate", None)]


def make_in_maps(inputs):
    f = lambda a: np.ascontiguousarray(np.asarray(a, dtype=np.float32))
    shared = {}
    for dst, src, idx in _W_KEYS:
        a = f(inputs[src])
        shared[dst] = f(a[idx]) if idx is not None else a
    norms = [f(inputs["attn_norm"])[0], f(inputs["ffn_norm"])[0], f(inputs["kv_norm"]), f(inputs["attn_norm"])[1],
             f(inputs["ffn_norm"])[1], f(inputs["final_norm"])]
    gam = np.stack([n.reshape(8, 128).T for n in norms], axis=1).reshape(128, 48)
    shared["gam"] = f(gam)
    shared["bf"] = f(inputs["b_f"]).reshape(16, 1)
    shared["rb"] = f(np.broadcast_to(f(inputs["router_b"]).reshape(1, 8), (128, 8)))
    x = f(inputs["x"])
    p = f(inputs["p"])
    maps = []
    for c in range(8):
        m = dict(shared)
        m["x"] = f(x[c])
        m["p"] = f(p[:, c])
        maps.append(m)
    return maps


def kernel(**inputs):
    nc = build()
    maps = make_in_maps(inputs)
    res = run_bass_kernel_spmd(nc, maps, core_ids=list(range(8)))
    return np.stack([np.asarray(res.results[c]["out"], dtype=np.float32) for c in range(8)], axis=0)
```

```python
import contextlib
import numpy as np
import concourse.bass as bass
import concourse.mybir as mybir
from concourse.bass_utils import run_bass_kernel_spmd

F32 = mybir.dt.float32
BF16 = mybir.dt.bfloat16
I32 = mybir.dt.int32
AF = mybir.ActivationFunctionType
ALU = mybir.AluOpType

S = 2048
D = 1024
NH = 16
FF = 2816
FE = 3584
NE = 8
NG = 4
NCH = 8
EPS = 1e-6
NEGBIG = -30000.0

DEBUG_STOP = None


class Region:
    __slots__ = ("w", "r")

    def __init__(self):
        self.w = None
        self.r = []


class FW:
    def __init__(self, nc, es):
        self.nc = nc
        self.es = es
        self.eng = {"pe": nc.tensor, "act": nc.scalar, "dve": nc.vector, "pool": nc.gpsimd, "sp": nc.sync}
        self.sem = {k: es.enter_context(nc.semaphore("s_" + k)) for k in self.eng}
        self.cnt = {k: 0 for k in self.eng}
        self.waited = {k: {} for k in self.eng}
        self.dsems = []

    def dsem(self, name):
        h = {"sem": self.es.enter_context(self.nc.semaphore(f"{name}_{len(self.dsems)}")), "val": 0}
        self.dsems.append(h)
        return h

    def _wait(self, e, tok):
        sem, val, owner = tok
        if owner is not None:
            if owner == "pe" and e == "pe":
                return
            assert val <= self.cnt[owner], f"wait on future token {owner}:{val} > {self.cnt[owner]}"
        key = id(sem)
        if self.waited[e].get(key, 0) >= val:
            return
        self.waited[e][key] = val
        self.eng[e].wait_ge(sem, val)

    def _deps(self, e, reads, writes):
        for r in reads:
            if r.w is not None:
                self._wait(e, r.w)
        for w in writes:
            if w.w is not None:
                self._wait(e, w.w)
            for t in w.r:
                self._wait(e, t)

    def _record(self, tok, reads, writes):
        for r in reads:
            r.r.append(tok)
            if len(r.r) > 64:
                best = {}
                for t in r.r:
                    k = id(t[0])
                    if k not in best or best[k][1] < t[1]:
                        best[k] = t
                r.r = list(best.values())
        for w in writes:
            w.w = tok
            w.r = []

    def op(self, e, fn, reads=(), writes=(), track=True):
        self._deps(e, reads, writes)
        inst = fn(self.eng[e])
        if track:
            self.cnt[e] += 1
            inst.then_inc(self.sem[e], 1)
            tok = (self.sem[e], self.cnt[e], e)
        else:
            tok = (self.sem[e], self.cnt[e] + 1, e)
        self._record(tok, reads, writes)
        return inst

    def dma(self, q, out, in_, ds, reads=(), writes=(), **kw):
        self._deps(q, reads, writes)
        inst = self.eng[q].dma_start(out=out, in_=in_, **kw)
        ds["val"] += 16
        inst.then_inc(ds["sem"], 16)
        tok = (ds["sem"], ds["val"], None)
        self._record(tok, reads, writes)
        return tok

    def barrier(self):
        for e in self.eng:
            for o in self.eng:
                if self.cnt[o] > 0:
                    self._wait(e, (self.sem[o], self.cnt[o], None))
            for ds in self.dsems:
                if ds["val"] > 0:
                    self._wait(e, (ds["sem"], ds["val"], None))


def build(stop=None):
    nc = bass.Bass("TRN2", target_bir_lowering=False)

    def din(name, shape, dt=F32):
        return nc.dram_tensor(name, list(shape), dt, kind="ExternalInput").ap()

    x = din("x", [S, D])
    p = din("p", [2, S, 256])
    gam_d = din("gam", [128, 48])
    bf_d = din("bf", [16, 1])
    rb_d = din("rb", [128, 8])
    w_in = din("w_in", [D, 3 * D + NH])
    w_o_a = din("w_o_a", [D, D])
    w_kv = din("w_kv", [D, 2 * D])
    w_q_b = din("w_q_b", [D, D])
    w_o_b = din("w_o_b", [D, D])
    w_gu_d = din("w_gu_d", [D, 2 * FF])
    w_dn_d = din("w_dn_d", [FF, D])
    r_w = din("r_w", [D, NE])
    lite = stop is not None and stop not in ("moe", "ple1")
    w_gu_m = din("w_gu_m", [NE, D, 2 * FE] if not lite else [1, 1, 2])
    w_dn_m = din("w_dn_m", [NE, FE, D] if not lite else [1, 1, 2])
    w_pp = din("w_pp", [2, 256, D])
    w_pg = din("w_pg", [2, D, D])
    out = nc.dram_tensor("out", [S, D], F32, kind="ExternalOutput").ap()
    cq_d = nc.dram_tensor("cq_d", [NH, 3, S], BF16, kind="Internal").ap()
    ck_d = nc.dram_tensor("ck_d", [NH, 3, S], BF16, kind="Internal").ap()
    dbg = None
    if stop is not None:
        dbg = nc.dram_tensor("dbg", [128, NCH, S], F32, kind="ExternalOutput").ap()

    def kview(w2d):
        return w2d.rearrange("(kc p) n -> p kc n", p=128)

    with contextlib.ExitStack() as es:
        fw = FW(nc, es)
        op = fw.op

        uid = [0]

        def sb(st, name, shape, dt):
            uid[0] += 1
            return st.enter_context(nc.sbuf_tensor(f"{name}_{uid[0]}", list(shape), dt))

        T = {}
        ps = es.enter_context(nc.psum_tensor("ps", [128, 8, 512], F32))
        psr = [Region() for _ in range(8)]

        hT = sb(es, "hT", [128, NCH, S], F32)
        R_h = [[Region() for _ in range(NG)] for _ in range(NCH)]
        def alloc_aT(st):
            T["aT"] = sb(st, "aT", [128, NCH, S], BF16)

        R_a = [[Region() for _ in range(NG)] for _ in range(NCH)]
        all_a = [R_a[m][g] for m in range(NCH) for g in range(NG)]

        ident_f = sb(es, "ident_f", [128, 128], F32)
        negI_b = sb(es, "negI_b", [128, 128], BF16)
        negbigI_b = sb(es, "negbigI_b", [128, 128], BF16)
        negtri_b = sb(es, "negtri_b", [128, 128], BF16)
        ones_b = sb(es, "ones_b", [128, 128], BF16)
        gam = sb(es, "gam", [128, 48], F32)
        nbf = sb(es, "nbf", [16, 1], F32)
        rb = sb(es, "rb", [128, 8], F32)
        R_const = Region()
        ds_c = fw.dsem("ds_c")
        with contextlib.ExitStack() as ph:
            io = sb(ph, "io", [128, 512], I32)
            R_io = Region()
            op("pool", lambda e: e.iota(io[:, 0:128], [[1, 128]], base=0, channel_multiplier=-1), writes=[R_io])
            op("dve", lambda e: e.tensor_scalar(ident_f[:], io[:, 0:128], 0.0, None, ALU.is_equal), reads=[R_io], writes=[R_const])
            op("dve", lambda e: e.tensor_scalar(negI_b[:], io[:, 0:128], 0.0, -1.0, ALU.is_equal, ALU.mult), reads=[R_io], writes=[R_const])
            op("dve", lambda e: e.tensor_scalar(negbigI_b[:], io[:, 0:128], 0.0, NEGBIG, ALU.is_equal, ALU.mult), reads=[R_io], writes=[R_const])
            op("dve", lambda e: e.tensor_scalar(negtri_b[:], io[:, 0:128], 0.0, -1.0, ALU.is_le, ALU.mult), reads=[R_io], writes=[R_const])
            op("dve", lambda e: e.memset(ones_b[:], 1.0), writes=[R_const])
            fw.dma("sp", gam[:], gam_d[:, :], ds_c, writes=[R_const])
            fw.dma("sp", nbf[:], bf_d[:, :], ds_c, writes=[R_const])
            fw.dma("sp", rb[:], rb_d[:, :], ds_c, writes=[R_const])
            op("dve", lambda e: e.tensor_scalar(nbf[:], nbf[:], -1.0, None, ALU.mult), reads=[R_const], writes=[R_const])
            fw.barrier()

        def gc(g):
            return slice(g * 512, (g + 1) * 512)

        def make_mask(st, name, cmp_op):
            mk = sb(st, name, [128, 4, 512], BF16)
            with contextlib.ExitStack() as tmpst:
                io_ = sb(tmpst, name + "_io", [128, 512], I32)
                R_io_ = Region()
                for v in range(4):
                    op("pool", lambda e: e.iota(io_[:, :], [[1, 512]], base=-128 * v, channel_multiplier=-1), writes=[R_io_])
                    op("dve", lambda e: e.tensor_scalar(mk[:, v, :], io_[:, :], 0.0, None, cmp_op), reads=[R_io_], writes=[R_const])
                fw.barrier()
            return mk

        def mm(out_ap, lhsT, rhs, start, stop, reads, writes, track=None):
            if track is None:
                track = stop
            return op("pe", lambda e: e.matmul(out_ap, lhsT, rhs, start=start, stop=stop), reads=reads, writes=writes, track=track)

        class Stream:
            def __init__(self, st, name, shape, dt, nbuf):
                self.t = [sb(st, f"{name}{i}", shape, dt) for i in range(nbuf)]
                self.r = [Region() for _ in range(nbuf)]
                self.ds = [fw.dsem(f"ds_{name}{i}") for i in range(nbuf)]
                self.i = 0
                self.n = nbuf

            def next(self):
                i = self.i
                self.i = (i + 1) % self.n
                return self.t[i], self.r[i], self.ds[i]

        def wload(stream, dst_fn, src, q="pool", **kw):
            t, r, ds = stream.next()
            fw.dma(q, dst_fn(t), src, ds, writes=[r], **kw)
            return t, r

        def dump(name):
            if stop == name:
                ds = fw.dsem("ds_dbg")
                tok = fw.dma("sp", dbg[:, :, :], hT[:], ds, reads=[R_h[m][g] for m in range(NCH) for g in range(NG)])
                fw._wait("sp", tok)
                return True
            return False

        def rmsnorm(gi, ph, out_fn=None, post=None):
            sq = [sb(ph, f"sq{gi}_{i}", [128, NCH, 512], BF16) for i in range(2)]
            R_sq = [Region(), Region()]
            lnt = sb(ph, f"lnt{gi}", [128, 512], F32)
            R_ln = Region()
            rstd = [sb(ph, f"rstd{gi}_{i}", [128, 512], F32) for i in range(2)]
            R_rs = [Region(), Region()]
            for g in range(NG):
                b = g % 2
                for m in range(NCH):
                    op("act", lambda e: e.activation(out=sq[b][:, m, :], in_=hT[:, m, gc(g)], func=AF.Square),
                       reads=[R_h[m][g]], writes=[R_sq[b]])
                bank = 6 + b
                for m in range(NCH):
                    mm(ps[:, bank, :], ones_b[:], sq[b][:, m, :], m == 0, m == NCH - 1, [R_sq[b]], [psr[bank]])
                op("act", lambda e: e.activation(out=lnt[:], in_=ps[:, bank, :], func=AF.Ln, bias=EPS, scale=1.0 / D),
                   reads=[psr[bank]], writes=[R_ln])
                op("act", lambda e: e.activation(out=rstd[b][:], in_=lnt[:], func=AF.Exp, scale=-0.5),
                   reads=[R_ln], writes=[R_rs[b]])
                for m in range(NCH):
                    if out_fn is None:
                        op("dve", lambda e: e.scalar_tensor_tensor(out=T["aT"][:, m, gc(g)], in0=hT[:, m, gc(g)],
                                                                   scalar=gam[:, gi * 8 + m:gi * 8 + m + 1], in1=rstd[b][:],
                                                                   op0=ALU.mult, op1=ALU.mult),
                           reads=[R_h[m][g], R_rs[b]], writes=[R_a[m][g]])
                    else:
                        out_fn(g, m, rstd[b], R_rs[b])
                if post is not None:
                    post(g)

        with contextlib.ExitStack() as ph:
            xs = Stream(ph, "xs", [128, 4, D], F32, 2)
            k = 0
            for g in range(NG):
                xt, xr = wload(xs, lambda t: t[:], x[g * 512:(g + 1) * 512, :].rearrange("(t q) d -> q t d", q=128), q="sp")
                for m in range(NCH):
                    bank = k % 4
                    k += 1
                    for t in range(4):
                        op("pe", lambda e: e.transpose(ps[:, bank, t * 128:(t + 1) * 128], xt[:, t, m * 128:(m + 1) * 128], ident_f[:]),
                           reads=[xr, R_const], writes=[psr[bank]], track=(t == 3))
                    if m % 2 == 0:
                        op("act", lambda e: e.activation(out=hT[:, m, gc(g)], in_=ps[:, bank, :], func=AF.Copy),
                           reads=[psr[bank]], writes=[R_h[m][g]])
                    else:
                        op("dve", lambda e: e.tensor_copy(hT[:, m, gc(g)], ps[:, bank, :]), reads=[psr[bank]], writes=[R_h[m][g]])
            fw.barrier()
        if dump("x"):
            return nc

        def out_proj_pair(wo, wor, OTp, R_ot, banks):
            k = 0
            for m in range(NCH):
                for g in range(NG):
                    bank = banks[k % len(banks)]
                    k += 1
                    mm(ps[:, bank, :], wo[:, m * 128:(m + 1) * 128], OTp[:, gc(g)], True, True, [wor, R_ot[g]], [psr[bank]])
                    op("dve", lambda e: e.tensor_tensor(out=hT[:, m, gc(g)], in0=ps[:, bank, :], in1=hT[:, m, gc(g)], op=ALU.add),
                       reads=[psr[bank], R_h[m][g]], writes=[R_h[m][g]])

        def vslice(h):
            if h % 2 == 0:
                return slice(h + 1, 18, 16 - h)
            return slice(0, h + 2, h + 1)

        R_v = [Region() for _ in range(16)]

        def v_proj(ph, wsrc, col0, fill):
            Vt = T["Vt"]
            wv = Stream(ph, "wv", [128, NCH, 512], BF16, 2)
            k = 0
            for half in range(2):
                wt, wr = wload(wv, lambda t: t[:], kview(wsrc)[:, :, col0 + half * 512: col0 + (half + 1) * 512])
                for tt in range(16):
                    bank = k % 4
                    k += 1
                    g = tt // 4
                    for kc in range(NCH):
                        mm(ps[:, bank, :], T["aT"][:, kc, tt * 128:(tt + 1) * 128], wt[:, kc, :], kc == 0, kc == NCH - 1,
                           [R_a[kc][g], wr], [psr[bank]])
                    dst = Vt[:, tt, half * 512:(half + 1) * 512]
                    src = ps[:, bank, :]
                    if tt % 2 == 0:
                        op("act", lambda e: e.activation(out=dst, in_=src, func=AF.Copy), reads=[psr[bank]], writes=[R_v[tt]])
                    else:
                        op("dve", lambda e: e.tensor_copy(dst, src), reads=[psr[bank]], writes=[R_v[tt]])

        phA = contextlib.ExitStack()
        Vt = T["Vt"] = sb(phA, "VtA", [128, 16, D], BF16)
        maskF = make_mask(phA, "maskF", ALU.is_lt)
        alloc_aT(phA)
        with contextlib.ExitStack() as ph:
            rmsnorm(0, ph)
            fw.barrier()
        with contextlib.ExitStack() as ph:
            wf = sb(ph, "wf", [128, NCH, NH], BF16)
            R_wf = Region()
            ds_wf = fw.dsem("ds_wf")
            fw.dma("pool", wf[:], kview(w_in)[:, :, 3 * D:3 * D + NH], ds_wf, writes=[R_wf])
            t1 = sb(ph, "t1", [NH, S], F32)
            cc = sb(ph, "cc", [NH, S], F32)
            one16 = sb(ph, "one16", [NH, S], F32)
            SQ = sb(ph, "SQ", [NH, 3, S], BF16)
            SK = sb(ph, "SK", [NH, 3, S], BF16)
            R_t = Region()
            op("pool", lambda e: e.memset(one16[:], 1.0), writes=[R_t])
            for g in range(NG):
                for kc in range(NCH):
                    mm(ps[0:NH, g, :], wf[:, kc, :], T["aT"][:, kc, gc(g)], kc == 0, kc == NCH - 1, [R_wf, R_a[kc][g]], [psr[g]])
                op("act", lambda e: e.activation(out=t1[:, gc(g)], in_=ps[0:NH, g, :], func=AF.Exp, bias=nbf[:, 0:1], scale=-1.0),
                   reads=[psr[g], R_const], writes=[R_t])
            op("act", lambda e: e.activation(out=t1[:], in_=t1[:], func=AF.Ln, bias=1.0, scale=1.0), reads=[R_t], writes=[R_t])
            op("dve", lambda e: e.tensor_tensor_scan(cc[:], one16[:], t1[:], 0.0, ALU.mult, ALU.subtract), reads=[R_t], writes=[R_t])
            op("dve", lambda e: e.tensor_copy(SQ[:, 0, :], cc[:]), reads=[R_t], writes=[R_t])
            op("dve", lambda e: e.tensor_tensor(out=cc[:], in0=cc[:], in1=SQ[:, 0, :], op=ALU.subtract), reads=[R_t], writes=[R_t])
            op("dve", lambda e: e.tensor_copy(SQ[:, 1, :], cc[:]), reads=[R_t], writes=[R_t])
            op("dve", lambda e: e.tensor_tensor(out=cc[:], in0=cc[:], in1=SQ[:, 1, :], op=ALU.subtract), reads=[R_t], writes=[R_t])
            op("dve", lambda e: e.tensor_copy(SQ[:, 2, :], cc[:]), reads=[R_t], writes=[R_t])
            op("dve", lambda e: e.tensor_scalar(SK[:], SQ[:], -1.0, None, ALU.mult), reads=[R_t], writes=[R_t])
            ds_cq = fw.dsem("ds_cq")
            fw.dma("sp", cq_d[:, :, :], SQ[:], ds_cq, reads=[R_t])
            fw.dma("sp", ck_d[:, :, :], SK[:], ds_cq, reads=[R_t])
            fw.barrier()
        with contextlib.ExitStack() as ph:
            v_proj(ph, w_in, 2 * D, 1.0)
            fw.barrier()
        with contextlib.ExitStack() as ph:
            wqs = Stream(ph, "wq", [128, NCH, 128], BF16, 2)
            wks = Stream(ph, "wk", [128, NCH, 128], BF16, 2)
            wos = Stream(ph, "wo", [128, D], BF16, 2)
            qa = [[sb(ph, f"qa{b}{hh}", [128, S], BF16) for hh in range(2)] for b in range(2)]
            ka = [[sb(ph, f"ka{b}{hh}", [128, S], BF16) for hh in range(2)] for b in range(2)]
            R_qa = [[[Region() for _ in range(NG)] for hh in range(2)] for b in range(2)]
            R_ka = [[[Region() for _ in range(NG)] for hh in range(2)] for b in range(2)]
            R_qaug = [[Region() for hh in range(2)] for b in range(2)]
            R_kaug = [[Region() for hh in range(2)] for b in range(2)]
            ds_aug = [[[fw.dsem(f"ds_aug{b}{hh}{z}") for z in range(2)] for hh in range(2)] for b in range(2)]
            PT = [sb(ph, f"PT{i}", [128, 512], BF16) for i in range(3)]
            R_pt = [Region() for _ in range(3)]
            OTp = [sb(ph, f"OTp{i}", [128, S], BF16) for i in range(2)]
            R_ot = [[Region() for _ in range(NG)] for _ in range(2)]
            rc = [sb(ph, f"rc{i}", [128, 512], F32) for i in range(2)]
            R_rc = [Region(), Region()]
            for b in range(2):
                for hh in range(2):
                    op("pool", lambda e: e.memset(qa[b][hh][64:128, :], 0.0), writes=[R_qaug[b][hh]])
                    op("pool", lambda e: e.memset(qa[b][hh][96:99, :], 1.0), writes=[R_qaug[b][hh]])
                    op("pool", lambda e: e.memset(ka[b][hh][64:128, :], 0.0), writes=[R_kaug[b][hh]])
                    op("pool", lambda e: e.memset(ka[b][hh][64:67, :], 1.0), writes=[R_kaug[b][hh]])

            def proj_pair(pr):
                b = pr % 2
                wq, wqr = wload(wqs, lambda t: t[:], kview(w_in)[:, :, pr * 128:(pr + 1) * 128])
                wk, wkr = wload(wks, lambda t: t[:], kview(w_in)[:, :, D + pr * 128: D + (pr + 1) * 128])
                for hh in range(2):
                    h = 2 * pr + hh
                    fw.dma("sp", qa[b][hh][64:67, :], cq_d[h, :, :], ds_aug[b][hh][0], writes=[R_qaug[b][hh]])
                    fw.dma("sp", ka[b][hh][96:99, :], ck_d[h, :, :], ds_aug[b][hh][1], writes=[R_kaug[b][hh]])
                k = 0
                for (wt, wr, dst, R_d, scl) in ((wq, wqr, qa[b], R_qa[b], 0.125), (wk, wkr, ka[b], R_ka[b], 1.0)):
                    for g in range(NG):
                        bank = 7
                        k += 1
                        for kc in range(NCH):
                            mm(ps[:, bank, :], wt[:, kc, :], T["aT"][:, kc, gc(g)], kc == 0, kc == NCH - 1, [wr, R_a[kc][g]], [psr[bank]])
                        for hh in range(2):
                            op("dve", lambda e: e.tensor_scalar(dst[hh][0:64, gc(g)], ps[hh * 64:(hh + 1) * 64, bank, :], scl, None, ALU.mult),
                               reads=[psr[bank]], writes=[R_d[hh][g]])

            sk = 0
            ak = 0
            proj_pair(0)
            for pr in range(NH // 2):
                b = pr % 2
                wo, wor = wload(wos, lambda t: t[:], w_o_a[pr * 128:(pr + 1) * 128, :])
                for hh in range(2):
                    h = 2 * pr + hh
                    for g in range(NG):
                        accb = 3 + (ak % 2)
                        denb = 5 + (ak % 2)
                        ak += 1
                        nj = 4 * g + 4
                        hp = slice(hh * 64, (hh + 1) * 64)

                        def s_mm(j, sbk):
                            diag = j >= 4 * g
                            mm(ps[:, sbk, :], ka[b][hh][0:99, j * 128:(j + 1) * 128], qa[b][hh][0:99, gc(g)], True, not diag,
                               [R_ka[b][hh][j // 4], R_kaug[b][hh], R_qa[b][hh][g], R_qaug[b][hh]], [psr[sbk]])
                            if diag:
                                mm(ps[:, sbk, :], negbigI_b[:], maskF[:, j - 4 * g, :], False, True, [R_const], [psr[sbk]])

                        sk0 = sk
                        sk += nj
                        s_mm(0, sk0 % 3)
                        if nj > 1:
                            s_mm(1, (sk0 + 1) % 3)

                        def pv_den(jj):
                            cb = (sk0 + jj) % 3
                            mm(ps[:, accb, :], Vt[:, jj, pr * 128:(pr + 1) * 128], PT[cb][:], jj == 0, jj == nj - 1,
                               [R_v[jj], R_pt[cb]], [psr[accb]], track=(jj == nj - 1))
                            mm(ps[:, denb, :], ones_b[:], PT[cb][:], jj == 0, jj == nj - 1,
                               [R_const, R_pt[cb]], [psr[denb]], track=True)

                        for j in range(nj):
                            cur = (sk0 + j) % 3
                            if j + 2 < nj:
                                s_mm(j + 2, (sk0 + j + 2) % 3)
                            op("act", lambda e: e.activation(out=PT[cur][:], in_=ps[:, cur, :], func=AF.Exp),
                               reads=[psr[cur]], writes=[R_pt[cur]])
                            if j >= 1:
                                pv_den(j - 1)
                        pv_den(nj - 1)
                        r2 = ak % 2
                        op("act", lambda e: e.activation(out=rc[r2][hp, :], in_=ps[hp, denb, :], func=AF.Ln), reads=[psr[denb]], writes=[R_rc[r2]])
                        op("act", lambda e: e.activation(out=rc[r2][hp, :], in_=rc[r2][hp, :], func=AF.Exp, scale=-1.0), reads=[R_rc[r2]], writes=[R_rc[r2]])
                        op("dve", lambda e: e.tensor_tensor(out=OTp[b][hp, gc(g)], in0=ps[hp, accb, :], in1=rc[r2][hp, :], op=ALU.mult),
                           reads=[psr[accb], R_rc[r2]], writes=[R_ot[b][g]])
                    if hh == 0 and pr + 1 < NH // 2:
                        proj_pair(pr + 1)
                out_proj_pair(wo, wor, OTp[b], R_ot[b], [7, 3, 4, 5, 6])
            fw.barrier()
        phA.close()
        if dump("attn0"):
            return nc

        def swiglu(ph, tag, w_gu2d, w_dn2d, ff, halves, gate_fn=None):
            nmb = ff // 256
            nkc = ff // 128
            wgs = Stream(ph, f"wg{tag}", [128, NCH, 256], BF16, 2)
            wus = Stream(ph, f"wu{tag}", [128, NCH, 256], BF16, 2)
            wds = Stream(ph, f"wd{tag}", [128, nkc, 128], BF16, 2)
            actT = sb(ph, f"actT{tag}", [128, nkc, 1024], BF16)
            R_act = [[Region() for _ in range(2)] for _ in range(nkc)]
            sgt = [sb(ph, f"sg{tag}{i}", [128, 512], BF16) for i in range(2)]
            R_sg = [Region(), Region()]
            tmp = [sb(ph, f"tmp{tag}{i}", [128, 512], F32) for i in range(2)]
            R_tmp = [Region(), Region()]
            return dict(nmb=nmb, nkc=nkc, wgs=wgs, wus=wus, wds=wds, actT=actT, R_act=R_act, sgt=sgt, R_sg=R_sg, tmp=tmp, R_tmp=R_tmp)

        def swiglu_run(st, w_gu2d, w_dn2d, ff, half, gate=None):
            nmb, nkc = st["nmb"], st["nkc"]
            actT, R_act = st["actT"], st["R_act"]
            k = 0
            sgk = 0
            wg_v = kview(w_gu2d)
            wd_v = kview(w_dn2d)
            nxt = (wload(st["wgs"], lambda t: t[:], wg_v[:, :, 0:256]), wload(st["wus"], lambda t: t[:], wg_v[:, :, ff:ff + 256]))
            for mb in range(nmb):
                (wg, wgr), (wu, wur) = nxt
                if mb + 1 < nmb:
                    c0 = (mb + 1) * 256
                    nxt = (wload(st["wgs"], lambda t: t[:], wg_v[:, :, c0:c0 + 256]),
                           wload(st["wus"], lambda t: t[:], wg_v[:, :, ff + c0:ff + c0 + 256]))
                for mi in range(2):
                    m = mb * 2 + mi
                    for gg in range(2):
                        g = half * 2 + gg
                        gb = (k % 2) * 2
                        ub = gb + 1
                        k += 1
                        for kc in range(NCH):
                            mm(ps[:, gb, :], wg[:, kc, mi * 128:(mi + 1) * 128], T["aT"][:, kc, gc(g)], kc == 0, kc == NCH - 1,
                               [wgr, R_a[kc][g]], [psr[gb]])
                        for kc in range(NCH):
                            mm(ps[:, ub, :], wu[:, kc, mi * 128:(mi + 1) * 128], T["aT"][:, kc, gc(g)], kc == 0, kc == NCH - 1,
                               [wur, R_a[kc][g]], [psr[ub]])
                        s = sgk % 2
                        sgk += 1
                        op("act", lambda e: e.activation(out=st["sgt"][s][:], in_=ps[:, gb, :], func=AF.Silu),
                           reads=[psr[gb]], writes=[st["R_sg"][s]])
                        op("dve", lambda e: e.tensor_tensor(out=actT[:, m, gg * 512:(gg + 1) * 512], in0=ps[:, ub, :], in1=st["sgt"][s][:], op=ALU.mult),
                           reads=[psr[ub], st["R_sg"][s]], writes=[R_act[m][gg]])
            nxt = wload(st["wds"], lambda t: t[:], wd_v[:, :, 0:128])
            k = 0
            for o in range(NCH):
                wd, wdr = nxt
                if o + 1 < NCH:
                    nxt = wload(st["wds"], lambda t: t[:], wd_v[:, :, (o + 1) * 128:(o + 2) * 128])
                for gg in range(2):
                    g = half * 2 + gg
                    bank = 4 + (k % 4)
                    k += 1
                    for kc in range(nkc):
                        mm(ps[:, bank, :], wd[:, kc, :], actT[:, kc, gg * 512:(gg + 1) * 512], kc == 0, kc == nkc - 1,
                           [wdr, R_act[kc][gg]], [psr[bank]])
                    if gate is None:
                        op("dve", lambda e: e.tensor_tensor(out=hT[:, o, gc(g)], in0=ps[:, bank, :], in1=hT[:, o, gc(g)], op=ALU.add),
                           reads=[psr[bank], R_h[o][g]], writes=[R_h[o][g]])
                    else:
                        gt, gr = gate
                        s = k % 2
                        op("dve", lambda e: e.tensor_tensor(out=st["tmp"][s][:], in0=ps[:, bank, :], in1=gt[:, gg * 512:(gg + 1) * 512], op=ALU.mult),
                           reads=[psr[bank], gr], writes=[st["R_tmp"][s]])
                        op("pool", lambda e: e.tensor_tensor(out=hT[:, o, gc(g)], in0=st["tmp"][s][:], in1=hT[:, o, gc(g)], op=ALU.add),
                           reads=[st["R_tmp"][s], R_h[o][g]], writes=[R_h[o][g]])

        phF = contextlib.ExitStack()
        alloc_aT(phF)
        with contextlib.ExitStack() as ph:
            rmsnorm(1, ph)
            fw.barrier()
        with contextlib.ExitStack() as ph:
            st = swiglu(ph, "d", w_gu_d, w_dn_d, FF, 2)
            for half in range(2):
                swiglu_run(st, w_gu_d, w_dn_d, FF, half)
            fw.barrier()
        phF.close()
        if dump("ffn0"):
            return nc

        def ple(i):
            with contextlib.ExitStack() as ph:
                alloc_aT(ph)
                for m in range(NCH):
                    for g in range(NG):
                        if (m + g) % 2 == 0:
                            op("act", lambda e: e.activation(out=T["aT"][:, m, gc(g)], in_=hT[:, m, gc(g)], func=AF.Copy),
                               reads=[R_h[m][g]], writes=[R_a[m][g]])
                        else:
                            op("pool", lambda e: e.tensor_copy(T["aT"][:, m, gc(g)], hT[:, m, gc(g)]), reads=[R_h[m][g]], writes=[R_a[m][g]])
                pt = sb(ph, f"pt{i}", [128, 16, 256], F32)
                R_p = Region()
                ds_p = fw.dsem(f"ds_p{i}")
                fw.dma("sp", pt[:], p[i, :, :].rearrange("(t q) f -> q t f", q=128), ds_p, writes=[R_p])
                pT = sb(ph, f"pT{i}", [128, 2, S], BF16)
                R_pT = [Region() for _ in range(NG)]
                wpp = sb(ph, f"wpp{i}", [128, 2, D], BF16)
                R_wpp = Region()
                ds_wpp = fw.dsem(f"ds_wpp{i}")
                fw.dma("pool", wpp[:], w_pp[i, :, :].rearrange("(kc q) n -> q kc n", q=128), ds_wpp, writes=[R_wpp])
                k = 0
                for g in range(NG):
                    for pc in range(2):
                        bank = k % 2
                        k += 1
                        for t in range(4):
                            op("pe", lambda e: e.transpose(ps[:, bank, t * 128:(t + 1) * 128], pt[:, 4 * g + t, pc * 128:(pc + 1) * 128], ident_f[:]),
                               reads=[R_p, R_const], writes=[psr[bank]], track=(t == 3))
                        op("dve", lambda e: e.tensor_copy(pT[:, pc, gc(g)], ps[:, bank, :]), reads=[psr[bank]], writes=[R_pT[g]])
                wpgs = Stream(ph, f"wpg{i}", [128, NCH, 128], BF16, 2)
                sgt = [sb(ph, f"psg{i}{j}", [128, 512], F32) for j in range(2)]
                R_sg = [Region(), Region()]
                tmp = [sb(ph, f"ptmp{i}{j}", [128, 512], F32) for j in range(2)]
                R_tmp = [Region(), Region()]
                wpg_v = kview(w_pg[i, :, :])
                nxt = wload(wpgs, lambda t: t[:], wpg_v[:, :, 0:128])
                k = 0
                for o in range(NCH):
                    wg, wgr = nxt
                    if o + 1 < NCH:
                        nxt = wload(wpgs, lambda t: t[:], wpg_v[:, :, (o + 1) * 128:(o + 2) * 128])
                    for g in range(NG):
                        gb = 2 + (k % 2) * 2
                        pb = gb + 1
                        s = k % 2
                        k += 1
                        for kc in range(NCH):
                            mm(ps[:, gb, :], wg[:, kc, :], T["aT"][:, kc, gc(g)], kc == 0, kc == NCH - 1, [wgr, R_a[kc][g]], [psr[gb]])
                        for kc in range(2):
                            mm(ps[:, pb, :], wpp[:, kc, o * 128:(o + 1) * 128], pT[:, kc, gc(g)], kc == 0, kc == 1, [R_wpp, R_pT[g]], [psr[pb]])
                        op("act", lambda e: e.activation(out=sgt[s][:], in_=ps[:, gb, :], func=AF.Sigmoid), reads=[psr[gb]], writes=[R_sg[s]])
                        op("dve", lambda e: e.tensor_tensor(out=tmp[s][:], in0=ps[:, pb, :], in1=sgt[s][:], op=ALU.mult),
                           reads=[psr[pb], R_sg[s]], writes=[R_tmp[s]])
                        op("pool", lambda e: e.tensor_tensor(out=hT[:, o, gc(g)], in0=tmp[s][:], in1=hT[:, o, gc(g)], op=ALU.add),
                           reads=[R_tmp[s], R_h[o][g]], writes=[R_h[o][g]])
                fw.barrier()

        ple(0)
        if dump("ple0"):
            return nc

        phB = contextlib.ExitStack()
        KT = sb(phB, "KT", [128, NCH, S], BF16)
        Vt = T["Vt"] = sb(phB, "VtB", [128, 16, D], BF16)
        maskS = make_mask(phB, "maskS", ALU.is_le)
        alloc_aT(phB)
        R_k = [[Region() for _ in range(NG)] for _ in range(NCH)]
        with contextlib.ExitStack() as ph:
            rmsnorm(2, ph)
            fw.barrier()
        with contextlib.ExitStack() as ph:
            wks = Stream(ph, "wkk", [128, NCH, 128], BF16, 2)
            wkv_v = kview(w_kv)
            k = 0
            for pr in range(NCH):
                wk, wkr = wload(wks, lambda t: t[:], wkv_v[:, :, pr * 128:(pr + 1) * 128])
                for g in range(NG):
                    bank = 4 + k % 4
                    k += 1
                    for kc in range(NCH):
                        mm(ps[:, bank, :], wk[:, kc, :], T["aT"][:, kc, gc(g)], kc == 0, kc == NCH - 1, [wkr, R_a[kc][g]], [psr[bank]])
                    if k % 2 == 0:
                        op("act", lambda e: e.activation(out=KT[:, pr, gc(g)], in_=ps[:, bank, :], func=AF.Copy), reads=[psr[bank]], writes=[R_k[pr][g]])
                    else:
                        op("dve", lambda e: e.tensor_copy(KT[:, pr, gc(g)], ps[:, bank, :]), reads=[psr[bank]], writes=[R_k[pr][g]])
            v_proj(ph, w_kv, D, 0.0)
            fw.barrier()

        with contextlib.ExitStack() as ph:
            rmsnorm(3, ph)
            fw.barrier()
        with contextlib.ExitStack() as ph:
            wqs = Stream(ph, "wq1", [128, NCH, 128], BF16, 2)
            wos = Stream(ph, "wo1", [128, D], BF16, 2)
            qz = [[sb(ph, f"qz{b}{hh}", [128, S], BF16) for hh in range(2)] for b in range(2)]
            R_q = [[[Region() for _ in range(NG)] for hh in range(2)] for _ in range(2)]
            R_qz = Region()
            for b_ in range(2):
                for hh_ in range(2):
                    op("pool", lambda e: e.memset(qz[b_][hh_][:], 0.0), writes=[R_qz])
            et = [sb(ph, f"et{i}", [128, 512], F32) for i in range(2)]
            R_e = [Region(), Region()]
            SPt = [sb(ph, f"SPt{i}", [128, 512], BF16) for i in range(3)]
            R_sp = [Region() for _ in range(3)]
            Wt = [sb(ph, f"Wt{i}", [128, 512], BF16) for i in range(2)]
            R_w = [Region(), Region()]
            Rsb = [sb(ph, f"Rsb{i}", [128, 512], BF16) for i in range(2)]
            R_r = [Region(), Region()]
            _ot1 = sb(ph, "OT1p", [128, S], BF16)
            OTp = [_ot1, _ot1]
            _rot1 = [Region() for _ in range(NG)]
            R_ot = [_rot1, _rot1]
            wq_v = kview(w_q_b)

            def qproj(pr):
                b = pr % 2
                wq, wqr = wload(wqs, lambda t: t[:], wq_v[:, :, pr * 128:(pr + 1) * 128])
                for g in range(NG):
                    bank = 7
                    for kc in range(NCH):
                        mm(ps[:, bank, :], wq[:, kc, :], T["aT"][:, kc, gc(g)], kc == 0, kc == NCH - 1, [wqr, R_a[kc][g]], [psr[bank]])
                    for hh_ in range(2):
                        hp_ = slice(hh_ * 64, (hh_ + 1) * 64)
                        op("dve", lambda e: e.tensor_scalar(qz[b][hh_][hp_, gc(g)], ps[hp_, bank, :], 0.125, None, ALU.mult),
                           reads=[psr[bank], R_qz], writes=[R_q[b][hh_][g]])

            cn = dict(a=0, b=0, e=0, sp=0, w=0, r=0)
            qproj(0)
            for pr in range(NH // 2):
                b = pr % 2
                wo, wor = wload(wos, lambda t: t[:], w_o_b[pr * 128:(pr + 1) * 128, :])
                for g in range(NG):
                    accb = 4 + (g % 2)
                    for hh in range(2):
                        h = 2 * pr + hh
                        hp = slice(hh * 64, (hh + 1) * 64)
                        nj = 4 * g + 4

                        def z_mm(bank, j, stop, track):
                            diag = j >= 4 * g
                            jc = slice(j * 128, (j + 1) * 128)
                            mm(ps[:, bank, :], KT[:, pr, jc], qz[b][hh][:, gc(g)], True, stop and not diag,
                               [R_k[pr][j // 4], R_q[b][hh][g]], [psr[bank]], track=(track and not diag))
                            if diag:
                                mm(ps[:, bank, :], negbigI_b[:], maskS[:, j - 4 * g, :], False, stop, [R_const], [psr[bank]], track=track)

                        def stageA(j):
                            A = cn["a"] % 2
                            cn["a"] += 1
                            z_mm(A, j, True, True)
                            e_i = cn["e"] % 2
                            cn["e"] += 1
                            op("act", lambda e: e.activation(out=et[e_i][:], in_=ps[:, A, :], func=AF.Exp), reads=[psr[A]], writes=[R_e[e_i]])
                            s_i = cn["sp"] % 3
                            cn["sp"] += 1
                            op("act", lambda e: e.activation(out=SPt[s_i][:], in_=et[e_i][:], func=AF.Ln, bias=1.0, scale=1.0),
                               reads=[R_e[e_i]], writes=[R_sp[s_i]])
                            return s_i

                        js = list(range(nj - 1, -1, -1))
                        nt = len(js)
                        sp_of = {0: stageA(js[0])}
                        if nt > 1:
                            sp_of[1] = stageA(js[1])
                        pend = None

                        def emit_pv(pv):
                            pj, pw, pfirst = pv
                            mm(ps[:, 4 + hh, :], Vt[:, pj, pr * 128:(pr + 1) * 128], Wt[pw][:], pfirst, pj == 0,
                               [R_v[pj], R_w[pw]], [psr[4 + hh]], track=True)

                        for idx, j in enumerate(js):
                            first = idx == 0
                            if idx + 2 < nt:
                                sp_of[idx + 2] = stageA(js[idx + 2])
                            s_i = sp_of[idx]
                            r_prev = (cn["r"] - 1) % 2
                            if j > 0:
                                mm(ps[:, 6, :], ones_b[:], SPt[s_i][:], True, True, [R_const, R_sp[s_i]], [psr[6]], track=True)
                                r_i = cn["r"] % 2
                                cn["r"] += 1
                                if first:
                                    op("dve", lambda e: e.tensor_copy(Rsb[r_i][:], ps[:, 6, :]), reads=[psr[6]], writes=[R_r[r_i]])
                                else:
                                    op("dve", lambda e: e.tensor_tensor(out=Rsb[r_i][:], in0=ps[:, 6, :], in1=Rsb[r_prev][:], op=ALU.add),
                                       reads=[psr[6], R_r[r_prev]], writes=[R_r[r_i]])
                            B = 2 + (cn["b"] % 2)
                            cn["b"] += 1
                            z_mm(B, j, False, False)
                            mm(ps[:, B, :], negtri_b[:], SPt[s_i][:], False, first, [R_const, R_sp[s_i]], [psr[B]], track=first)
                            if not first:
                                mm(ps[:, B, :], negI_b[:], Rsb[r_prev][:], False, True, [R_const, R_r[r_prev]], [psr[B]])
                            w_i = cn["w"] % 2
                            cn["w"] += 1
                            op("act", lambda e: e.activation(out=Wt[w_i][:], in_=ps[:, B, :], func=AF.Exp), reads=[psr[B]], writes=[R_w[w_i]])
                            if pend is not None:
                                emit_pv(pend)
                            pend = (j, w_i, first)
                        emit_pv(pend)
                        op("dve", lambda e: e.tensor_copy(OTp[b][hp, gc(g)], ps[hp, 4 + hh, :]), reads=[psr[4 + hh]], writes=[R_ot[b][g]])
                    if g == 1 and pr + 1 < NH // 2:
                        qproj(pr + 1)
                out_proj_pair(wo, wor, OTp[b], R_ot[b], [7, 0, 1, 2, 3])
            fw.barrier()
        phB.close()
        if dump("attn1"):
            return nc

        CAP = 384
        NSC = CAP // 128
        with contextlib.ExitStack() as ph:
            G = sb(ph, "G", [128, 16, NE], F32)
            R_G = Region()
            GT = sb(ph, "GT", [NE, S], BF16)
            R_GT = Region()
            sel = sb(ph, "sel", [NE, NE, 128], BF16)
            R_sel = Region()
            f_tok = sb(ph, "f_tok", [128, 16, D], BF16)
            R_ft = [Region() for _ in range(16)]
            posT = sb(ph, "posT", [NE, 2, S], BF16)
            R_posT = Region()
            pos = sb(ph, "pos", [128, 16, NE], F32)
            Mf = sb(ph, "Mf", [128, 16, NE], F32)
            R_pos = Region()
            iotaC = sb(ph, "iotaC", [128, CAP], F32)
            slotid = sb(ph, "slotid", [128, NSC], F32)
            R_io = Region()
            with contextlib.ExitStack() as ph2:
                alloc_aT(ph2)
                aT = T["aT"]
                wr_f = sb(ph2, "wr_f", [128, NCH, NE], F32)
                wr_hi = sb(ph2, "wr_hi", [128, NCH, NE], BF16)
                wr_lo = sb(ph2, "wr_lo", [128, NCH, NE], BF16)
                R_wr = Region()
                ds_wr = fw.dsem("ds_wr")
                fw.dma("sp", wr_f[:], kview(r_w), ds_wr, writes=[R_wr])
                op("dve", lambda e: e.tensor_copy(wr_hi[:], wr_f[:]), reads=[R_wr], writes=[R_wr])
                op("dve", lambda e: e.tensor_tensor(out=wr_f[:], in0=wr_f[:], in1=wr_hi[:], op=ALU.subtract), reads=[R_wr], writes=[R_wr])
                op("dve", lambda e: e.tensor_copy(wr_lo[:], wr_f[:]), reads=[R_wr], writes=[R_wr])
                fT32 = sb(ph2, "fT32", [128, 512], F32)
                R_f32 = Region()
                loT = [sb(ph2, f"loT{i}", [128, NCH, 512], BF16) for i in range(2)]
                R_lo = [Region(), Region()]
                lg = sb(ph2, "lg", [128, 16, NE], F32)
                R_lg = Region()
                mx = sb(ph2, "mx", [128, 8], F32)
                nv1 = sb(ph2, "nv1", [128, 1], F32)
                msk = sb(ph2, "msk", [128, NE], F32)
                eg = sb(ph2, "eg", [128, NE], F32)
                den = sb(ph2, "den", [128, 1], F32)
                R_s = Region()
                io2 = sb(ph2, "io2", [NE, NE, 128], I32)
                op("pool", lambda e: e.iota(io2[:], [[-1, NE], [0, 128]], base=0, channel_multiplier=1), writes=[R_sel])
                op("dve", lambda e: e.tensor_scalar(sel[:], io2[:], 0.0, None, ALU.is_equal), reads=[R_sel], writes=[R_sel])
                io3 = sb(ph2, "io3", [128, CAP], I32)
                op("pool", lambda e: e.iota(io3[:], [[1, CAP]], base=0, channel_multiplier=0), writes=[R_io])
                op("dve", lambda e: e.tensor_copy(iotaC[:], io3[:]), reads=[R_io], writes=[R_io])
                op("pool", lambda e: e.iota(io3[:, 0:NSC], [[128, NSC]], base=0, channel_multiplier=1), reads=[R_io], writes=[R_io])
                op("dve", lambda e: e.tensor_copy(slotid[:], io3[:, 0:NSC]), reads=[R_io], writes=[R_io])

                def norm_out(g, m, rstd_t, R_rs):
                    b = g % 2
                    op("dve", lambda e: e.scalar_tensor_tensor(out=fT32[:], in0=hT[:, m, gc(g)], scalar=gam[:, 4 * 8 + m:4 * 8 + m + 1],
                                                               in1=rstd_t[:], op0=ALU.mult, op1=ALU.mult),
                       reads=[R_h[m][g], R_rs], writes=[R_f32])
                    op("dve", lambda e: e.tensor_copy(aT[:, m, gc(g)], fT32[:]), reads=[R_f32], writes=[R_a[m][g]])
                    op("dve", lambda e: e.tensor_tensor(out=loT[b][:, m, :], in0=fT32[:], in1=aT[:, m, gc(g)], op=ALU.subtract),
                       reads=[R_f32, R_a[m][g]], writes=[R_lo[b]])

                def route(g):
                    b = g % 2
                    for t in range(4):
                        tt = 4 * g + t
                        tc_ = slice(t * 128, (t + 1) * 128)
                        bank = 4 + (tt % 2)
                        n = 0
                        for kc in range(NCH):
                            for (l, r_, rr) in ((aT[:, kc, tt * 128:(tt + 1) * 128], wr_hi, R_a[kc][g]),
                                                (loT[b][:, kc, tc_], wr_hi, R_lo[b]),
                                                (aT[:, kc, tt * 128:(tt + 1) * 128], wr_lo, R_a[kc][g])):
                                mm(ps[:, bank, 0:NE], l, r_[:, kc, :], n == 0, n == 3 * NCH - 1, [rr, R_wr], [psr[bank]])
                                n += 1
                        op("dve", lambda e: e.tensor_tensor(out=lg[:, tt, :], in0=ps[:, bank, 0:NE], in1=rb[:], op=ALU.add),
                           reads=[psr[bank], R_const], writes=[R_lg])
                        op("dve", lambda e: e.max(mx[:], lg[:, tt, :]), reads=[R_lg], writes=[R_s])
                        op("dve", lambda e: e.tensor_scalar(nv1[:], mx[:, 0:1], -1.0, None, ALU.mult), reads=[R_s], writes=[R_s])
                        op("dve", lambda e: e.tensor_scalar(msk[:], lg[:, tt, :], mx[:, 1:2], None, ALU.is_ge), reads=[R_lg, R_s], writes=[R_s])
                        op("dve", lambda e: e.tensor_copy(Mf[:, tt, :], msk[:]), reads=[R_s], writes=[R_pos])
                        op("act", lambda e: e.activation(out=eg[:], in_=lg[:, tt, :], func=AF.Exp, bias=nv1[:, 0:1], scale=1.0),
                           reads=[R_lg, R_s], writes=[R_s])
                        op("dve", lambda e: e.tensor_tensor(out=eg[:], in0=eg[:], in1=msk[:], op=ALU.mult), reads=[R_s], writes=[R_s])
                        op("dve", lambda e: e.tensor_reduce(out=den[:], in_=eg[:], axis=mybir.AxisListType.X, op=ALU.add), reads=[R_s], writes=[R_s])
                        op("dve", lambda e: e.reciprocal(out=den[:], in_=den[:]), reads=[R_s], writes=[R_s])
                        op("dve", lambda e: e.tensor_scalar(G[:, tt, :], eg[:], den[:, 0:1], None, ALU.mult), reads=[R_s], writes=[R_G])
                        for mq in range(2):
                            fb = 2 * (tt % 2) + mq
                            for mi in range(4):
                                m = mq * 4 + mi
                                mm(ps[:, fb, mi * 128:(mi + 1) * 128], aT[:, m, tt * 128:(tt + 1) * 128], negI_b[:], True, True,
                                   [R_a[m][g], R_const], [psr[fb]], track=(mi == 3))
                            op("dve", lambda e: e.tensor_scalar(f_tok[:, tt, mq * 512:(mq + 1) * 512], ps[:, fb, :], -1.0, None, ALU.mult),
                               reads=[psr[fb]], writes=[R_ft[tt]])

                rmsnorm(4, ph2, out_fn=norm_out, post=route)
                for g in range(NG):
                    bank = g % 2
                    for t in range(4):
                        op("pe", lambda e: e.transpose(ps[0:NE, bank, t * 128:(t + 1) * 128], G[:, 4 * g + t, :], ident_f[:]),
                           reads=[R_G, R_const], writes=[psr[bank]], track=(t == 3))
                    op("dve", lambda e: e.tensor_copy(GT[:, gc(g)], ps[0:NE, bank, :]), reads=[psr[bank]], writes=[R_GT])
                Mk = sb(ph2, "Mk", [128, 16 * NE], BF16)
                tot = sb(ph2, "tot", [128, 16, NE], F32)
                offs = sb(ph2, "offs", [128, 16, NE], F32)
                stri_b = sb(ph2, "stri_b", [128, 128], BF16)
                io4 = sb(ph2, "io4", [128, 128], I32)
                op("pool", lambda e: e.iota(io4[:], [[1, 128]], base=0, channel_multiplier=-1), writes=[R_io])
                op("dve", lambda e: e.tensor_scalar(stri_b[:], io4[:], 0.0, None, ALU.is_gt), reads=[R_io], writes=[R_io])
                op("dve", lambda e: e.tensor_copy(Mk[:], Mf[:].rearrange("p a b -> p (a b)")), reads=[R_pos], writes=[R_pos])
                mm(ps[:, 4, 0:128], stri_b[:], Mk[:], True, True, [R_io, R_pos], [psr[4]])
                mm(ps[:, 5, 0:128], ones_b[:], Mk[:], True, True, [R_const, R_pos], [psr[5]])
                op("dve", lambda e: e.tensor_copy(tot[:].rearrange("p a b -> p (a b)"), ps[:, 5, 0:128]), reads=[psr[5]], writes=[R_pos])
                op("dve", lambda e: e.memset(offs[:], 0.0), writes=[R_pos])
                for k_ in list(range(1, 8)) + list(range(9, 16)):
                    op("dve", lambda e: e.tensor_tensor(out=offs[:, k_, :], in0=offs[:, k_ - 1, :], in1=tot[:, k_ - 1, :], op=ALU.add),
                       reads=[R_pos], writes=[R_pos])
                op("dve", lambda e: e.tensor_tensor(out=pos[:].rearrange("p a b -> p (a b)"), in0=ps[:, 4, 0:128],
                                                    in1=offs[:].rearrange("p a b -> p (a b)"), op=ALU.add),
                   reads=[psr[4], R_pos], writes=[R_pos])
                for g in range(NG):
                    bank = 6 + g % 2
                    for t in range(4):
                        op("pe", lambda e: e.transpose(ps[0:NE, bank, t * 128:(t + 1) * 128], pos[:, 4 * g + t, :], ident_f[:]),
                           reads=[R_pos, R_const], writes=[psr[bank]], track=(t == 3))
                    op("dve", lambda e: e.tensor_copy(posT[:, 0, gc(g)], ps[0:NE, bank, :]), reads=[psr[bank]], writes=[R_posT])
                    op("dve", lambda e: e.tensor_tensor(out=posT[:, 1, gc(g)], in0=ps[0:NE, bank, :], in1=posT[:, 0, gc(g)], op=ALU.subtract),
                       reads=[psr[bank], R_posT], writes=[R_posT])
                fw.barrier()
            with contextlib.ExitStack() as ph2:
                NKF = FE // 128
                wgs = Stream(ph2, "wgm", [128, NCH, 256], BF16, 3)
                wus = Stream(ph2, "wum", [128, NCH, 256], BF16, 3)
                wds = Stream(ph2, "wdm", [128, 4, 512], BF16, 3)
                Se = sb(ph2, "Se", [128, 8, CAP], BF16)
                R_se = [Region() for _ in range(8)]
                Xg = sb(ph2, "Xg", [128, NCH, CAP], BF16)
                R_xg = [Region() for _ in range(NCH)]
                actT = sb(ph2, "actTm", [128, NKF, CAP], BF16)
                R_act = [Region() for _ in range(NKF)]
                Ysm = [sb(ph2, f"Ysm{i}", [128, NSC, 512], BF16) for i in range(2)]
                R_y = [Region(), Region()]
                GS = sb(ph2, "GS", [128, NSC, 1024], BF16)
                R_gs = [Region(), Region()]
                Gb = sb(ph2, "Gb", [128, 1024], BF16)
                R_gb = [Region(), Region()]
                sgt = [sb(ph2, f"sgm{i}", [128, CAP], BF16) for i in range(2)]
                R_sg = [Region(), Region()]
                kk = dict(b=0, s=0, d=0, y=0, e=0)
                eqt = [sb(ph2, f"eqt{i}", [128, 512], BF16) for i in range(2)]
                R_eq = [Region(), Region()]
                items = [(half, ex) for half in range(2) for ex in range(NE)]
                pre = {}

                def prefetch_gu(it):
                    half, ex = it
                    wg_v = kview(w_gu_m[ex, :, :])
                    pre[it] = (wload(wgs, lambda t: t[:], wg_v[:, :, 0:256]), wload(wus, lambda t: t[:], wg_v[:, :, FE:FE + 256]))

                def build_Se(it):
                    half, ex = it
                    for ti in range(8):
                        tt = half * 8 + ti
                        op("dve", lambda e: e.tensor_scalar(Se[:, ti, :], iotaC[:], pos[:, tt, ex:ex + 1], Mf[:, tt, ex:ex + 1],
                                                            ALU.is_equal, ALU.mult),
                           reads=[R_io, R_pos], writes=[R_se[ti]])

                def gather(it):
                    half, ex = it
                    for m in range(NCH):
                        bank = m % 4
                        for ti in range(8):
                            tt = half * 8 + ti
                            mm(ps[:, bank, 0:CAP], f_tok[:, tt, m * 128:(m + 1) * 128], Se[:, ti, :], ti == 0, ti == 7,
                               [R_ft[tt], R_se[ti]], [psr[bank]])
                        if m % 2 == 0:
                            op("act", lambda e: e.activation(out=Xg[:, m, :], in_=ps[:, bank, 0:CAP], func=AF.Copy), reads=[psr[bank]], writes=[R_xg[m]])
                        else:
                            op("dve", lambda e: e.tensor_copy(Xg[:, m, :], ps[:, bank, 0:CAP]), reads=[psr[bank]], writes=[R_xg[m]])

                def build_GS(it):
                    half, ex = it
                    for gg in range(2):
                        g = half * 2 + gg
                        mm(ps[:, 4 + gg, :], sel[:, ex, :], posT[:, 0, gc(g)], True, False, [R_sel, R_posT], [psr[4 + gg]], track=False)
                        mm(ps[:, 4 + gg, :], sel[:, ex, :], posT[:, 1, gc(g)], False, True, [R_sel, R_posT], [psr[4 + gg]])
                        mm(ps[:, 6 + gg, :], sel[:, ex, :], GT[:, gc(g)], True, True, [R_sel, R_GT], [psr[6 + gg]])
                        op("act", lambda e: e.activation(out=Gb[:, gg * 512:(gg + 1) * 512], in_=ps[:, 6 + gg, :], func=AF.Copy),
                           reads=[psr[6 + gg]], writes=[R_gb[gg]])
                        for sc in range(NSC):
                            e_i = kk["e"] % 2
                            kk["e"] += 1
                            op("dve", lambda e: e.tensor_scalar(eqt[e_i][:], ps[:, 4 + gg, :], slotid[:, sc:sc + 1], None, ALU.is_equal),
                               reads=[psr[4 + gg], R_io], writes=[R_eq[e_i]])
                            op("dve", lambda e: e.tensor_tensor(out=GS[:, sc, gg * 512:(gg + 1) * 512], in0=eqt[e_i][:],
                                                                 in1=Gb[:, gg * 512:(gg + 1) * 512], op=ALU.mult),
                               reads=[R_eq[e_i], R_gb[gg]], writes=[R_gs[gg]])

                def gu(it):
                    half, ex = it
                    wg_v = kview(w_gu_m[ex, :, :])
                    nxt = pre.pop(it)
                    for mb in range(FE // 256):
                        (wg, wgr), (wu, wur) = nxt
                        if mb + 1 < FE // 256:
                            c0 = (mb + 1) * 256
                            nxt = (wload(wgs, lambda t: t[:], wg_v[:, :, c0:c0 + 256]),
                                   wload(wus, lambda t: t[:], wg_v[:, :, FE + c0:FE + c0 + 256]))
                        for mi in range(2):
                            m = mb * 2 + mi
                            gb = (kk["b"] % 2) * 2
                            ub = gb + 1
                            kk["b"] += 1
                            for kc in range(NCH):
                                mm(ps[:, gb, 0:CAP], wg[:, kc, mi * 128:(mi + 1) * 128], Xg[:, kc, :], kc == 0, kc == NCH - 1,
                                   [wgr, R_xg[kc]], [psr[gb]])
                            for kc in range(NCH):
                                mm(ps[:, ub, 0:CAP], wu[:, kc, mi * 128:(mi + 1) * 128], Xg[:, kc, :], kc == 0, kc == NCH - 1,
                                   [wur, R_xg[kc]], [psr[ub]])
                            s_ = kk["s"] % 2
                            kk["s"] += 1
                            op("act", lambda e: e.activation(out=sgt[s_][:], in_=ps[:, gb, 0:CAP], func=AF.Silu), reads=[psr[gb]], writes=[R_sg[s_]])
                            op("dve", lambda e: e.tensor_tensor(out=actT[:, m, :], in0=ps[:, ub, 0:CAP], in1=sgt[s_][:], op=ALU.mult),
                               reads=[psr[ub], R_sg[s_]], writes=[R_act[m]])

                def down(it, oh, banks):
                    half, ex = it
                    wd_v = kview(w_dn_m[ex, :, :])
                    for k4 in range(NKF // 4):
                        wd, wdr = wload(wds, lambda t: t[:], wd_v[:, 4 * k4:4 * k4 + 4, oh * 512:(oh + 1) * 512])
                        for kq in range(4):
                            kc = 4 * k4 + kq
                            for sc in range(NSC):
                                mm(ps[:, banks[sc], :], actT[:, kc, sc * 128:(sc + 1) * 128], wd[:, kq, :], kc == 0, kc == NKF - 1,
                                   [R_act[kc], wdr], [psr[banks[sc]]], track=(kc == NKF - 1 or (kq == 3 and sc == NSC - 1)))

                def evac_y(oh, banks):
                    for sc in range(NSC):
                        if sc % 2 == 0:
                            op("act", lambda e: e.activation(out=Ysm[oh][:, sc, :], in_=ps[:, banks[sc], :], func=AF.Copy), reads=[psr[banks[sc]]], writes=[R_y[oh]])
                        else:
                            op("dve", lambda e: e.tensor_copy(Ysm[oh][:, sc, :], ps[:, banks[sc], :]), reads=[psr[banks[sc]]], writes=[R_y[oh]])

                def scatter(it, oh, banks):
                    half, ex = it
                    for o4 in range(4):
                        o = oh * 4 + o4
                        for gg in range(2):
                            g = half * 2 + gg
                            bank = banks[kk["d"] % len(banks)]
                            kk["d"] += 1
                            for sc in range(NSC):
                                mm(ps[:, bank, :], Ysm[oh][:, sc, o4 * 128:(o4 + 1) * 128], GS[:, sc, gg * 512:(gg + 1) * 512],
                                   sc == 0, sc == NSC - 1, [R_y[oh], R_gs[gg]], [psr[bank]])
                            op("dve", lambda e: e.tensor_tensor(out=hT[:, o, gc(g)], in0=ps[:, bank, :], in1=hT[:, o, gc(g)], op=ALU.add),
                               reads=[psr[bank], R_h[o][g]], writes=[R_h[o][g]])

                prefetch_gu(items[0])
                build_Se(items[0])
                gather(items[0])
                for i, it in enumerate(items):
                    nx = items[i + 1] if i + 1 < len(items) else None
                    if nx is not None:
                        build_Se(nx)
                    gu(it)
                    if nx is not None:
                        prefetch_gu(nx)
                    build_GS(it)
                    down(it, 0, [4, 5, 6])
                    evac_y(0, [4, 5, 6])
                    if nx is not None:
                        gather(nx)
                    down(it, 1, [7, 0, 1])
                    scatter(it, 0, [2, 3])
                    evac_y(1, [7, 0, 1])
                    scatter(it, 1, [4, 5])
                fw.barrier()
        if dump("moe"):
            return nc

        ple(1)
        if dump("ple1"):
            return nc

        with contextlib.ExitStack() as ph:
            nrm = [sb(ph, f"nrm{i}", [128, NCH, 512], F32) for i in range(2)]
            R_n = [Region(), Region()]
            ot = [sb(ph, f"ot{i}", [128, D], F32) for i in range(2)]
            R_o = [Region(), Region()]
            ds_o = [fw.dsem("ds_o0"), fw.dsem("ds_o1")]
            cnt = [0]
            last_tok = []

            def norm_out(g, m, rstd_t, R_rs):
                b = g % 2
                op("dve", lambda e: e.scalar_tensor_tensor(out=nrm[b][:, m, :], in0=hT[:, m, gc(g)], scalar=gam[:, 5 * 8 + m:5 * 8 + m + 1],
                                                           in1=rstd_t[:], op0=ALU.mult, op1=ALU.mult),
                   reads=[R_h[m][g], R_rs], writes=[R_n[b]])

            def store(g):
                b = g % 2
                for t in range(4):
                    o_i = cnt[0] % 2
                    cnt[0] += 1
                    for hf in range(2):
                        bank = 2 * o_i + hf
                        for mi in range(4):
                            m = hf * 4 + mi
                            op("pe", lambda e: e.transpose(ps[:, bank, mi * 128:(mi + 1) * 128], nrm[b][:, m, t * 128:(t + 1) * 128], ident_f[:]),
                               reads=[R_n[b], R_const], writes=[psr[bank]], track=(mi == 3))
                        if hf == 0:
                            op("act", lambda e: e.activation(out=ot[o_i][:, 0:512], in_=ps[:, bank, :], func=AF.Copy), reads=[psr[bank]], writes=[R_o[o_i]])
                        else:
                            op("dve", lambda e: e.tensor_copy(ot[o_i][:, 512:1024], ps[:, bank, :]), reads=[psr[bank]], writes=[R_o[o_i]])
                    tok = fw.dma("sp", out[g * 512 + t * 128: g * 512 + (t + 1) * 128, :], ot[o_i][:], ds_o[o_i], reads=[R_o[o_i]])
                    last_tok.append(tok)

            rmsnorm(5, ph, out_fn=norm_out, post=store)
            for tok in last_tok[-2:]:
                fw._wait("sp", tok)
            fw.barrier()
    return nc


_W_KEYS = [("w_in", "w_in_a", 0), ("w_o_a", "w_o_a", 0), ("w_kv", "w_kv", None), ("w_q_b", "w_q_b", 0), ("w_o_b", "w_o_b", 0),
           ("w_gu_d", "w_gu_dense", 0), ("w_dn_d", "w_down_dense", 0), ("r_w", "router_w", 0), ("w_gu_m", "w_gu_moe", 0),
           ("w_dn_m", "w_down_moe", 0), ("w_pp", "w_ple_proj", None), ("w_pg", "w_ple_gate", None)]


def make_in_maps(inputs):
    f = lambda a: np.ascontiguousarray(np.asarray(a, dtype=np.float32))
    shared = {}
    for dst, src, idx in _W_KEYS:
        a = f(inputs[src])
        shared[dst] = f(a[idx]) if idx is not None else a
    norms = [f(inputs["attn_norm"])[0], f(inputs["ffn_norm"])[0], f(inputs["kv_norm"]), f(inputs["attn_norm"])[1],
             f(inputs["ffn_norm"])[1], f(inputs["final_norm"])]
    gam = np.stack([n.reshape(8, 128).T for n in norms], axis=1).reshape(128, 48)
    shared["gam"] = f(gam)
    shared["bf"] = f(inputs["b_f"]).reshape(16, 1)
    shared["rb"] = f(np.broadcast_to(f(inputs["router_b"]).reshape(1, 8), (128, 8)))
    x = f(inputs["x"])
    p = f(inputs["p"])
    maps = []
    for c in range(8):
        m = dict(shared)
        m["x"] = f(x[c])
        m["p"] = f(p[:, c])
        maps.append(m)
    return maps


def kernel(**inputs):
    nc = build()
    maps = make_in_maps(inputs)
    res = run_bass_kernel_spmd(nc, maps, core_ids=list(range(8)))
    return np.stack([np.asarray(res.results[c]["out"], dtype=np.float32) for c in range(8)], axis=0)
```

```python
import contextlib
import numpy as np
import concourse.bass as bass
import concourse.mybir as mybir
from concourse.bass_utils import run_bass_kernel_spmd

F32 = mybir.dt.float32
BF16 = mybir.dt.bfloat16
I32 = mybir.dt.int32
AF = mybir.ActivationFunctionType
ALU = mybir.AluOpType

S = 2048
D = 1024
NH = 16
FF = 2816
FE = 3584
NE = 8
NG = 4
NCH = 8
EPS = 1e-6
NEGBIG = -30000.0

DEBUG_STOP = None


class Region:
    __slots__ = ("w", "r")

    def __init__(self):
        self.w = None
        self.r = []


class FW:
    def __init__(self, nc, es):
        self.nc = nc
        self.es = es
        self.eng = {"pe": nc.tensor, "act": nc.scalar, "dve": nc.vector, "pool": nc.gpsimd, "sp": nc.sync}
        self.sem = {k: es.enter_context(nc.semaphore("s_" + k)) for k in self.eng}
        self.cnt = {k: 0 for k in self.eng}
        self.waited = {k: {} for k in self.eng}
        self.dsems = []

    def dsem(self, name):
        h = {"sem": self.es.enter_context(self.nc.semaphore(f"{name}_{len(self.dsems)}")), "val": 0}
        self.dsems.append(h)
        return h

    def _wait(self, e, tok):
        sem, val, owner = tok
        if owner is not None:
            if owner == "pe" and e == "pe":
                return
            assert val <= self.cnt[owner], f"wait on future token {owner}:{val} > {self.cnt[owner]}"
        key = id(sem)
        if self.waited[e].get(key, 0) >= val:
            return
        self.waited[e][key] = val
        self.eng[e].wait_ge(sem, val)

    def _deps(self, e, reads, writes):
        for r in reads:
            if r.w is not None:
                self._wait(e, r.w)
        for w in writes:
            if w.w is not None:
                self._wait(e, w.w)
            for t in w.r:
                self._wait(e, t)

    def _record(self, tok, reads, writes):
        for r in reads:
            r.r.append(tok)
            if len(r.r) > 64:
                best = {}
                for t in r.r:
                    k = id(t[0])
                    if k not in best or best[k][1] < t[1]:
                        best[k] = t
                r.r = list(best.values())
        for w in writes:
            w.w = tok
            w.r = []

    def op(self, e, fn, reads=(), writes=(), track=True):
        self._deps(e, reads, writes)
        inst = fn(self.eng[e])
        if track:
            self.cnt[e] += 1
            inst.then_inc(self.sem[e], 1)
            tok = (self.sem[e], self.cnt[e], e)
        else:
            tok = (self.sem[e], self.cnt[e] + 1, e)
        self._record(tok, reads, writes)
        return inst

    def dma(self, q, out, in_, ds, reads=(), writes=(), **kw):
        self._deps(q, reads, writes)
        inst = self.eng[q].dma_start(out=out, in_=in_, **kw)
        ds["val"] += 16
        inst.then_inc(ds["sem"], 16)
        tok = (ds["sem"], ds["val"], None)
        self._record(tok, reads, writes)
        return tok

    def barrier(self):
        for e in self.eng:
            for o in self.eng:
                if self.cnt[o] > 0:
                    self._wait(e, (self.sem[o], self.cnt[o], None))
            for ds in self.dsems:
                if ds["val"] > 0:
                    self._wait(e, (ds["sem"], ds["val"], None))


def build(stop=None):
    nc = bass.Bass("TRN2", target_bir_lowering=False)

    def din(name, shape, dt=F32):
        return nc.dram_tensor(name, list(shape), dt, kind="ExternalInput").ap()

    x = din("x", [S, D])
    p = din("p", [2, S, 256])
    gam_d = din("gam", [128, 48])
    bf_d = din("bf", [16, 1])
    rb_d = din("rb", [128, 8])
    w_in = din("w_in", [D, 3 * D + NH])
    w_o_a = din("w_o_a", [D, D])
    w_kv = din("w_kv", [D, 2 * D])
    w_q_b = din("w_q_b", [D, D])
    w_o_b = din("w_o_b", [D, D])
    w_gu_d = din("w_gu_d", [D, 2 * FF])
    w_dn_d = din("w_dn_d", [FF, D])
    r_w = din("r_w", [D, NE])
    lite = stop is not None and stop not in ("moe", "ple1")
    w_gu_m = din("w_gu_m", [NE, D, 2 * FE] if not lite else [1, 1, 2])
    w_dn_m = din("w_dn_m", [NE, FE, D] if not lite else [1, 1, 2])
    w_pp = din("w_pp", [2, 256, D])
    w_pg = din("w_pg", [2, D, D])
    out = nc.dram_tensor("out", [S, D], F32, kind="ExternalOutput").ap()
    cq_d = nc.dram_tensor("cq_d", [NH, 3, S], BF16, kind="Internal").ap()
    ck_d = nc.dram_tensor("ck_d", [NH, 3, S], BF16, kind="Internal").ap()
    dbg = None
    if stop is not None:
        dbg = nc.dram_tensor("dbg", [128, NCH, S], F32, kind="ExternalOutput").ap()

    def kview(w2d):
        return w2d.rearrange("(kc p) n -> p kc n", p=128)

    with contextlib.ExitStack() as es:
        fw = FW(nc, es)
        op = fw.op

        uid = [0]

        def sb(st, name, shape, dt):
            uid[0] += 1
            return st.enter_context(nc.sbuf_tensor(f"{name}_{uid[0]}", list(shape), dt))

        T = {}
        ps = es.enter_context(nc.psum_tensor("ps", [128, 8, 512], F32))
        psr = [Region() for _ in range(8)]

        hT = sb(es, "hT", [128, NCH, S], F32)
        R_h = [[Region() for _ in range(NG)] for _ in range(NCH)]
        def alloc_aT(st):
            T["aT"] = sb(st, "aT", [128, NCH, S], BF16)

        R_a = [[Region() for _ in range(NG)] for _ in range(NCH)]
        all_a = [R_a[m][g] for m in range(NCH) for g in range(NG)]

        ident_f = sb(es, "ident_f", [128, 128], F32)
        negI_b = sb(es, "negI_b", [128, 128], BF16)
        negbigI_b = sb(es, "negbigI_b", [128, 128], BF16)
        negtri_b = sb(es, "negtri_b", [128, 128], BF16)
        ones_b = sb(es, "ones_b", [128, 128], BF16)
        gam = sb(es, "gam", [128, 48], F32)
        nbf = sb(es, "nbf", [16, 1], F32)
        rb = sb(es, "rb", [128, 8], F32)
        R_const = Region()
        ds_c = fw.dsem("ds_c")
        with contextlib.ExitStack() as ph:
            io = sb(ph, "io", [128, 512], I32)
            R_io = Region()
            op("pool", lambda e: e.iota(io[:, 0:128], [[1, 128]], base=0, channel_multiplier=-1), writes=[R_io])
            op("dve", lambda e: e.tensor_scalar(ident_f[:], io[:, 0:128], 0.0, None, ALU.is_equal), reads=[R_io], writes=[R_const])
            op("dve", lambda e: e.tensor_scalar(negI_b[:], io[:, 0:128], 0.0, -1.0, ALU.is_equal, ALU.mult), reads=[R_io], writes=[R_const])
            op("dve", lambda e: e.tensor_scalar(negbigI_b[:], io[:, 0:128], 0.0, NEGBIG, ALU.is_equal, ALU.mult), reads=[R_io], writes=[R_const])
            op("dve", lambda e: e.tensor_scalar(negtri_b[:], io[:, 0:128], 0.0, -1.0, ALU.is_le, ALU.mult), reads=[R_io], writes=[R_const])
            op("dve", lambda e: e.memset(ones_b[:], 1.0), writes=[R_const])
            fw.dma("sp", gam[:], gam_d[:, :], ds_c, writes=[R_const])
            fw.dma("sp", nbf[:], bf_d[:, :], ds_c, writes=[R_const])
            fw.dma("sp", rb[:], rb_d[:, :], ds_c, writes=[R_const])
            op("dve", lambda e: e.tensor_scalar(nbf[:], nbf[:], -1.0, None, ALU.mult), reads=[R_const], writes=[R_const])
            fw.barrier()

        def gc(g):
            return slice(g * 512, (g + 1) * 512)

        def make_mask(st, name, cmp_op):
            mk = sb(st, name, [128, 4, 512], BF16)
            with contextlib.ExitStack() as tmpst:
                io_ = sb(tmpst, name + "_io", [128, 512], I32)
                R_io_ = Region()
                for v in range(4):
                    op("pool", lambda e: e.iota(io_[:, :], [[1, 512]], base=-128 * v, channel_multiplier=-1), writes=[R_io_])
                    op("dve", lambda e: e.tensor_scalar(mk[:, v, :], io_[:, :], 0.0, None, cmp_op), reads=[R_io_], writes=[R_const])
                fw.barrier()
            return mk

        def mm(out_ap, lhsT, rhs, start, stop, reads, writes, track=None):
            if track is None:
                track = stop
            return op("pe", lambda e: e.matmul(out_ap, lhsT, rhs, start=start, stop=stop), reads=reads, writes=writes, track=track)

        class Stream:
            def __init__(self, st, name, shape, dt, nbuf):
                self.t = [sb(st, f"{name}{i}", shape, dt) for i in range(nbuf)]
                self.r = [Region() for _ in range(nbuf)]
                self.ds = [fw.dsem(f"ds_{name}{i}") for i in range(nbuf)]
                self.i = 0
                self.n = nbuf

            def next(self):
                i = self.i
                self.i = (i + 1) % self.n
                return self.t[i], self.r[i], self.ds[i]

        def wload(stream, dst_fn, src, q="pool", **kw):
            t, r, ds = stream.next()
            fw.dma(q, dst_fn(t), src, ds, writes=[r], **kw)
            return t, r

        def dump(name):
            if stop == name:
                ds = fw.dsem("ds_dbg")
                tok = fw.dma("sp", dbg[:, :, :], hT[:], ds, reads=[R_h[m][g] for m in range(NCH) for g in range(NG)])
                fw._wait("sp", tok)
                return True
            return False

        def rmsnorm(gi, ph, out_fn=None, post=None):
            sq = [sb(ph, f"sq{gi}_{i}", [128, NCH, 512], BF16) for i in range(2)]
            R_sq = [Region(), Region()]
            lnt = sb(ph, f"lnt{gi}", [128, 512], F32)
            R_ln = Region()
            rstd = [sb(ph, f"rstd{gi}_{i}", [128, 512], F32) for i in range(2)]
            R_rs = [Region(), Region()]
            for g in range(NG):
                b = g % 2
                for m in range(NCH):
                    op("act", lambda e: e.activation(out=sq[b][:, m, :], in_=hT[:, m, gc(g)], func=AF.Square),
                       reads=[R_h[m][g]], writes=[R_sq[b]])
                bank = 6 + b
                for m in range(NCH):
                    mm(ps[:, bank, :], ones_b[:], sq[b][:, m, :], m == 0, m == NCH - 1, [R_sq[b]], [psr[bank]])
                op("act", lambda e: e.activation(out=lnt[:], in_=ps[:, bank, :], func=AF.Ln, bias=EPS, scale=1.0 / D),
                   reads=[psr[bank]], writes=[R_ln])
                op("act", lambda e: e.activation(out=rstd[b][:], in_=lnt[:], func=AF.Exp, scale=-0.5),
                   reads=[R_ln], writes=[R_rs[b]])
                for m in range(NCH):
                    if out_fn is None:
                        op("dve", lambda e: e.scalar_tensor_tensor(out=T["aT"][:, m, gc(g)], in0=hT[:, m, gc(g)],
                                                                   scalar=gam[:, gi * 8 + m:gi * 8 + m + 1], in1=rstd[b][:],
                                                                   op0=ALU.mult, op1=ALU.mult),
                           reads=[R_h[m][g], R_rs[b]], writes=[R_a[m][g]])
                    else:
                        out_fn(g, m, rstd[b], R_rs[b])
                if post is not None:
                    post(g)

        with contextlib.ExitStack() as ph:
            xs = Stream(ph, "xs", [128, 4, D], F32, 2)
            k = 0
            for g in range(NG):
                xt, xr = wload(xs, lambda t: t[:], x[g * 512:(g + 1) * 512, :].rearrange("(t q) d -> q t d", q=128), q="sp")
                for m in range(NCH):
                    bank = k % 4
                    k += 1
                    for t in range(4):
                        op("pe", lambda e: e.transpose(ps[:, bank, t * 128:(t + 1) * 128], xt[:, t, m * 128:(m + 1) * 128], ident_f[:]),
                           reads=[xr, R_const], writes=[psr[bank]], track=(t == 3))
                    if m % 2 == 0:
                        op("act", lambda e: e.activation(out=hT[:, m, gc(g)], in_=ps[:, bank, :], func=AF.Copy),
                           reads=[psr[bank]], writes=[R_h[m][g]])
                    else:
                        op("dve", lambda e: e.tensor_copy(hT[:, m, gc(g)], ps[:, bank, :]), reads=[psr[bank]], writes=[R_h[m][g]])
            fw.barrier()
        if dump("x"):
            return nc

        def out_proj_pair(wo, wor, OTp, R_ot, banks):
            k = 0
            for m in range(NCH):
                for g in range(NG):
                    bank = banks[k % len(banks)]
                    k += 1
                    mm(ps[:, bank, :], wo[:, m * 128:(m + 1) * 128], OTp[:, gc(g)], True, True, [wor, R_ot[g]], [psr[bank]])
                    op("dve", lambda e: e.tensor_tensor(out=hT[:, m, gc(g)], in0=ps[:, bank, :], in1=hT[:, m, gc(g)], op=ALU.add),
                       reads=[psr[bank], R_h[m][g]], writes=[R_h[m][g]])

        def vslice(h):
            if h % 2 == 0:
                return slice(h + 1, 18, 16 - h)
            return slice(0, h + 2, h + 1)

        R_v = [Region() for _ in range(16)]

        def v_proj(ph, wsrc, col0, fill):
            Vt = T["Vt"]
            wv = Stream(ph, "wv", [128, NCH, 512], BF16, 2)
            k = 0
            for half in range(2):
                wt, wr = wload(wv, lambda t: t[:], kview(wsrc)[:, :, col0 + half * 512: col0 + (half + 1) * 512])
                for tt in range(16):
                    bank = k % 4
                    k += 1
                    g = tt // 4
                    for kc in range(NCH):
                        mm(ps[:, bank, :], T["aT"][:, kc, tt * 128:(tt + 1) * 128], wt[:, kc, :], kc == 0, kc == NCH - 1,
                           [R_a[kc][g], wr], [psr[bank]])
                    dst = Vt[:, tt, half * 512:(half + 1) * 512]
                    src = ps[:, bank, :]
                    if tt % 2 == 0:
                        op("act", lambda e: e.activation(out=dst, in_=src, func=AF.Copy), reads=[psr[bank]], writes=[R_v[tt]])
                    else:
                        op("dve", lambda e: e.tensor_copy(dst, src), reads=[psr[bank]], writes=[R_v[tt]])

        phA = contextlib.ExitStack()
        Vt = T["Vt"] = sb(phA, "VtA", [128, 16, D], BF16)
        maskF = make_mask(phA, "maskF", ALU.is_lt)
        alloc_aT(phA)
        with contextlib.ExitStack() as ph:
            rmsnorm(0, ph)
            fw.barrier()
        with contextlib.ExitStack() as ph:
            wf = sb(ph, "wf", [128, NCH, NH], BF16)
            R_wf = Region()
            ds_wf = fw.dsem("ds_wf")
            fw.dma("pool", wf[:], kview(w_in)[:, :, 3 * D:3 * D + NH], ds_wf, writes=[R_wf])
            t1 = sb(ph, "t1", [NH, S], F32)
            cc = sb(ph, "cc", [NH, S], F32)
            one16 = sb(ph, "one16", [NH, S], F32)
            SQ = sb(ph, "SQ", [NH, 3, S], BF16)
            SK = sb(ph, "SK", [NH, 3, S], BF16)
            R_t = Region()
            op("pool", lambda e: e.memset(one16[:], 1.0), writes=[R_t])
            for g in range(NG):
                for kc in range(NCH):
                    mm(ps[0:NH, g, :], wf[:, kc, :], T["aT"][:, kc, gc(g)], kc == 0, kc == NCH - 1, [R_wf, R_a[kc][g]], [psr[g]])
                op("act", lambda e: e.activation(out=t1[:, gc(g)], in_=ps[0:NH, g, :], func=AF.Exp, bias=nbf[:, 0:1], scale=-1.0),
                   reads=[psr[g], R_const], writes=[R_t])
            op("act", lambda e: e.activation(out=t1[:], in_=t1[:], func=AF.Ln, bias=1.0, scale=1.0), reads=[R_t], writes=[R_t])
            op("dve", lambda e: e.tensor_tensor_scan(cc[:], one16[:], t1[:], 0.0, ALU.mult, ALU.subtract), reads=[R_t], writes=[R_t])
            op("dve", lambda e: e.tensor_copy(SQ[:, 0, :], cc[:]), reads=[R_t], writes=[R_t])
            op("dve", lambda e: e.tensor_tensor(out=cc[:], in0=cc[:], in1=SQ[:, 0, :], op=ALU.subtract), reads=[R_t], writes=[R_t])
            op("dve", lambda e: e.tensor_copy(SQ[:, 1, :], cc[:]), reads=[R_t], writes=[R_t])
            op("dve", lambda e: e.tensor_tensor(out=cc[:], in0=cc[:], in1=SQ[:, 1, :], op=ALU.subtract), reads=[R_t], writes=[R_t])
            op("dve", lambda e: e.tensor_copy(SQ[:, 2, :], cc[:]), reads=[R_t], writes=[R_t])
            op("dve", lambda e: e.tensor_scalar(SK[:], SQ[:], -1.0, None, ALU.mult), reads=[R_t], writes=[R_t])
            ds_cq = fw.dsem("ds_cq")
            fw.dma("sp", cq_d[:, :, :], SQ[:], ds_cq, reads=[R_t])
            fw.dma("sp", ck_d[:, :, :], SK[:], ds_cq, reads=[R_t])
            fw.barrier()
        with contextlib.ExitStack() as ph:
            v_proj(ph, w_in, 2 * D, 1.0)
            fw.barrier()
        with contextlib.ExitStack() as ph:
            wqs = Stream(ph, "wq", [128, NCH, 128], BF16, 2)
            wks = Stream(ph, "wk", [128, NCH, 128], BF16, 2)
            wos = Stream(ph, "wo", [128, D], BF16, 2)
            qa = [[sb(ph, f"qa{b}{hh}", [128, S], BF16) for hh in range(2)] for b in range(2)]
            ka = [[sb(ph, f"ka{b}{hh}", [128, S], BF16) for hh in range(2)] for b in range(2)]
            R_qa = [[[Region() for _ in range(NG)] for hh in range(2)] for b in range(2)]
            R_ka = [[[Region() for _ in range(NG)] for hh in range(2)] for b in range(2)]
            R_qaug = [[Region() for hh in range(2)] for b in range(2)]
            R_kaug = [[Region() for hh in range(2)] for b in range(2)]
            ds_aug = [[[fw.dsem(f"ds_aug{b}{hh}{z}") for z in range(2)] for hh in range(2)] for b in range(2)]
            PT = [sb(ph, f"PT{i}", [128, 512], BF16) for i in range(3)]
            R_pt = [Region() for _ in range(3)]
            OTp = [sb(ph, f"OTp{i}", [128, S], BF16) for i in range(2)]
            R_ot = [[Region() for _ in range(NG)] for _ in range(2)]
            _rc = sb(ph, "rc", [128, 512], F32)
            rc = [_rc, _rc]
            _rrc = Region()
            R_rc = [_rrc, _rrc]
            VA = [sb(ph, f"VA{i}", [128, 16, 128], BF16) for i in range(2)]
            VB = [sb(ph, f"VB{i}", [128, 16, 128], BF16) for i in range(2)]
            R_va = [Region(), Region()]
            R_vb = [Region(), Region()]
            for b in range(2):
                op("pool", lambda e: e.memset(VA[b][:, :, 64:128], 1.0), writes=[R_va[b]])
                op("pool", lambda e: e.memset(VB[b][:, :, 0:64], 1.0), writes=[R_vb[b]])
            for b in range(2):
                for hh in range(2):
                    op("pool", lambda e: e.memset(qa[b][hh][64:128, :], 0.0), writes=[R_qaug[b][hh]])
                    op("pool", lambda e: e.memset(qa[b][hh][96:99, :], 1.0), writes=[R_qaug[b][hh]])
                    op("pool", lambda e: e.memset(ka[b][hh][64:128, :], 0.0), writes=[R_kaug[b][hh]])
                    op("pool", lambda e: e.memset(ka[b][hh][64:67, :], 1.0), writes=[R_kaug[b][hh]])

            def proj_pair(pr):
                b = pr % 2
                wq, wqr = wload(wqs, lambda t: t[:], kview(w_in)[:, :, pr * 128:(pr + 1) * 128])
                wk, wkr = wload(wks, lambda t: t[:], kview(w_in)[:, :, D + pr * 128: D + (pr + 1) * 128])
                for hh in range(2):
                    h = 2 * pr + hh
                    fw.dma("sp", qa[b][hh][64:67, :], cq_d[h, :, :], ds_aug[b][hh][0], writes=[R_qaug[b][hh]])
                    fw.dma("sp", ka[b][hh][96:99, :], ck_d[h, :, :], ds_aug[b][hh][1], writes=[R_kaug[b][hh]])
                op("pool", lambda e: e.tensor_copy(VA[b][:, :, 0:64], Vt[:, :, (2 * pr) * 64:(2 * pr + 1) * 64]), reads=R_v, writes=[R_va[b]])
                op("pool", lambda e: e.tensor_copy(VB[b][:, :, 64:128], Vt[:, :, (2 * pr + 1) * 64:(2 * pr + 2) * 64]), reads=R_v, writes=[R_vb[b]])
                k = 0
                for (wt, wr, dst, R_d, scl) in ((wq, wqr, qa[b], R_qa[b], 0.125), (wk, wkr, ka[b], R_ka[b], 1.0)):
                    for g in range(NG):
                        bank = (7, 5, 6)[k % 3]
                        k += 1
                        for kc in range(NCH):
                            mm(ps[:, bank, :], wt[:, kc, :], T["aT"][:, kc, gc(g)], kc == 0, kc == NCH - 1, [wr, R_a[kc][g]], [psr[bank]])
                        for hh in range(2):
                            op("dve", lambda e: e.tensor_scalar(dst[hh][0:64, gc(g)], ps[hh * 64:(hh + 1) * 64, bank, :], scl, None, ALU.mult),
                               reads=[psr[bank]], writes=[R_d[hh][g]])

            sk = 0
            ak = 0
            proj_pair(0)
            for pr in range(NH // 2):
                b = pr % 2
                wo, wor = wload(wos, lambda t: t[:], w_o_a[pr * 128:(pr + 1) * 128, :])
                for hh in range(2):
                    h = 2 * pr + hh
                    for g in range(NG):
                        accb = 3 + (ak % 2)
                        denb = 5 + (ak % 2)
                        ak += 1
                        nj = 4 * g + 4
                        hp = slice(hh * 64, (hh + 1) * 64)

                        def s_mm(j, sbk):
                            diag = j >= 4 * g
                            mm(ps[:, sbk, :], ka[b][hh][0:99, j * 128:(j + 1) * 128], qa[b][hh][0:99, gc(g)], True, not diag,
                               [R_ka[b][hh][j // 4], R_kaug[b][hh], R_qa[b][hh][g], R_qaug[b][hh]], [psr[sbk]])
                            if diag:
                                mm(ps[:, sbk, :], negbigI_b[:], maskF[:, j - 4 * g, :], False, True, [R_const], [psr[sbk]])

                        sk0 = sk
                        sk += nj
                        s_mm(0, sk0 % 3)
                        if nj > 1:
                            s_mm(1, (sk0 + 1) % 3)

                        def pv_den(jj):
                            cb = (sk0 + jj) % 3
                            vsrc, vreg = (VA[b], R_va[b]) if hh == 0 else (VB[b], R_vb[b])
                            mm(ps[:, accb, :], vsrc[:, jj, :], PT[cb][:], jj == 0, jj == nj - 1,
                               [vreg, R_pt[cb]], [psr[accb]], track=True)

                        for j in range(nj):
                            cur = (sk0 + j) % 3
                            if j + 2 < nj:
                                s_mm(j + 2, (sk0 + j + 2) % 3)
                            op("act", lambda e: e.activation(out=PT[cur][:], in_=ps[:, cur, :], func=AF.Exp),
                               reads=[psr[cur]], writes=[R_pt[cur]])
                            if j >= 1:
                                pv_den(j - 1)
                        pv_den(nj - 1)
                        r2 = ak % 2
                        hq = slice((1 - hh) * 64, (2 - hh) * 64)
                        op("act", lambda e: e.activation(out=rc[r2][hp, :], in_=ps[hq, accb, :], func=AF.Ln), reads=[psr[accb]], writes=[R_rc[r2]])
                        op("act", lambda e: e.activation(out=rc[r2][hp, :], in_=rc[r2][hp, :], func=AF.Exp, scale=-1.0), reads=[R_rc[r2]], writes=[R_rc[r2]])
                        op("dve", lambda e: e.tensor_tensor(out=OTp[b][hp, gc(g)], in0=ps[hp, accb, :], in1=rc[r2][hp, :], op=ALU.mult),
                           reads=[psr[accb], R_rc[r2]], writes=[R_ot[b][g]])
                    if hh == 0 and pr + 1 < NH // 2:
                        proj_pair(pr + 1)
                out_proj_pair(wo, wor, OTp[b], R_ot[b], [7, 3, 4, 5, 6])
            fw.barrier()
        phA.close()
        if dump("attn0"):
            return nc

        def swiglu(ph, tag, w_gu2d, w_dn2d, ff, halves, gate_fn=None):
            nmb = ff // 256
            nkc = ff // 128
            wgs = Stream(ph, f"wg{tag}", [128, NCH, 256], BF16, 2)
            wus = Stream(ph, f"wu{tag}", [128, NCH, 256], BF16, 2)
            wds = Stream(ph, f"wd{tag}", [128, nkc, 128], BF16, 2)
            actT = sb(ph, f"actT{tag}", [128, nkc, 1024], BF16)
            R_act = [[Region() for _ in range(2)] for _ in range(nkc)]
            sgt = [sb(ph, f"sg{tag}{i}", [128, 512], BF16) for i in range(2)]
            R_sg = [Region(), Region()]
            tmp = [sb(ph, f"tmp{tag}{i}", [128, 512], F32) for i in range(2)]
            R_tmp = [Region(), Region()]
            return dict(nmb=nmb, nkc=nkc, wgs=wgs, wus=wus, wds=wds, actT=actT, R_act=R_act, sgt=sgt, R_sg=R_sg, tmp=tmp, R_tmp=R_tmp)

        def swiglu_run(st, w_gu2d, w_dn2d, ff, half, gate=None):
            nmb, nkc = st["nmb"], st["nkc"]
            actT, R_act = st["actT"], st["R_act"]
            k = 0
            sgk = 0
            wg_v = kview(w_gu2d)
            wd_v = kview(w_dn2d)
            nxt = (wload(st["wgs"], lambda t: t[:], wg_v[:, :, 0:256]), wload(st["wus"], lambda t: t[:], wg_v[:, :, ff:ff + 256]))
            for mb in range(nmb):
                (wg, wgr), (wu, wur) = nxt
                if mb + 1 < nmb:
                    c0 = (mb + 1) * 256
                    nxt = (wload(st["wgs"], lambda t: t[:], wg_v[:, :, c0:c0 + 256]),
                           wload(st["wus"], lambda t: t[:], wg_v[:, :, ff + c0:ff + c0 + 256]))
                for mi in range(2):
                    m = mb * 2 + mi
                    for gg in range(2):
                        g = half * 2 + gg
                        gb = (k % 2) * 2
                        ub = gb + 1
                        k += 1
                        for kc in range(NCH):
                            mm(ps[:, gb, :], wg[:, kc, mi * 128:(mi + 1) * 128], T["aT"][:, kc, gc(g)], kc == 0, kc == NCH - 1,
                               [wgr, R_a[kc][g]], [psr[gb]])
                        for kc in range(NCH):
                            mm(ps[:, ub, :], wu[:, kc, mi * 128:(mi + 1) * 128], T["aT"][:, kc, gc(g)], kc == 0, kc == NCH - 1,
                               [wur, R_a[kc][g]], [psr[ub]])
                        s = sgk % 2
                        sgk += 1
                        op("act", lambda e: e.activation(out=st["sgt"][s][:], in_=ps[:, gb, :], func=AF.Silu),
                           reads=[psr[gb]], writes=[st["R_sg"][s]])
                        op("dve", lambda e: e.tensor_tensor(out=actT[:, m, gg * 512:(gg + 1) * 512], in0=ps[:, ub, :], in1=st["sgt"][s][:], op=ALU.mult),
                           reads=[psr[ub], st["R_sg"][s]], writes=[R_act[m][gg]])
            nxt = wload(st["wds"], lambda t: t[:], wd_v[:, :, 0:128])
            k = 0
            for o in range(NCH):
                wd, wdr = nxt
                if o + 1 < NCH:
                    nxt = wload(st["wds"], lambda t: t[:], wd_v[:, :, (o + 1) * 128:(o + 2) * 128])
                for gg in range(2):
                    g = half * 2 + gg
                    bank = 4 + (k % 4)
                    k += 1
                    for kc in range(nkc):
                        mm(ps[:, bank, :], wd[:, kc, :], actT[:, kc, gg * 512:(gg + 1) * 512], kc == 0, kc == nkc - 1,
                           [wdr, R_act[kc][gg]], [psr[bank]])
                    if gate is None:
                        op("dve", lambda e: e.tensor_tensor(out=hT[:, o, gc(g)], in0=ps[:, bank, :], in1=hT[:, o, gc(g)], op=ALU.add),
                           reads=[psr[bank], R_h[o][g]], writes=[R_h[o][g]])
                    else:
                        gt, gr = gate
                        s = k % 2
                        op("dve", lambda e: e.tensor_tensor(out=st["tmp"][s][:], in0=ps[:, bank, :], in1=gt[:, gg * 512:(gg + 1) * 512], op=ALU.mult),
                           reads=[psr[bank], gr], writes=[st["R_tmp"][s]])
                        op("pool", lambda e: e.tensor_tensor(out=hT[:, o, gc(g)], in0=st["tmp"][s][:], in1=hT[:, o, gc(g)], op=ALU.add),
                           reads=[st["R_tmp"][s], R_h[o][g]], writes=[R_h[o][g]])

        phF = contextlib.ExitStack()
        alloc_aT(phF)
        with contextlib.ExitStack() as ph:
            rmsnorm(1, ph)
            fw.barrier()
        with contextlib.ExitStack() as ph:
            st = swiglu(ph, "d", w_gu_d, w_dn_d, FF, 2)
            for half in range(2):
                swiglu_run(st, w_gu_d, w_dn_d, FF, half)
            fw.barrier()
        phF.close()
        if dump("ffn0"):
            return nc

        def ple(i):
            with contextlib.ExitStack() as ph:
                alloc_aT(ph)
                for m in range(NCH):
                    for g in range(NG):
                        if (m + g) % 2 == 0:
                            op("act", lambda e: e.activation(out=T["aT"][:, m, gc(g)], in_=hT[:, m, gc(g)], func=AF.Copy),
                               reads=[R_h[m][g]], writes=[R_a[m][g]])
                        else:
                            op("pool", lambda e: e.tensor_copy(T["aT"][:, m, gc(g)], hT[:, m, gc(g)]), reads=[R_h[m][g]], writes=[R_a[m][g]])
                pt = sb(ph, f"pt{i}", [128, 16, 256], F32)
                R_p = Region()
                ds_p = fw.dsem(f"ds_p{i}")
                fw.dma("sp", pt[:], p[i, :, :].rearrange("(t q) f -> q t f", q=128), ds_p, writes=[R_p])
                pT = sb(ph, f"pT{i}", [128, 2, S], BF16)
                R_pT = [Region() for _ in range(NG)]
                wpp = sb(ph, f"wpp{i}", [128, 2, D], BF16)
                R_wpp = Region()
                ds_wpp = fw.dsem(f"ds_wpp{i}")
                fw.dma("pool", wpp[:], w_pp[i, :, :].rearrange("(kc q) n -> q kc n", q=128), ds_wpp, writes=[R_wpp])
                k = 0
                for g in range(NG):
                    for pc in range(2):
                        bank = k % 2
                        k += 1
                        for t in range(4):
                            op("pe", lambda e: e.transpose(ps[:, bank, t * 128:(t + 1) * 128], pt[:, 4 * g + t, pc * 128:(pc + 1) * 128], ident_f[:]),
                               reads=[R_p, R_const], writes=[psr[bank]], track=(t == 3))
                        op("dve", lambda e: e.tensor_copy(pT[:, pc, gc(g)], ps[:, bank, :]), reads=[psr[bank]], writes=[R_pT[g]])
                wpgs = Stream(ph, f"wpg{i}", [128, NCH, 128], BF16, 2)
                sgt = [sb(ph, f"psg{i}{j}", [128, 512], F32) for j in range(2)]
                R_sg = [Region(), Region()]
                tmp = [sb(ph, f"ptmp{i}{j}", [128, 512], F32) for j in range(2)]
                R_tmp = [Region(), Region()]
                wpg_v = kview(w_pg[i, :, :])
                nxt = wload(wpgs, lambda t: t[:], wpg_v[:, :, 0:128])
                k = 0
                for o in range(NCH):
                    wg, wgr = nxt
                    if o + 1 < NCH:
                        nxt = wload(wpgs, lambda t: t[:], wpg_v[:, :, (o + 1) * 128:(o + 2) * 128])
                    for g in range(NG):
                        gb = 2 + (k % 2) * 2
                        pb = gb + 1
                        s = k % 2
                        k += 1
                        for kc in range(NCH):
                            mm(ps[:, gb, :], wg[:, kc, :], T["aT"][:, kc, gc(g)], kc == 0, kc == NCH - 1, [wgr, R_a[kc][g]], [psr[gb]])
                        for kc in range(2):
                            mm(ps[:, pb, :], wpp[:, kc, o * 128:(o + 1) * 128], pT[:, kc, gc(g)], kc == 0, kc == 1, [R_wpp, R_pT[g]], [psr[pb]])
                        op("act", lambda e: e.activation(out=sgt[s][:], in_=ps[:, gb, :], func=AF.Sigmoid), reads=[psr[gb]], writes=[R_sg[s]])
                        op("dve", lambda e: e.tensor_tensor(out=tmp[s][:], in0=ps[:, pb, :], in1=sgt[s][:], op=ALU.mult),
                           reads=[psr[pb], R_sg[s]], writes=[R_tmp[s]])
                        op("pool", lambda e: e.tensor_tensor(out=hT[:, o, gc(g)], in0=tmp[s][:], in1=hT[:, o, gc(g)], op=ALU.add),
                           reads=[R_tmp[s], R_h[o][g]], writes=[R_h[o][g]])
                fw.barrier()

        ple(0)
        if dump("ple0"):
            return nc

        phB = contextlib.ExitStack()
        KT = sb(phB, "KT", [128, NCH, S], BF16)
        Vt = T["Vt"] = sb(phB, "VtB", [128, 16, D], BF16)
        maskS = make_mask(phB, "maskS", ALU.is_le)
        alloc_aT(phB)
        R_k = [[Region() for _ in range(NG)] for _ in range(NCH)]
        with contextlib.ExitStack() as ph:
            rmsnorm(2, ph)
            fw.barrier()
        with contextlib.ExitStack() as ph:
            wks = Stream(ph, "wkk", [128, NCH, 128], BF16, 2)
            wkv_v = kview(w_kv)
            k = 0
            for pr in range(NCH):
                wk, wkr = wload(wks, lambda t: t[:], wkv_v[:, :, pr * 128:(pr + 1) * 128])
                for g in range(NG):
                    bank = 4 + k % 4
                    k += 1
                    for kc in range(NCH):
                        mm(ps[:, bank, :], wk[:, kc, :], T["aT"][:, kc, gc(g)], kc == 0, kc == NCH - 1, [wkr, R_a[kc][g]], [psr[bank]])
                    if k % 2 == 0:
                        op("act", lambda e: e.activation(out=KT[:, pr, gc(g)], in_=ps[:, bank, :], func=AF.Copy), reads=[psr[bank]], writes=[R_k[pr][g]])
                    else:
                        op("dve", lambda e: e.tensor_copy(KT[:, pr, gc(g)], ps[:, bank, :]), reads=[psr[bank]], writes=[R_k[pr][g]])
            v_proj(ph, w_kv, D, 0.0)
            fw.barrier()

        with contextlib.ExitStack() as ph:
            rmsnorm(3, ph)
            fw.barrier()
        with contextlib.ExitStack() as ph:
            wqs = Stream(ph, "wq1", [128, NCH, 128], BF16, 2)
            wos = Stream(ph, "wo1", [128, D], BF16, 2)
            qz = [[sb(ph, f"qz{b}{hh}", [128, S], BF16) for hh in range(2)] for b in range(2)]
            R_q = [[[Region() for _ in range(NG)] for hh in range(2)] for _ in range(2)]
            R_qz = Region()
            for b_ in range(2):
                for hh_ in range(2):
                    op("pool", lambda e: e.memset(qz[b_][hh_][:], 0.0), writes=[R_qz])
            et = [sb(ph, f"et{i}", [128, 512], F32) for i in range(2)]
            R_e = [Region(), Region()]
            SPt = [sb(ph, f"SPt{i}", [128, 512], BF16) for i in range(3)]
            R_sp = [Region() for _ in range(3)]
            Wt = [sb(ph, f"Wt{i}", [128, 512], BF16) for i in range(2)]
            R_w = [Region(), Region()]
            Rsb = [sb(ph, f"Rsb{i}", [128, 512], BF16) for i in range(2)]
            R_r = [Region(), Region()]
            _ot1 = sb(ph, "OT1p", [128, S], BF16)
            OTp = [_ot1, _ot1]
            _rot1 = [Region() for _ in range(NG)]
            R_ot = [_rot1, _rot1]
            wq_v = kview(w_q_b)

            def qproj(pr):
                b = pr % 2
                wq, wqr = wload(wqs, lambda t: t[:], wq_v[:, :, pr * 128:(pr + 1) * 128])
                for g in range(NG):
                    bank = 7
                    for kc in range(NCH):
                        mm(ps[:, bank, :], wq[:, kc, :], T["aT"][:, kc, gc(g)], kc == 0, kc == NCH - 1, [wqr, R_a[kc][g]], [psr[bank]])
                    for hh_ in range(2):
                        hp_ = slice(hh_ * 64, (hh_ + 1) * 64)
                        op("dve", lambda e: e.tensor_scalar(qz[b][hh_][hp_, gc(g)], ps[hp_, bank, :], 0.125, None, ALU.mult),
                           reads=[psr[bank], R_qz], writes=[R_q[b][hh_][g]])

            cn = dict(a=0, b=0, e=0, sp=0, w=0, r=0)
            qproj(0)
            for pr in range(NH // 2):
                b = pr % 2
                wo, wor = wload(wos, lambda t: t[:], w_o_b[pr * 128:(pr + 1) * 128, :])
                for g in range(NG):
                    accb = 4 + (g % 2)
                    for hh in range(2):
                        h = 2 * pr + hh
                        hp = slice(hh * 64, (hh + 1) * 64)
                        nj = 4 * g + 4

                        def z_mm(bank, j, stop, track):
                            diag = j >= 4 * g
                            jc = slice(j * 128, (j + 1) * 128)
                            mm(ps[:, bank, :], KT[:, pr, jc], qz[b][hh][:, gc(g)], True, stop and not diag,
                               [R_k[pr][j // 4], R_q[b][hh][g]], [psr[bank]], track=(track and not diag))
                            if diag:
                                mm(ps[:, bank, :], negbigI_b[:], maskS[:, j - 4 * g, :], False, stop, [R_const], [psr[bank]], track=track)

                        def stageA(j):
                            A = cn["a"] % 2
                            cn["a"] += 1
                            z_mm(A, j, True, True)
                            e_i = cn["e"] % 2
                            cn["e"] += 1
                            op("act", lambda e: e.activation(out=et[e_i][:], in_=ps[:, A, :], func=AF.Exp), reads=[psr[A]], writes=[R_e[e_i]])
                            s_i = cn["sp"] % 3
                            cn["sp"] += 1
                            op("act", lambda e: e.activation(out=SPt[s_i][:], in_=et[e_i][:], func=AF.Ln, bias=1.0, scale=1.0),
                               reads=[R_e[e_i]], writes=[R_sp[s_i]])
                            return s_i

                        js = list(range(nj - 1, -1, -1))
                        nt = len(js)
                        sp_of = {0: stageA(js[0])}
                        if nt > 1:
                            sp_of[1] = stageA(js[1])
                        pend = None

                        def emit_pv(pv):
                            pj, pw, pfirst = pv
                            mm(ps[:, 4 + hh, :], Vt[:, pj, pr * 128:(pr + 1) * 128], Wt[pw][:], pfirst, pj == 0,
                               [R_v[pj], R_w[pw]], [psr[4 + hh]], track=True)

                        for idx, j in enumerate(js):
                            first = idx == 0
                            if idx + 2 < nt:
                                sp_of[idx + 2] = stageA(js[idx + 2])
                            s_i = sp_of[idx]
                            r_prev = (cn["r"] - 1) % 2
                            if j > 0:
                                mm(ps[:, 6, :], ones_b[:], SPt[s_i][:], True, True, [R_const, R_sp[s_i]], [psr[6]], track=True)
                                r_i = cn["r"] % 2
                                cn["r"] += 1
                                if first:
                                    op("dve", lambda e: e.tensor_copy(Rsb[r_i][:], ps[:, 6, :]), reads=[psr[6]], writes=[R_r[r_i]])
                                else:
                                    op("dve", lambda e: e.tensor_tensor(out=Rsb[r_i][:], in0=ps[:, 6, :], in1=Rsb[r_prev][:], op=ALU.add),
                                       reads=[psr[6], R_r[r_prev]], writes=[R_r[r_i]])
                            B = 2 + (cn["b"] % 2)
                            cn["b"] += 1
                            z_mm(B, j, False, False)
                            mm(ps[:, B, :], negtri_b[:], SPt[s_i][:], False, first, [R_const, R_sp[s_i]], [psr[B]], track=first)
                            if not first:
                                mm(ps[:, B, :], negI_b[:], Rsb[r_prev][:], False, True, [R_const, R_r[r_prev]], [psr[B]])
                            w_i = cn["w"] % 2
                            cn["w"] += 1
                            op("act", lambda e: e.activation(out=Wt[w_i][:], in_=ps[:, B, :], func=AF.Exp), reads=[psr[B]], writes=[R_w[w_i]])
                            if pend is not None:
                                emit_pv(pend)
                            pend = (j, w_i, first)
                        emit_pv(pend)
                        op("dve", lambda e: e.tensor_copy(OTp[b][hp, gc(g)], ps[hp, 4 + hh, :]), reads=[psr[4 + hh]], writes=[R_ot[b][g]])
                    if g == 1 and pr + 1 < NH // 2:
                        qproj(pr + 1)
                out_proj_pair(wo, wor, OTp[b], R_ot[b], [7, 0, 1, 2, 3])
            fw.barrier()
        phB.close()
        if dump("attn1"):
            return nc

        CAP = 384
        NSC = CAP // 128
        with contextlib.ExitStack() as ph:
            G = sb(ph, "G", [128, 16, NE], F32)
            R_G = Region()
            GT = sb(ph, "GT", [NE, S], BF16)
            R_GT = Region()
            sel = sb(ph, "sel", [NE, NE, 128], BF16)
            R_sel = Region()
            f_tok = sb(ph, "f_tok", [128, 16, D], BF16)
            R_ft = [Region() for _ in range(16)]
            posT = sb(ph, "posT", [NE, 2, S], BF16)
            R_posT = Region()
            pos = sb(ph, "pos", [128, 16, NE], F32)
            Mf = sb(ph, "Mf", [128, 16, NE], F32)
            R_pos = Region()
            iotaC = sb(ph, "iotaC", [128, CAP], F32)
            slotid = sb(ph, "slotid", [128, NSC], F32)
            R_io = Region()
            with contextlib.ExitStack() as ph2:
                alloc_aT(ph2)
                aT = T["aT"]
                wr_f = sb(ph2, "wr_f", [128, NCH, NE], F32)
                wr_hi = sb(ph2, "wr_hi", [128, NCH, NE], BF16)
                wr_lo = sb(ph2, "wr_lo", [128, NCH, NE], BF16)
                R_wr = Region()
                ds_wr = fw.dsem("ds_wr")
                fw.dma("sp", wr_f[:], kview(r_w), ds_wr, writes=[R_wr])
                op("dve", lambda e: e.tensor_copy(wr_hi[:], wr_f[:]), reads=[R_wr], writes=[R_wr])
                op("dve", lambda e: e.tensor_tensor(out=wr_f[:], in0=wr_f[:], in1=wr_hi[:], op=ALU.subtract), reads=[R_wr], writes=[R_wr])
                op("dve", lambda e: e.tensor_copy(wr_lo[:], wr_f[:]), reads=[R_wr], writes=[R_wr])
                fT32 = sb(ph2, "fT32", [128, 512], F32)
                R_f32 = Region()
                loT = [sb(ph2, f"loT{i}", [128, NCH, 512], BF16) for i in range(2)]
                R_lo = [Region(), Region()]
                lg = sb(ph2, "lg", [128, 16, NE], F32)
                R_lg = Region()
                mx = sb(ph2, "mx", [128, 8], F32)
                nv1 = sb(ph2, "nv1", [128, 1], F32)
                msk = sb(ph2, "msk", [128, NE], F32)
                eg = sb(ph2, "eg", [128, NE], F32)
                den = sb(ph2, "den", [128, 1], F32)
                R_s = Region()
                io2 = sb(ph2, "io2", [NE, NE, 128], I32)
                op("pool", lambda e: e.iota(io2[:], [[-1, NE], [0, 128]], base=0, channel_multiplier=1), writes=[R_sel])
                op("dve", lambda e: e.tensor_scalar(sel[:], io2[:], 0.0, None, ALU.is_equal), reads=[R_sel], writes=[R_sel])
                io3 = sb(ph2, "io3", [128, CAP], I32)
                op("pool", lambda e: e.iota(io3[:], [[1, CAP]], base=0, channel_multiplier=0), writes=[R_io])
                op("dve", lambda e: e.tensor_copy(iotaC[:], io3[:]), reads=[R_io], writes=[R_io])
                op("pool", lambda e: e.iota(io3[:, 0:NSC], [[128, NSC]], base=0, channel_multiplier=1), reads=[R_io], writes=[R_io])
                op("dve", lambda e: e.tensor_copy(slotid[:], io3[:, 0:NSC]), reads=[R_io], writes=[R_io])

                def norm_out(g, m, rstd_t, R_rs):
                    b = g % 2
                    op("dve", lambda e: e.scalar_tensor_tensor(out=fT32[:], in0=hT[:, m, gc(g)], scalar=gam[:, 4 * 8 + m:4 * 8 + m + 1],
                                                               in1=rstd_t[:], op0=ALU.mult, op1=ALU.mult),
                       reads=[R_h[m][g], R_rs], writes=[R_f32])
                    op("dve", lambda e: e.tensor_copy(aT[:, m, gc(g)], fT32[:]), reads=[R_f32], writes=[R_a[m][g]])
                    op("dve", lambda e: e.tensor_tensor(out=loT[b][:, m, :], in0=fT32[:], in1=aT[:, m, gc(g)], op=ALU.subtract),
                       reads=[R_f32, R_a[m][g]], writes=[R_lo[b]])

                def route(g):
                    b = g % 2
                    for t in range(4):
                        tt = 4 * g + t
                        tc_ = slice(t * 128, (t + 1) * 128)
                        bank = 4 + (tt % 2)
                        n = 0
                        for kc in range(NCH):
                            for (l, r_, rr) in ((aT[:, kc, tt * 128:(tt + 1) * 128], wr_hi, R_a[kc][g]),
                                                (loT[b][:, kc, tc_], wr_hi, R_lo[b]),
                                                (aT[:, kc, tt * 128:(tt + 1) * 128], wr_lo, R_a[kc][g])):
                                mm(ps[:, bank, 0:NE], l, r_[:, kc, :], n == 0, n == 3 * NCH - 1, [rr, R_wr], [psr[bank]])
                                n += 1
                        op("dve", lambda e: e.tensor_tensor(out=lg[:, tt, :], in0=ps[:, bank, 0:NE], in1=rb[:], op=ALU.add),
                           reads=[psr[bank], R_const], writes=[R_lg])
                        op("dve", lambda e: e.max(mx[:], lg[:, tt, :]), reads=[R_lg], writes=[R_s])
                        op("dve", lambda e: e.tensor_scalar(nv1[:], mx[:, 0:1], -1.0, None, ALU.mult), reads=[R_s], writes=[R_s])
                        op("dve", lambda e: e.tensor_scalar(msk[:], lg[:, tt, :], mx[:, 1:2], None, ALU.is_ge), reads=[R_lg, R_s], writes=[R_s])
                        op("dve", lambda e: e.tensor_copy(Mf[:, tt, :], msk[:]), reads=[R_s], writes=[R_pos])
                        op("act", lambda e: e.activation(out=eg[:], in_=lg[:, tt, :], func=AF.Exp, bias=nv1[:, 0:1], scale=1.0),
                           reads=[R_lg, R_s], writes=[R_s])
                        op("dve", lambda e: e.tensor_tensor(out=eg[:], in0=eg[:], in1=msk[:], op=ALU.mult), reads=[R_s], writes=[R_s])
                        op("dve", lambda e: e.tensor_reduce(out=den[:], in_=eg[:], axis=mybir.AxisListType.X, op=ALU.add), reads=[R_s], writes=[R_s])
                        op("dve", lambda e: e.reciprocal(out=den[:], in_=den[:]), reads=[R_s], writes=[R_s])
                        op("dve", lambda e: e.tensor_scalar(G[:, tt, :], eg[:], den[:, 0:1], None, ALU.mult), reads=[R_s], writes=[R_G])
                        for mq in range(2):
                            fb = 2 * (tt % 2) + mq
                            for mi in range(4):
                                m = mq * 4 + mi
                                mm(ps[:, fb, mi * 128:(mi + 1) * 128], aT[:, m, tt * 128:(tt + 1) * 128], negI_b[:], True, True,
                                   [R_a[m][g], R_const], [psr[fb]], track=(mi == 3))
                            op("dve", lambda e: e.tensor_scalar(f_tok[:, tt, mq * 512:(mq + 1) * 512], ps[:, fb, :], -1.0, None, ALU.mult),
                               reads=[psr[fb]], writes=[R_ft[tt]])

                rmsnorm(4, ph2, out_fn=norm_out, post=route)
                for g in range(NG):
                    bank = g % 2
                    for t in range(4):
                        op("pe", lambda e: e.transpose(ps[0:NE, bank, t * 128:(t + 1) * 128], G[:, 4 * g + t, :], ident_f[:]),
                           reads=[R_G, R_const], writes=[psr[bank]], track=(t == 3))
                    op("dve", lambda e: e.tensor_copy(GT[:, gc(g)], ps[0:NE, bank, :]), reads=[psr[bank]], writes=[R_GT])
                Mk = sb(ph2, "Mk", [128, 16 * NE], BF16)
                tot = sb(ph2, "tot", [128, 16, NE], F32)
                offs = sb(ph2, "offs", [128, 16, NE], F32)
                stri_b = sb(ph2, "stri_b", [128, 128], BF16)
                io4 = sb(ph2, "io4", [128, 128], I32)
                op("pool", lambda e: e.iota(io4[:], [[1, 128]], base=0, channel_multiplier=-1), writes=[R_io])
                op("dve", lambda e: e.tensor_scalar(stri_b[:], io4[:], 0.0, None, ALU.is_gt), reads=[R_io], writes=[R_io])
                op("dve", lambda e: e.tensor_copy(Mk[:], Mf[:].rearrange("p a b -> p (a b)")), reads=[R_pos], writes=[R_pos])
                mm(ps[:, 4, 0:128], stri_b[:], Mk[:], True, True, [R_io, R_pos], [psr[4]])
                mm(ps[:, 5, 0:128], ones_b[:], Mk[:], True, True, [R_const, R_pos], [psr[5]])
                op("dve", lambda e: e.tensor_copy(tot[:].rearrange("p a b -> p (a b)"), ps[:, 5, 0:128]), reads=[psr[5]], writes=[R_pos])
                op("dve", lambda e: e.memset(offs[:], 0.0), writes=[R_pos])
                for k_ in list(range(1, 8)) + list(range(9, 16)):
                    op("dve", lambda e: e.tensor_tensor(out=offs[:, k_, :], in0=offs[:, k_ - 1, :], in1=tot[:, k_ - 1, :], op=ALU.add),
                       reads=[R_pos], writes=[R_pos])
                op("dve", lambda e: e.tensor_tensor(out=pos[:].rearrange("p a b -> p (a b)"), in0=ps[:, 4, 0:128],
                                                    in1=offs[:].rearrange("p a b -> p (a b)"), op=ALU.add),
                   reads=[psr[4], R_pos], writes=[R_pos])
                for g in range(NG):
                    bank = 6 + g % 2
                    for t in range(4):
                        op("pe", lambda e: e.transpose(ps[0:NE, bank, t * 128:(t + 1) * 128], pos[:, 4 * g + t, :], ident_f[:]),
                           reads=[R_pos, R_const], writes=[psr[bank]], track=(t == 3))
                    op("dve", lambda e: e.tensor_copy(posT[:, 0, gc(g)], ps[0:NE, bank, :]), reads=[psr[bank]], writes=[R_posT])
                    op("dve", lambda e: e.tensor_tensor(out=posT[:, 1, gc(g)], in0=ps[0:NE, bank, :], in1=posT[:, 0, gc(g)], op=ALU.subtract),
                       reads=[psr[bank], R_posT], writes=[R_posT])
                fw.barrier()
            with contextlib.ExitStack() as ph2:
                NKF = FE // 128
                wgs = Stream(ph2, "wgm", [128, NCH, 256], BF16, 3)
                wus = Stream(ph2, "wum", [128, NCH, 256], BF16, 3)
                wds = Stream(ph2, "wdm", [128, 4, 512], BF16, 3)
                Se = sb(ph2, "Se", [128, 8, CAP], BF16)
                R_se = [Region() for _ in range(8)]
                Xg = sb(ph2, "Xg", [128, NCH, CAP], BF16)
                R_xg = [Region() for _ in range(NCH)]
                actT = sb(ph2, "actTm", [128, NKF, CAP], BF16)
                R_act = [Region() for _ in range(NKF)]
                Ysm = [sb(ph2, f"Ysm{i}", [128, NSC, 512], BF16) for i in range(2)]
                R_y = [Region(), Region()]
                GS = sb(ph2, "GS", [128, NSC, 1024], BF16)
                R_gs = [Region(), Region()]
                Gb = sb(ph2, "Gb", [128, 1024], BF16)
                R_gb = [Region(), Region()]
                sgt = [sb(ph2, f"sgm{i}", [128, CAP], BF16) for i in range(2)]
                R_sg = [Region(), Region()]
                kk = dict(b=0, s=0, d=0, y=0, e=0)
                eqt = [sb(ph2, f"eqt{i}", [128, 512], BF16) for i in range(2)]
                R_eq = [Region(), Region()]
                items = [(half, ex) for half in range(2) for ex in range(NE)]
                pre = {}

                def prefetch_gu(it):
                    half, ex = it
                    wg_v = kview(w_gu_m[ex, :, :])
                    pre[it] = (wload(wgs, lambda t: t[:], wg_v[:, :, 0:256]), wload(wus, lambda t: t[:], wg_v[:, :, FE:FE + 256]))

                def build_Se(it):
                    half, ex = it
                    for ti in range(8):
                        tt = half * 8 + ti
                        op("dve", lambda e: e.tensor_scalar(Se[:, ti, :], iotaC[:], pos[:, tt, ex:ex + 1], Mf[:, tt, ex:ex + 1],
                                                            ALU.is_equal, ALU.mult),
                           reads=[R_io, R_pos], writes=[R_se[ti]])

                def gather(it):
                    half, ex = it
                    for m in range(NCH):
                        bank = m % 4
                        for ti in range(8):
                            tt = half * 8 + ti
                            mm(ps[:, bank, 0:CAP], f_tok[:, tt, m * 128:(m + 1) * 128], Se[:, ti, :], ti == 0, ti == 7,
                               [R_ft[tt], R_se[ti]], [psr[bank]])
                        if m % 2 == 0:
                            op("act", lambda e: e.activation(out=Xg[:, m, :], in_=ps[:, bank, 0:CAP], func=AF.Copy), reads=[psr[bank]], writes=[R_xg[m]])
                        else:
                            op("dve", lambda e: e.tensor_copy(Xg[:, m, :], ps[:, bank, 0:CAP]), reads=[psr[bank]], writes=[R_xg[m]])

                def build_GS(it):
                    half, ex = it
                    for gg in range(2):
                        g = half * 2 + gg
                        mm(ps[:, 4 + gg, :], sel[:, ex, :], posT[:, 0, gc(g)], True, False, [R_sel, R_posT], [psr[4 + gg]], track=False)
                        mm(ps[:, 4 + gg, :], sel[:, ex, :], posT[:, 1, gc(g)], False, True, [R_sel, R_posT], [psr[4 + gg]])
                        mm(ps[:, 6 + gg, :], sel[:, ex, :], GT[:, gc(g)], True, True, [R_sel, R_GT], [psr[6 + gg]])
                        op("act", lambda e: e.activation(out=Gb[:, gg * 512:(gg + 1) * 512], in_=ps[:, 6 + gg, :], func=AF.Copy),
                           reads=[psr[6 + gg]], writes=[R_gb[gg]])
                        for sc in range(NSC):
                            e_i = kk["e"] % 2
                            kk["e"] += 1
                            op("dve", lambda e: e.tensor_scalar(eqt[e_i][:], ps[:, 4 + gg, :], slotid[:, sc:sc + 1], None, ALU.is_equal),
                               reads=[psr[4 + gg], R_io], writes=[R_eq[e_i]])
                            op("dve", lambda e: e.tensor_tensor(out=GS[:, sc, gg * 512:(gg + 1) * 512], in0=eqt[e_i][:],
                                                                 in1=Gb[:, gg * 512:(gg + 1) * 512], op=ALU.mult),
                               reads=[R_eq[e_i], R_gb[gg]], writes=[R_gs[gg]])

                def gu(it):
                    half, ex = it
                    wg_v = kview(w_gu_m[ex, :, :])
                    nxt = pre.pop(it)
                    for mb in range(FE // 256):
                        (wg, wgr), (wu, wur) = nxt
                        if mb + 1 < FE // 256:
                            c0 = (mb + 1) * 256
                            nxt = (wload(wgs, lambda t: t[:], wg_v[:, :, c0:c0 + 256]),
                                   wload(wus, lambda t: t[:], wg_v[:, :, FE + c0:FE + c0 + 256]))
                        for mi in range(2):
                            m = mb * 2 + mi
                            gb = (kk["b"] % 2) * 2
                            ub = gb + 1
                            kk["b"] += 1
                            for kc in range(NCH):
                                mm(ps[:, gb, 0:CAP], wg[:, kc, mi * 128:(mi + 1) * 128], Xg[:, kc, :], kc == 0, kc == NCH - 1,
                                   [wgr, R_xg[kc]], [psr[gb]])
                            for kc in range(NCH):
                                mm(ps[:, ub, 0:CAP], wu[:, kc, mi * 128:(mi + 1) * 128], Xg[:, kc, :], kc == 0, kc == NCH - 1,
                                   [wur, R_xg[kc]], [psr[ub]])
                            s_ = kk["s"] % 2
                            kk["s"] += 1
                            op("act", lambda e: e.activation(out=sgt[s_][:], in_=ps[:, gb, 0:CAP], func=AF.Silu), reads=[psr[gb]], writes=[R_sg[s_]])
                            op("dve", lambda e: e.tensor_tensor(out=actT[:, m, :], in0=ps[:, ub, 0:CAP], in1=sgt[s_][:], op=ALU.mult),
                               reads=[psr[ub], R_sg[s_]], writes=[R_act[m]])

                def down(it, oh, banks):
                    half, ex = it
                    wd_v = kview(w_dn_m[ex, :, :])
                    for k4 in range(NKF // 4):
                        wd, wdr = wload(wds, lambda t: t[:], wd_v[:, 4 * k4:4 * k4 + 4, oh * 512:(oh + 1) * 512])
                        for kq in range(4):
                            kc = 4 * k4 + kq
                            for sc in range(NSC):
                                mm(ps[:, banks[sc], :], actT[:, kc, sc * 128:(sc + 1) * 128], wd[:, kq, :], kc == 0, kc == NKF - 1,
                                   [R_act[kc], wdr], [psr[banks[sc]]], track=(kc == NKF - 1 or (kq == 3 and sc == NSC - 1)))

                def evac_y(oh, banks):
                    for sc in range(NSC):
                        if sc % 2 == 0:
                            op("act", lambda e: e.activation(out=Ysm[oh][:, sc, :], in_=ps[:, banks[sc], :], func=AF.Copy), reads=[psr[banks[sc]]], writes=[R_y[oh]])
                        else:
                            op("dve", lambda e: e.tensor_copy(Ysm[oh][:, sc, :], ps[:, banks[sc], :]), reads=[psr[banks[sc]]], writes=[R_y[oh]])

                def scatter(it, oh, banks):
                    half, ex = it
                    for o4 in range(4):
                        o = oh * 4 + o4
                        for gg in range(2):
                            g = half * 2 + gg
                            bank = banks[kk["d"] % len(banks)]
                            kk["d"] += 1
                            for sc in range(NSC):
                                mm(ps[:, bank, :], Ysm[oh][:, sc, o4 * 128:(o4 + 1) * 128], GS[:, sc, gg * 512:(gg + 1) * 512],
                                   sc == 0, sc == NSC - 1, [R_y[oh], R_gs[gg]], [psr[bank]])
                            op("dve", lambda e: e.tensor_tensor(out=hT[:, o, gc(g)], in0=ps[:, bank, :], in1=hT[:, o, gc(g)], op=ALU.add),
                               reads=[psr[bank], R_h[o][g]], writes=[R_h[o][g]])

                prefetch_gu(items[0])
                build_Se(items[0])
                gather(items[0])
                for i, it in enumerate(items):
                    nx = items[i + 1] if i + 1 < len(items) else None
                    if nx is not None:
                        build_Se(nx)
                    gu(it)
                    if nx is not None:
                        prefetch_gu(nx)
                    build_GS(it)
                    down(it, 0, [4, 5, 6])
                    evac_y(0, [4, 5, 6])
                    if nx is not None:
                        gather(nx)
                    down(it, 1, [7, 0, 1])
                    scatter(it, 0, [2, 3])
                    evac_y(1, [7, 0, 1])
                    scatter(it, 1, [4, 5])
                fw.barrier()
        if dump("moe"):
            return nc

        ple(1)
        if dump("ple1"):
            return nc

        with contextlib.ExitStack() as ph:
            nrm = [sb(ph, f"nrm{i}", [128, NCH, 512], F32) for i in range(2)]
            R_n = [Region(), Region()]
            ot = [sb(ph, f"ot{i}", [128, D], F32) for i in range(2)]
            R_o = [Region(), Region()]
            ds_o = [fw.dsem("ds_o0"), fw.dsem("ds_o1")]
            cnt = [0]
            last_tok = []

            def norm_out(g, m, rstd_t, R_rs):
                b = g % 2
                op("dve", lambda e: e.scalar_tensor_tensor(out=nrm[b][:, m, :], in0=hT[:, m, gc(g)], scalar=gam[:, 5 * 8 + m:5 * 8 + m + 1],
                                                           in1=rstd_t[:], op0=ALU.mult, op1=ALU.mult),
                   reads=[R_h[m][g], R_rs], writes=[R_n[b]])

            def store(g):
                b = g % 2
                for t in range(4):
                    o_i = cnt[0] % 2
                    cnt[0] += 1
                    for hf in range(2):
                        bank = 2 * o_i + hf
                        for mi in range(4):
                            m = hf * 4 + mi
                            op("pe", lambda e: e.transpose(ps[:, bank, mi * 128:(mi + 1) * 128], nrm[b][:, m, t * 128:(t + 1) * 128], ident_f[:]),
                               reads=[R_n[b], R_const], writes=[psr[bank]], track=(mi == 3))
                        if hf == 0:
                            op("act", lambda e: e.activation(out=ot[o_i][:, 0:512], in_=ps[:, bank, :], func=AF.Copy), reads=[psr[bank]], writes=[R_o[o_i]])
                        else:
                            op("dve", lambda e: e.tensor_copy(ot[o_i][:, 512:1024], ps[:, bank, :]), reads=[psr[bank]], writes=[R_o[o_i]])
                    tok = fw.dma("sp", out[g * 512 + t * 128: g * 512 + (t + 1) * 128, :], ot[o_i][:], ds_o[o_i], reads=[R_o[o_i]])
                    last_tok.append(tok)

            rmsnorm(5, ph, out_fn=norm_out, post=store)
            for tok in last_tok[-2:]:
                fw._wait("sp", tok)
            fw.barrier()
    return nc


_W_KEYS = [("w_in", "w_in_a", 0), ("w_o_a", "w_o_a", 0), ("w_kv", "w_kv", None), ("w_q_b", "w_q_b", 0), ("w_o_b", "w_o_b", 0),
           ("w_gu_d", "w_gu_dense", 0), ("w_dn_d", "w_down_dense", 0), ("r_w", "router_w", 0), ("w_gu_m", "w_gu_moe", 0),
           ("w_dn_m", "w_down_moe", 0), ("w_pp", "w_ple_proj", None), ("w_pg", "w_ple_gate", None)]


def make_in_maps(inputs):
    f = lambda a: np.ascontiguousarray(np.asarray(a, dtype=np.float32))
    shared = {}
    for dst, src, idx in _W_KEYS:
        a = f(inputs[src])
        shared[dst] = f(a[idx]) if idx is not None else a
    norms = [f(inputs["attn_norm"])[0], f(inputs["ffn_norm"])[0], f(inputs["kv_norm"]), f(inputs["attn_norm"])[1],
             f(inputs["ffn_norm"])[1], f(inputs["final_norm"])]
    gam = np.stack([n.reshape(8, 128).T for n in norms], axis=1).reshape(128, 48)
    shared["gam"] = f(gam)
    shared["bf"] = f(inputs["b_f"]).reshape(16, 1)
    shared["rb"] = f(np.broadcast_to(f(inputs["router_b"]).reshape(1, 8), (128, 8)))
    x = f(inputs["x"])
    p = f(inputs["p"])
    maps = []
    for c in range(8):
        m = dict(shared)
        m["x"] = f(x[c])
        m["p"] = f(p[:, c])
        maps.append(m)
    return maps


def kernel(**inputs):
    nc = build()
    maps = make_in_maps(inputs)
    res = run_bass_kernel_spmd(nc, maps, core_ids=list(range(8)))
    return np.stack([np.asarray(res.results[c]["out"], dtype=np.float32) for c in range(8)], axis=0)
```

```python
import contextlib
import numpy as np
import concourse.bass as bass
import concourse.mybir as mybir
from concourse.bass_utils import run_bass_kernel_spmd

F32 = mybir.dt.float32
BF16 = mybir.dt.bfloat16
I32 = mybir.dt.int32
AF = mybir.ActivationFunctionType
ALU = mybir.AluOpType

S = 2048
D = 1024
NH = 16
FF = 2816
FE = 3584
NE = 8
NG = 4
NCH = 8
EPS = 1e-6
NEGBIG = -30000.0

DEBUG_STOP = None


class Region:
    __slots__ = ("w", "r")

    def __init__(self):
        self.w = None
        self.r = []


class FW:
    def __init__(self, nc, es):
        self.nc = nc
        self.es = es
        self.eng = {"pe": nc.tensor, "act": nc.scalar, "dve": nc.vector, "pool": nc.gpsimd, "sp": nc.sync}
        self.sem = {k: es.enter_context(nc.semaphore("s_" + k)) for k in self.eng}
        self.cnt = {k: 0 for k in self.eng}
        self.waited = {k: {} for k in self.eng}
        self.dsems = []

    def dsem(self, name):
        h = {"sem": self.es.enter_context(self.nc.semaphore(f"{name}_{len(self.dsems)}")), "val": 0}
        self.dsems.append(h)
        return h

    def _wait(self, e, tok):
        sem, val, owner = tok
        if owner is not None:
            if owner == "pe" and e == "pe":
                return
            assert val <= self.cnt[owner], f"wait on future token {owner}:{val} > {self.cnt[owner]}"
        key = id(sem)
        if self.waited[e].get(key, 0) >= val:
            return
        self.waited[e][key] = val
        self.eng[e].wait_ge(sem, val)

    def _deps(self, e, reads, writes):
        for r in reads:
            if r.w is not None:
                self._wait(e, r.w)
        for w in writes:
            if w.w is not None:
                self._wait(e, w.w)
            for t in w.r:
                self._wait(e, t)

    def _record(self, tok, reads, writes):
        for r in reads:
            r.r.append(tok)
            if len(r.r) > 64:
                best = {}
                for t in r.r:
                    k = id(t[0])
                    if k not in best or best[k][1] < t[1]:
                        best[k] = t
                r.r = list(best.values())
        for w in writes:
            w.w = tok
            w.r = []

    def op(self, e, fn, reads=(), writes=(), track=True):
        self._deps(e, reads, writes)
        inst = fn(self.eng[e])
        if track:
            self.cnt[e] += 1
            inst.then_inc(self.sem[e], 1)
            tok = (self.sem[e], self.cnt[e], e)
        else:
            tok = (self.sem[e], self.cnt[e] + 1, e)
        self._record(tok, reads, writes)
        return inst

    def dma(self, q, out, in_, ds, reads=(), writes=(), **kw):
        self._deps(q, reads, writes)
        inst = self.eng[q].dma_start(out=out, in_=in_, **kw)
        ds["val"] += 16
        inst.then_inc(ds["sem"], 16)
        tok = (ds["sem"], ds["val"], None)
        self._record(tok, reads, writes)
        return tok

    def barrier(self):
        for e in self.eng:
            for o in self.eng:
                if self.cnt[o] > 0:
                    self._wait(e, (self.sem[o], self.cnt[o], None))
            for ds in self.dsems:
                if ds["val"] > 0:
                    self._wait(e, (ds["sem"], ds["val"], None))


def build(stop=None):
    nc = bass.Bass("TRN2", target_bir_lowering=False)

    def din(name, shape, dt=F32):
        return nc.dram_tensor(name, list(shape), dt, kind="ExternalInput").ap()

    x = din("x", [S, D])
    p = din("p", [2, S, 256])
    gam_d = din("gam", [128, 48])
    bf_d = din("bf", [16, 1])
    rb_d = din("rb", [128, 8])
    w_in = din("w_in", [D, 3 * D + NH])
    w_o_a = din("w_o_a", [D, D])
    w_kv = din("w_kv", [D, 2 * D])
    w_q_b = din("w_q_b", [D, D])
    w_o_b = din("w_o_b", [D, D])
    w_gu_d = din("w_gu_d", [D, 2 * FF])
    w_dn_d = din("w_dn_d", [FF, D])
    r_w = din("r_w", [D, NE])
    lite = stop is not None and stop not in ("moe", "ple1")
    w_gu_m = din("w_gu_m", [NE, D, 2 * FE] if not lite else [1, 1, 2])
    w_dn_m = din("w_dn_m", [NE, FE, D] if not lite else [1, 1, 2])
    w_pp = din("w_pp", [2, 256, D])
    w_pg = din("w_pg", [2, D, D])
    out = nc.dram_tensor("out", [S, D], F32, kind="ExternalOutput").ap()
    cq_d = nc.dram_tensor("cq_d", [NH, 3, S], BF16, kind="Internal").ap()
    ck_d = nc.dram_tensor("ck_d", [NH, 3, S], BF16, kind="Internal").ap()
    dbg = None
    if stop is not None:
        dbg = nc.dram_tensor("dbg", [128, NCH, S], F32, kind="ExternalOutput").ap()

    def kview(w2d):
        return w2d.rearrange("(kc p) n -> p kc n", p=128)

    with contextlib.ExitStack() as es:
        fw = FW(nc, es)
        op = fw.op

        uid = [0]

        def sb(st, name, shape, dt):
            uid[0] += 1
            return st.enter_context(nc.sbuf_tensor(f"{name}_{uid[0]}", list(shape), dt))

        T = {}
        ps = es.enter_context(nc.psum_tensor("ps", [128, 8, 512], F32))
        psr = [Region() for _ in range(8)]

        hT = sb(es, "hT", [128, NCH, S], F32)
        R_h = [[Region() for _ in range(NG)] for _ in range(NCH)]
        def alloc_aT(st):
            T["aT"] = sb(st, "aT", [128, NCH, S], BF16)

        R_a = [[Region() for _ in range(NG)] for _ in range(NCH)]
        all_a = [R_a[m][g] for m in range(NCH) for g in range(NG)]

        ident_f = sb(es, "ident_f", [128, 128], F32)
        negI_b = sb(es, "negI_b", [128, 128], BF16)
        negbigI_b = sb(es, "negbigI_b", [128, 128], BF16)
        negtri_b = sb(es, "negtri_b", [128, 128], BF16)
        ones_b = sb(es, "ones_b", [128, 128], BF16)
        gam = sb(es, "gam", [128, 48], F32)
        nbf = sb(es, "nbf", [16, 1], F32)
        rb = sb(es, "rb", [128, 8], F32)
        R_const = Region()
        ds_c = fw.dsem("ds_c")
        with contextlib.ExitStack() as ph:
            io = sb(ph, "io", [128, 512], I32)
            R_io = Region()
            op("pool", lambda e: e.iota(io[:, 0:128], [[1, 128]], base=0, channel_multiplier=-1), writes=[R_io])
            op("dve", lambda e: e.tensor_scalar(ident_f[:], io[:, 0:128], 0.0, None, ALU.is_equal), reads=[R_io], writes=[R_const])
            op("dve", lambda e: e.tensor_scalar(negI_b[:], io[:, 0:128], 0.0, -1.0, ALU.is_equal, ALU.mult), reads=[R_io], writes=[R_const])
            op("dve", lambda e: e.tensor_scalar(negbigI_b[:], io[:, 0:128], 0.0, NEGBIG, ALU.is_equal, ALU.mult), reads=[R_io], writes=[R_const])
            op("dve", lambda e: e.tensor_scalar(negtri_b[:], io[:, 0:128], 0.0, -1.0, ALU.is_le, ALU.mult), reads=[R_io], writes=[R_const])
            op("dve", lambda e: e.memset(ones_b[:], 1.0), writes=[R_const])
            fw.dma("sp", gam[:], gam_d[:, :], ds_c, writes=[R_const])
            fw.dma("sp", nbf[:], bf_d[:, :], ds_c, writes=[R_const])
            fw.dma("sp", rb[:], rb_d[:, :], ds_c, writes=[R_const])
            op("dve", lambda e: e.tensor_scalar(nbf[:], nbf[:], -1.0, None, ALU.mult), reads=[R_const], writes=[R_const])
            fw.barrier()

        def gc(g):
            return slice(g * 512, (g + 1) * 512)

        def make_mask(st, name, cmp_op):
            mk = sb(st, name, [128, 4, 512], BF16)
            with contextlib.ExitStack() as tmpst:
                io_ = sb(tmpst, name + "_io", [128, 512], I32)
                R_io_ = Region()
                for v in range(4):
                    op("pool", lambda e: e.iota(io_[:, :], [[1, 512]], base=-128 * v, channel_multiplier=-1), writes=[R_io_])
                    op("dve", lambda e: e.tensor_scalar(mk[:, v, :], io_[:, :], 0.0, None, cmp_op), reads=[R_io_], writes=[R_const])
                fw.barrier()
            return mk

        def mm(out_ap, lhsT, rhs, start, stop, reads, writes, track=None):
            if track is None:
                track = stop
            return op("pe", lambda e: e.matmul(out_ap, lhsT, rhs, start=start, stop=stop), reads=reads, writes=writes, track=track)

        class Stream:
            def __init__(self, st, name, shape, dt, nbuf):
                self.t = [sb(st, f"{name}{i}", shape, dt) for i in range(nbuf)]
                self.r = [Region() for _ in range(nbuf)]
                self.ds = [fw.dsem(f"ds_{name}{i}") for i in range(nbuf)]
                self.i = 0
                self.n = nbuf

            def next(self):
                i = self.i
                self.i = (i + 1) % self.n
                return self.t[i], self.r[i], self.ds[i]

        def wload(stream, dst_fn, src, q="pool", **kw):
            t, r, ds = stream.next()
            fw.dma(q, dst_fn(t), src, ds, writes=[r], **kw)
            return t, r

        def dump(name):
            if stop == name:
                ds = fw.dsem("ds_dbg")
                tok = fw.dma("sp", dbg[:, :, :], hT[:], ds, reads=[R_h[m][g] for m in range(NCH) for g in range(NG)])
                fw._wait("sp", tok)
                return True
            return False

        def rmsnorm(gi, ph, out_fn=None, post=None):
            sq = [sb(ph, f"sq{gi}_{i}", [128, NCH, 512], BF16) for i in range(2)]
            R_sq = [Region(), Region()]
            lnt = sb(ph, f"lnt{gi}", [128, 512], F32)
            R_ln = Region()
            rstd = [sb(ph, f"rstd{gi}_{i}", [128, 512], F32) for i in range(2)]
            R_rs = [Region(), Region()]
            for g in range(NG):
                b = g % 2
                for m in range(NCH):
                    op("act", lambda e: e.activation(out=sq[b][:, m, :], in_=hT[:, m, gc(g)], func=AF.Square),
                       reads=[R_h[m][g]], writes=[R_sq[b]])
                bank = 6 + b
                for m in range(NCH):
                    mm(ps[:, bank, :], ones_b[:], sq[b][:, m, :], m == 0, m == NCH - 1, [R_sq[b]], [psr[bank]])
                op("act", lambda e: e.activation(out=lnt[:], in_=ps[:, bank, :], func=AF.Ln, bias=EPS, scale=1.0 / D),
                   reads=[psr[bank]], writes=[R_ln])
                op("act", lambda e: e.activation(out=rstd[b][:], in_=lnt[:], func=AF.Exp, scale=-0.5),
                   reads=[R_ln], writes=[R_rs[b]])
                for m in range(NCH):
                    if out_fn is None:
                        op("dve", lambda e: e.scalar_tensor_tensor(out=T["aT"][:, m, gc(g)], in0=hT[:, m, gc(g)],
                                                                   scalar=gam[:, gi * 8 + m:gi * 8 + m + 1], in1=rstd[b][:],
                                                                   op0=ALU.mult, op1=ALU.mult),
                           reads=[R_h[m][g], R_rs[b]], writes=[R_a[m][g]])
                    else:
                        out_fn(g, m, rstd[b], R_rs[b])
                if post is not None:
                    post(g)

        with contextlib.ExitStack() as ph:
            xs = Stream(ph, "xs", [128, 4, D], F32, 2)
            k = 0
            for g in range(NG):
                xt, xr = wload(xs, lambda t: t[:], x[g * 512:(g + 1) * 512, :].rearrange("(t q) d -> q t d", q=128), q="sp")
                for m in range(NCH):
                    bank = k % 4
                    k += 1
                    for t in range(4):
                        op("pe", lambda e: e.transpose(ps[:, bank, t * 128:(t + 1) * 128], xt[:, t, m * 128:(m + 1) * 128], ident_f[:]),
                           reads=[xr, R_const], writes=[psr[bank]], track=(t == 3))
                    if m % 2 == 0:
                        op("act", lambda e: e.activation(out=hT[:, m, gc(g)], in_=ps[:, bank, :], func=AF.Copy),
                           reads=[psr[bank]], writes=[R_h[m][g]])
                    else:
                        op("dve", lambda e: e.tensor_copy(hT[:, m, gc(g)], ps[:, bank, :]), reads=[psr[bank]], writes=[R_h[m][g]])
            fw.barrier()
        if dump("x"):
            return nc

        def out_proj_pair(wo, wor, OTp, R_ot, banks):
            k = 0
            for m in range(NCH):
                for g in range(NG):
                    bank = banks[k % len(banks)]
                    k += 1
                    mm(ps[:, bank, :], wo[:, m * 128:(m + 1) * 128], OTp[:, gc(g)], True, True, [wor, R_ot[g]], [psr[bank]])
                    op("dve", lambda e: e.tensor_tensor(out=hT[:, m, gc(g)], in0=ps[:, bank, :], in1=hT[:, m, gc(g)], op=ALU.add),
                       reads=[psr[bank], R_h[m][g]], writes=[R_h[m][g]])

        def vslice(h):
            if h % 2 == 0:
                return slice(h + 1, 18, 16 - h)
            return slice(0, h + 2, h + 1)

        R_v = [Region() for _ in range(16)]

        def v_proj(ph, wsrc, col0, fill):
            Vt = T["Vt"]
            wv = Stream(ph, "wv", [128, NCH, 512], BF16, 2)
            k = 0
            for half in range(2):
                wt, wr = wload(wv, lambda t: t[:], kview(wsrc)[:, :, col0 + half * 512: col0 + (half + 1) * 512])
                for tt in range(16):
                    bank = k % 4
                    k += 1
                    g = tt // 4
                    for kc in range(NCH):
                        mm(ps[:, bank, :], T["aT"][:, kc, tt * 128:(tt + 1) * 128], wt[:, kc, :], kc == 0, kc == NCH - 1,
                           [R_a[kc][g], wr], [psr[bank]])
                    dst = Vt[:, tt, half * 512:(half + 1) * 512]
                    src = ps[:, bank, :]
                    if tt % 2 == 0:
                        op("act", lambda e: e.activation(out=dst, in_=src, func=AF.Copy), reads=[psr[bank]], writes=[R_v[tt]])
                    else:
                        op("dve", lambda e: e.tensor_copy(dst, src), reads=[psr[bank]], writes=[R_v[tt]])

        phA = contextlib.ExitStack()
        Vt = T["Vt"] = sb(phA, "VtA", [128, 16, D], BF16)
        maskF = make_mask(phA, "maskF", ALU.is_lt)
        alloc_aT(phA)
        with contextlib.ExitStack() as ph:
            rmsnorm(0, ph)
            fw.barrier()
        with contextlib.ExitStack() as ph:
            wf = sb(ph, "wf", [128, NCH, NH], BF16)
            R_wf = Region()
            ds_wf = fw.dsem("ds_wf")
            fw.dma("pool", wf[:], kview(w_in)[:, :, 3 * D:3 * D + NH], ds_wf, writes=[R_wf])
            t1 = sb(ph, "t1", [NH, S], F32)
            cc = sb(ph, "cc", [NH, S], F32)
            one16 = sb(ph, "one16", [NH, S], F32)
            SQ = sb(ph, "SQ", [NH, 3, S], BF16)
            SK = sb(ph, "SK", [NH, 3, S], BF16)
            R_t = Region()
            op("pool", lambda e: e.memset(one16[:], 1.0), writes=[R_t])
            for g in range(NG):
                for kc in range(NCH):
                    mm(ps[0:NH, g, :], wf[:, kc, :], T["aT"][:, kc, gc(g)], kc == 0, kc == NCH - 1, [R_wf, R_a[kc][g]], [psr[g]])
                op("act", lambda e: e.activation(out=t1[:, gc(g)], in_=ps[0:NH, g, :], func=AF.Exp, bias=nbf[:, 0:1], scale=-1.0),
                   reads=[psr[g], R_const], writes=[R_t])
            op("act", lambda e: e.activation(out=t1[:], in_=t1[:], func=AF.Ln, bias=1.0, scale=1.0), reads=[R_t], writes=[R_t])
            op("dve", lambda e: e.tensor_tensor_scan(cc[:], one16[:], t1[:], 0.0, ALU.mult, ALU.subtract), reads=[R_t], writes=[R_t])
            op("dve", lambda e: e.tensor_copy(SQ[:, 0, :], cc[:]), reads=[R_t], writes=[R_t])
            op("dve", lambda e: e.tensor_tensor(out=cc[:], in0=cc[:], in1=SQ[:, 0, :], op=ALU.subtract), reads=[R_t], writes=[R_t])
            op("dve", lambda e: e.tensor_copy(SQ[:, 1, :], cc[:]), reads=[R_t], writes=[R_t])
            op("dve", lambda e: e.tensor_tensor(out=cc[:], in0=cc[:], in1=SQ[:, 1, :], op=ALU.subtract), reads=[R_t], writes=[R_t])
            op("dve", lambda e: e.tensor_copy(SQ[:, 2, :], cc[:]), reads=[R_t], writes=[R_t])
            op("dve", lambda e: e.tensor_scalar(SK[:], SQ[:], -1.0, None, ALU.mult), reads=[R_t], writes=[R_t])
            ds_cq = fw.dsem("ds_cq")
            fw.dma("sp", cq_d[:, :, :], SQ[:], ds_cq, reads=[R_t])
            fw.dma("sp", ck_d[:, :, :], SK[:], ds_cq, reads=[R_t])
            fw.barrier()
        with contextlib.ExitStack() as ph:
            v_proj(ph, w_in, 2 * D, 1.0)
            fw.barrier()
        with contextlib.ExitStack() as ph:
            wqs = Stream(ph, "wq", [128, NCH, 128], BF16, 2)
            wks = Stream(ph, "wk", [128, NCH, 128], BF16, 2)
            wos = Stream(ph, "wo", [128, D], BF16, 2)
            qa = [[sb(ph, f"qa{b}{hh}", [128, S], BF16) for hh in range(2)] for b in range(2)]
            ka = [[sb(ph, f"ka{b}{hh}", [128, S], BF16) for hh in range(2)] for b in range(2)]
            R_qa = [[[Region() for _ in range(NG)] for hh in range(2)] for b in range(2)]
            R_ka = [[[Region() for _ in range(NG)] for hh in range(2)] for b in range(2)]
            R_qaug = [[Region() for hh in range(2)] for b in range(2)]
            R_kaug = [[Region() for hh in range(2)] for b in range(2)]
            ds_aug = [[[fw.dsem(f"ds_aug{b}{hh}{z}") for z in range(2)] for hh in range(2)] for b in range(2)]
            PT = [sb(ph, f"PT{i}", [128, 512], BF16) for i in range(3)]
            R_pt = [Region() for _ in range(3)]
            OTp = [sb(ph, f"OTp{i}", [128, S], BF16) for i in range(2)]
            R_ot = [[Region() for _ in range(NG)] for _ in range(2)]
            _rc = sb(ph, "rc", [128, 512], F32)
            rc = [_rc, _rc]
            _rrc = Region()
            R_rc = [_rrc, _rrc]
            VA = [sb(ph, f"VA{i}", [128, 16, 128], BF16) for i in range(2)]
            VB = [sb(ph, f"VB{i}", [128, 16, 128], BF16) for i in range(2)]
            R_va = [Region(), Region()]
            R_vb = [Region(), Region()]
            for b in range(2):
                op("pool", lambda e: e.memset(VA[b][:, :, 64:128], 1.0), writes=[R_va[b]])
                op("pool", lambda e: e.memset(VB[b][:, :, 0:64], 1.0), writes=[R_vb[b]])
            for b in range(2):
                for hh in range(2):
                    op("pool", lambda e: e.memset(qa[b][hh][64:128, :], 0.0), writes=[R_qaug[b][hh]])
                    op("pool", lambda e: e.memset(qa[b][hh][96:99, :], 1.0), writes=[R_qaug[b][hh]])
                    op("pool", lambda e: e.memset(ka[b][hh][64:128, :], 0.0), writes=[R_kaug[b][hh]])
                    op("pool", lambda e: e.memset(ka[b][hh][64:67, :], 1.0), writes=[R_kaug[b][hh]])

            def proj_pair(pr):
                b = pr % 2
                wq, wqr = wload(wqs, lambda t: t[:], kview(w_in)[:, :, pr * 128:(pr + 1) * 128])
                wk, wkr = wload(wks, lambda t: t[:], kview(w_in)[:, :, D + pr * 128: D + (pr + 1) * 128])
                for hh in range(2):
                    h = 2 * pr + hh
                    fw.dma("sp", qa[b][hh][64:67, :], cq_d[h, :, :], ds_aug[b][hh][0], writes=[R_qaug[b][hh]])
                    fw.dma("sp", ka[b][hh][96:99, :], ck_d[h, :, :], ds_aug[b][hh][1], writes=[R_kaug[b][hh]])
                op("pool", lambda e: e.tensor_copy(VA[b][:, :, 0:64], Vt[:, :, (2 * pr) * 64:(2 * pr + 1) * 64]), reads=R_v, writes=[R_va[b]])
                op("pool", lambda e: e.tensor_copy(VB[b][:, :, 64:128], Vt[:, :, (2 * pr + 1) * 64:(2 * pr + 2) * 64]), reads=R_v, writes=[R_vb[b]])
                k = 0
                for (wt, wr, dst, R_d, scl) in ((wq, wqr, qa[b], R_qa[b], 0.125), (wk, wkr, ka[b], R_ka[b], 1.0)):
                    for g in range(NG):
                        bank = (7, 5, 6)[k % 3]
                        k += 1
                        for kc in range(NCH):
                            mm(ps[:, bank, :], wt[:, kc, :], T["aT"][:, kc, gc(g)], kc == 0, kc == NCH - 1, [wr, R_a[kc][g]], [psr[bank]])
                        for hh in range(2):
                            op("dve", lambda e: e.tensor_scalar(dst[hh][0:64, gc(g)], ps[hh * 64:(hh + 1) * 64, bank, :], scl, None, ALU.mult),
                               reads=[psr[bank]], writes=[R_d[hh][g]])

            sk = 0
            ak = 0
            proj_pair(0)
            for pr in range(NH // 2):
                b = pr % 2
                wo, wor = wload(wos, lambda t: t[:], w_o_a[pr * 128:(pr + 1) * 128, :])
                for hh in range(2):
                    h = 2 * pr + hh
                    for g in range(NG):
                        accb = 3 + (ak % 2)
                        denb = 5 + (ak % 2)
                        ak += 1
                        nj = 4 * g + 4
                        hp = slice(hh * 64, (hh + 1) * 64)

                        def s_mm(j, sbk):
                            diag = j >= 4 * g
                            mm(ps[:, sbk, :], ka[b][hh][0:99, j * 128:(j + 1) * 128], qa[b][hh][0:99, gc(g)], True, not diag,
                               [R_ka[b][hh][j // 4], R_kaug[b][hh], R_qa[b][hh][g], R_qaug[b][hh]], [psr[sbk]])
                            if diag:
                                mm(ps[:, sbk, :], negbigI_b[:], maskF[:, j - 4 * g, :], False, True, [R_const], [psr[sbk]])

                        sk0 = sk
                        sk += nj
                        s_mm(0, sk0 % 3)
                        if nj > 1:
                            s_mm(1, (sk0 + 1) % 3)

                        def pv_den(jj):
                            cb = (sk0 + jj) % 3
                            vsrc, vreg = (VA[b], R_va[b]) if hh == 0 else (VB[b], R_vb[b])
                            mm(ps[:, accb, :], vsrc[:, jj, :], PT[cb][:], jj == 0, jj == nj - 1,
                               [vreg, R_pt[cb]], [psr[accb]], track=True)

                        for j in range(nj):
                            cur = (sk0 + j) % 3
                            if j + 2 < nj:
                                s_mm(j + 2, (sk0 + j + 2) % 3)
                            op("act", lambda e: e.activation(out=PT[cur][:], in_=ps[:, cur, :], func=AF.Exp),
                               reads=[psr[cur]], writes=[R_pt[cur]])
                            if j >= 1:
                                pv_den(j - 1)
                        pv_den(nj - 1)
                        r2 = ak % 2
                        hq = slice((1 - hh) * 64, (2 - hh) * 64)
                        op("act", lambda e: e.activation(out=rc[r2][hp, :], in_=ps[hq, accb, :], func=AF.Ln), reads=[psr[accb]], writes=[R_rc[r2]])
                        op("act", lambda e: e.activation(out=rc[r2][hp, :], in_=rc[r2][hp, :], func=AF.Exp, scale=-1.0), reads=[R_rc[r2]], writes=[R_rc[r2]])
                        op("dve", lambda e: e.tensor_tensor(out=OTp[b][hp, gc(g)], in0=ps[hp, accb, :], in1=rc[r2][hp, :], op=ALU.mult),
                           reads=[psr[accb], R_rc[r2]], writes=[R_ot[b][g]])
                    if hh == 0 and pr + 1 < NH // 2:
                        proj_pair(pr + 1)
                out_proj_pair(wo, wor, OTp[b], R_ot[b], [7, 3, 4, 5, 6])
            fw.barrier()
        phA.close()
        if dump("attn0"):
            return nc

        def swiglu(ph, tag, w_gu2d, w_dn2d, ff, halves, gate_fn=None):
            nmb = ff // 256
            nkc = ff // 128
            wgs = Stream(ph, f"wg{tag}", [128, NCH, 256], BF16, 3)
            wus = Stream(ph, f"wu{tag}", [128, NCH, 256], BF16, 3)
            wds = Stream(ph, f"wd{tag}", [128, nkc, 128], BF16, 3)
            actT = sb(ph, f"actT{tag}", [128, nkc, 1024], BF16)
            R_act = [[Region() for _ in range(2)] for _ in range(nkc)]
            sgt = [sb(ph, f"sg{tag}{i}", [128, 512], BF16) for i in range(2)]
            R_sg = [Region(), Region()]
            tmp = [sb(ph, f"tmp{tag}{i}", [128, 512], F32) for i in range(2)]
            R_tmp = [Region(), Region()]
            return dict(nmb=nmb, nkc=nkc, wgs=wgs, wus=wus, wds=wds, actT=actT, R_act=R_act, sgt=sgt, R_sg=R_sg, tmp=tmp, R_tmp=R_tmp)

        def swiglu_run(st, w_gu2d, w_dn2d, ff, half, gate=None):
            nmb, nkc = st["nmb"], st["nkc"]
            actT, R_act = st["actT"], st["R_act"]
            k = 0
            sgk = 0
            wg_v = kview(w_gu2d)
            wd_v = kview(w_dn2d)
            nxt = (wload(st["wgs"], lambda t: t[:], wg_v[:, :, 0:256]), wload(st["wus"], lambda t: t[:], wg_v[:, :, ff:ff + 256]))
            for mb in range(nmb):
                (wg, wgr), (wu, wur) = nxt
                if mb + 1 < nmb:
                    c0 = (mb + 1) * 256
                    nxt = (wload(st["wgs"], lambda t: t[:], wg_v[:, :, c0:c0 + 256]),
                           wload(st["wus"], lambda t: t[:], wg_v[:, :, ff + c0:ff + c0 + 256]))
                for mi in range(2):
                    m = mb * 2 + mi
                    for gg in range(2):
                        g = half * 2 + gg
                        gb = (k % 2) * 2
                        ub = gb + 1
                        k += 1
                        for kc in range(NCH):
                            mm(ps[:, gb, :], wg[:, kc, mi * 128:(mi + 1) * 128], T["aT"][:, kc, gc(g)], kc == 0, kc == NCH - 1,
                               [wgr, R_a[kc][g]], [psr[gb]])
                        for kc in range(NCH):
                            mm(ps[:, ub, :], wu[:, kc, mi * 128:(mi + 1) * 128], T["aT"][:, kc, gc(g)], kc == 0, kc == NCH - 1,
                               [wur, R_a[kc][g]], [psr[ub]])
                        s = sgk % 2
                        sgk += 1
                        op("act", lambda e: e.activation(out=st["sgt"][s][:], in_=ps[:, gb, :], func=AF.Silu),
                           reads=[psr[gb]], writes=[st["R_sg"][s]])
                        op("dve", lambda e: e.tensor_tensor(out=actT[:, m, gg * 512:(gg + 1) * 512], in0=ps[:, ub, :], in1=st["sgt"][s][:], op=ALU.mult),
                           reads=[psr[ub], st["R_sg"][s]], writes=[R_act[m][gg]])
            nxt = wload(st["wds"], lambda t: t[:], wd_v[:, :, 0:128])
            k = 0
            for o in range(NCH):
                wd, wdr = nxt
                if o + 1 < NCH:
                    nxt = wload(st["wds"], lambda t: t[:], wd_v[:, :, (o + 1) * 128:(o + 2) * 128])
                for gg in range(2):
                    g = half * 2 + gg
                    bank = 4 + (k % 4)
                    k += 1
                    for kc in range(nkc):
                        mm(ps[:, bank, :], wd[:, kc, :], actT[:, kc, gg * 512:(gg + 1) * 512], kc == 0, kc == nkc - 1,
                           [wdr, R_act[kc][gg]], [psr[bank]])
                    if gate is None:
                        op("dve", lambda e: e.tensor_tensor(out=hT[:, o, gc(g)], in0=ps[:, bank, :], in1=hT[:, o, gc(g)], op=ALU.add),
                           reads=[psr[bank], R_h[o][g]], writes=[R_h[o][g]])
                    else:
                        gt, gr = gate
                        s = k % 2
                        op("dve", lambda e: e.tensor_tensor(out=st["tmp"][s][:], in0=ps[:, bank, :], in1=gt[:, gg * 512:(gg + 1) * 512], op=ALU.mult),
                           reads=[psr[bank], gr], writes=[st["R_tmp"][s]])
                        op("pool", lambda e: e.tensor_tensor(out=hT[:, o, gc(g)], in0=st["tmp"][s][:], in1=hT[:, o, gc(g)], op=ALU.add),
                           reads=[st["R_tmp"][s], R_h[o][g]], writes=[R_h[o][g]])

        phF = contextlib.ExitStack()
        alloc_aT(phF)
        with contextlib.ExitStack() as ph:
            rmsnorm(1, ph)
            fw.barrier()
        with contextlib.ExitStack() as ph:
            st = swiglu(ph, "d", w_gu_d, w_dn_d, FF, 2)
            for half in range(2):
                swiglu_run(st, w_gu_d, w_dn_d, FF, half)
            fw.barrier()
        phF.close()
        if dump("ffn0"):
            return nc

        def ple(i):
            with contextlib.ExitStack() as ph:
                alloc_aT(ph)
                for m in range(NCH):
                    for g in range(NG):
                        if (m + g) % 2 == 0:
                            op("act", lambda e: e.activation(out=T["aT"][:, m, gc(g)], in_=hT[:, m, gc(g)], func=AF.Copy),
                               reads=[R_h[m][g]], writes=[R_a[m][g]])
                        else:
                            op("pool", lambda e: e.tensor_copy(T["aT"][:, m, gc(g)], hT[:, m, gc(g)]), reads=[R_h[m][g]], writes=[R_a[m][g]])
                pt = sb(ph, f"pt{i}", [128, 16, 256], F32)
                R_p = Region()
                ds_p = fw.dsem(f"ds_p{i}")
                fw.dma("sp", pt[:], p[i, :, :].rearrange("(t q) f -> q t f", q=128), ds_p, writes=[R_p])
                pT = sb(ph, f"pT{i}", [128, 2, S], BF16)
                R_pT = [Region() for _ in range(NG)]
                wpp = sb(ph, f"wpp{i}", [128, 2, D], BF16)
                R_wpp = Region()
                ds_wpp = fw.dsem(f"ds_wpp{i}")
                fw.dma("pool", wpp[:], w_pp[i, :, :].rearrange("(kc q) n -> q kc n", q=128), ds_wpp, writes=[R_wpp])
                k = 0
                for g in range(NG):
                    for pc in range(2):
                        bank = k % 2
                        k += 1
                        for t in range(4):
                            op("pe", lambda e: e.transpose(ps[:, bank, t * 128:(t + 1) * 128], pt[:, 4 * g + t, pc * 128:(pc + 1) * 128], ident_f[:]),
                               reads=[R_p, R_const], writes=[psr[bank]], track=(t == 3))
                        op("dve", lambda e: e.tensor_copy(pT[:, pc, gc(g)], ps[:, bank, :]), reads=[psr[bank]], writes=[R_pT[g]])
                wpgs = Stream(ph, f"wpg{i}", [128, NCH, 128], BF16, 3)
                sgt = [sb(ph, f"psg{i}{j}", [128, 512], F32) for j in range(2)]
                R_sg = [Region(), Region()]
                tmp = [sb(ph, f"ptmp{i}{j}", [128, 512], F32) for j in range(2)]
                R_tmp = [Region(), Region()]
                wpg_v = kview(w_pg[i, :, :])
                nxt = wload(wpgs, lambda t: t[:], wpg_v[:, :, 0:128])
                k = 0
                for o in range(NCH):
                    wg, wgr = nxt
                    if o + 1 < NCH:
                        nxt = wload(wpgs, lambda t: t[:], wpg_v[:, :, (o + 1) * 128:(o + 2) * 128])
                    for g in range(NG):
                        gb = 2 + (k % 2) * 2
                        pb = gb + 1
                        s = k % 2
                        k += 1
                        for kc in range(NCH):
                            mm(ps[:, gb, :], wg[:, kc, :], T["aT"][:, kc, gc(g)], kc == 0, kc == NCH - 1, [wgr, R_a[kc][g]], [psr[gb]])
                        for kc in range(2):
                            mm(ps[:, pb, :], wpp[:, kc, o * 128:(o + 1) * 128], pT[:, kc, gc(g)], kc == 0, kc == 1, [R_wpp, R_pT[g]], [psr[pb]])
                        op("act", lambda e: e.activation(out=sgt[s][:], in_=ps[:, gb, :], func=AF.Sigmoid), reads=[psr[gb]], writes=[R_sg[s]])
                        op("dve", lambda e: e.tensor_tensor(out=tmp[s][:], in0=ps[:, pb, :], in1=sgt[s][:], op=ALU.mult),
                           reads=[psr[pb], R_sg[s]], writes=[R_tmp[s]])
                        op("pool", lambda e: e.tensor_tensor(out=hT[:, o, gc(g)], in0=tmp[s][:], in1=hT[:, o, gc(g)], op=ALU.add),
                           reads=[R_tmp[s], R_h[o][g]], writes=[R_h[o][g]])
                fw.barrier()

        ple(0)
        if dump("ple0"):
            return nc

        phB = contextlib.ExitStack()
        KT = sb(phB, "KT", [128, NCH, S], BF16)
        Vt = T["Vt"] = sb(phB, "VtB", [128, 16, D], BF16)
        maskS = make_mask(phB, "maskS", ALU.is_le)
        alloc_aT(phB)
        R_k = [[Region() for _ in range(NG)] for _ in range(NCH)]
        with contextlib.ExitStack() as ph:
            rmsnorm(2, ph)
            fw.barrier()
        with contextlib.ExitStack() as ph:
            wks = Stream(ph, "wkk", [128, NCH, 128], BF16, 3)
            wkv_v = kview(w_kv)
            k = 0
            for pr in range(NCH):
                wk, wkr = wload(wks, lambda t: t[:], wkv_v[:, :, pr * 128:(pr + 1) * 128])
                for g in range(NG):
                    bank = 4 + k % 4
                    k += 1
                    for kc in range(NCH):
                        mm(ps[:, bank, :], wk[:, kc, :], T["aT"][:, kc, gc(g)], kc == 0, kc == NCH - 1, [wkr, R_a[kc][g]], [psr[bank]])
                    if k % 2 == 0:
                        op("act", lambda e: e.activation(out=KT[:, pr, gc(g)], in_=ps[:, bank, :], func=AF.Copy), reads=[psr[bank]], writes=[R_k[pr][g]])
                    else:
                        op("dve", lambda e: e.tensor_copy(KT[:, pr, gc(g)], ps[:, bank, :]), reads=[psr[bank]], writes=[R_k[pr][g]])
            v_proj(ph, w_kv, D, 0.0)
            fw.barrier()

        with contextlib.ExitStack() as ph:
            rmsnorm(3, ph)
            fw.barrier()
        with contextlib.ExitStack() as ph:
            wqs = Stream(ph, "wq1", [128, NCH, 128], BF16, 2)
            wos = Stream(ph, "wo1", [128, D], BF16, 2)
            qz = [[sb(ph, f"qz{b}{hh}", [128, S], BF16) for hh in range(2)] for b in range(2)]
            R_q = [[[Region() for _ in range(NG)] for hh in range(2)] for _ in range(2)]
            R_qz = Region()
            for b_ in range(2):
                for hh_ in range(2):
                    op("pool", lambda e: e.memset(qz[b_][hh_][:], 0.0), writes=[R_qz])
            et = [sb(ph, f"et{i}", [128, 512], F32) for i in range(2)]
            R_e = [Region(), Region()]
            SPt = [sb(ph, f"SPt{i}", [128, 512], BF16) for i in range(3)]
            R_sp = [Region() for _ in range(3)]
            Wt = [sb(ph, f"Wt{i}", [128, 512], BF16) for i in range(2)]
            R_w = [Region(), Region()]
            Rsb = [sb(ph, f"Rsb{i}", [128, 512], BF16) for i in range(2)]
            R_r = [Region(), Region()]
            _ot1 = sb(ph, "OT1p", [128, S], BF16)
            OTp = [_ot1, _ot1]
            _rot1 = [Region() for _ in range(NG)]
            R_ot = [_rot1, _rot1]
            wq_v = kview(w_q_b)

            def qproj(pr):
                b = pr % 2
                wq, wqr = wload(wqs, lambda t: t[:], wq_v[:, :, pr * 128:(pr + 1) * 128])
                for g in range(NG):
                    bank = 7
                    for kc in range(NCH):
                        mm(ps[:, bank, :], wq[:, kc, :], T["aT"][:, kc, gc(g)], kc == 0, kc == NCH - 1, [wqr, R_a[kc][g]], [psr[bank]])
                    for hh_ in range(2):
                        hp_ = slice(hh_ * 64, (hh_ + 1) * 64)
                        op("dve", lambda e: e.tensor_scalar(qz[b][hh_][hp_, gc(g)], ps[hp_, bank, :], 0.125, None, ALU.mult),
                           reads=[psr[bank], R_qz], writes=[R_q[b][hh_][g]])

            cn = dict(a=0, b=0, e=0, sp=0, w=0, r=0)
            qproj(0)
            for pr in range(NH // 2):
                b = pr % 2
                wo, wor = wload(wos, lambda t: t[:], w_o_b[pr * 128:(pr + 1) * 128, :])
                for g in range(NG):
                    accb = 4 + (g % 2)
                    for hh in range(2):
                        h = 2 * pr + hh
                        hp = slice(hh * 64, (hh + 1) * 64)
                        nj = 4 * g + 4

                        def z_mm(bank, j, stop, track):
                            diag = j >= 4 * g
                            jc = slice(j * 128, (j + 1) * 128)
                            mm(ps[:, bank, :], KT[:, pr, jc], qz[b][hh][:, gc(g)], True, stop and not diag,
                               [R_k[pr][j // 4], R_q[b][hh][g]], [psr[bank]], track=(track and not diag))
                            if diag:
                                mm(ps[:, bank, :], negbigI_b[:], maskS[:, j - 4 * g, :], False, stop, [R_const], [psr[bank]], track=track)

                        def stageA(j):
                            A = cn["a"] % 2
                            cn["a"] += 1
                            z_mm(A, j, True, True)
                            e_i = cn["e"] % 2
                            cn["e"] += 1
                            op("act", lambda e: e.activation(out=et[e_i][:], in_=ps[:, A, :], func=AF.Exp), reads=[psr[A]], writes=[R_e[e_i]])
                            s_i = cn["sp"] % 3
                            cn["sp"] += 1
                            op("act", lambda e: e.activation(out=SPt[s_i][:], in_=et[e_i][:], func=AF.Ln, bias=1.0, scale=1.0),
                               reads=[R_e[e_i]], writes=[R_sp[s_i]])
                            return s_i

                        js = list(range(nj - 1, -1, -1))
                        nt = len(js)
                        sp_of = {0: stageA(js[0])}
                        if nt > 1:
                            sp_of[1] = stageA(js[1])
                        pend = None

                        def emit_pv(pv):
                            pj, pw, pfirst = pv
                            mm(ps[:, 4 + hh, :], Vt[:, pj, pr * 128:(pr + 1) * 128], Wt[pw][:], pfirst, pj == 0,
                               [R_v[pj], R_w[pw]], [psr[4 + hh]], track=True)

                        for idx, j in enumerate(js):
                            first = idx == 0
                            if idx + 2 < nt:
                                sp_of[idx + 2] = stageA(js[idx + 2])
                            s_i = sp_of[idx]
                            r_prev = (cn["r"] - 1) % 2
                            if j > 0:
                                mm(ps[:, 6, :], ones_b[:], SPt[s_i][:], True, True, [R_const, R_sp[s_i]], [psr[6]], track=True)
                                r_i = cn["r"] % 2
                                cn["r"] += 1
                                if first:
                                    op("dve", lambda e: e.tensor_copy(Rsb[r_i][:], ps[:, 6, :]), reads=[psr[6]], writes=[R_r[r_i]])
                                else:
                                    op("dve", lambda e: e.tensor_tensor(out=Rsb[r_i][:], in0=ps[:, 6, :], in1=Rsb[r_prev][:], op=ALU.add),
                                       reads=[psr[6], R_r[r_prev]], writes=[R_r[r_i]])
                            B = 2 + (cn["b"] % 2)
                            cn["b"] += 1
                            z_mm(B, j, False, False)
                            mm(ps[:, B, :], negtri_b[:], SPt[s_i][:], False, first, [R_const, R_sp[s_i]], [psr[B]], track=first)
                            if not first:
                                mm(ps[:, B, :], negI_b[:], Rsb[r_prev][:], False, True, [R_const, R_r[r_prev]], [psr[B]])
                            w_i = cn["w"] % 2
                            cn["w"] += 1
                            op("act", lambda e: e.activation(out=Wt[w_i][:], in_=ps[:, B, :], func=AF.Exp), reads=[psr[B]], writes=[R_w[w_i]])
                            if pend is not None:
                                emit_pv(pend)
                            pend = (j, w_i, first)
                        emit_pv(pend)
                        op("dve", lambda e: e.tensor_copy(OTp[b][hp, gc(g)], ps[hp, 4 + hh, :]), reads=[psr[4 + hh]], writes=[R_ot[b][g]])
                    if g == 1 and pr + 1 < NH // 2:
                        qproj(pr + 1)
                out_proj_pair(wo, wor, OTp[b], R_ot[b], [7, 0, 1, 2, 3])
            fw.barrier()
        phB.close()
        if dump("attn1"):
            return nc

        CAP = 384
        NSC = CAP // 128
        with contextlib.ExitStack() as ph:
            G = sb(ph, "G", [128, 16, NE], F32)
            R_G = Region()
            GT = sb(ph, "GT", [NE, S], BF16)
            R_GT = Region()
            sel = sb(ph, "sel", [NE, NE, 128], BF16)
            R_sel = Region()
            f_tok = sb(ph, "f_tok", [128, 16, D], BF16)
            R_ft = [Region() for _ in range(16)]
            posT = sb(ph, "posT", [NE, 2, S], BF16)
            R_posT = Region()
            pos = sb(ph, "pos", [128, 16, NE], F32)
            Mf = sb(ph, "Mf", [128, 16, NE], F32)
            R_pos = Region()
            iotaC = sb(ph, "iotaC", [128, CAP], F32)
            slotid = sb(ph, "slotid", [128, NSC], F32)
            R_io = Region()
            with contextlib.ExitStack() as ph2:
                alloc_aT(ph2)
                aT = T["aT"]
                wr_f = sb(ph2, "wr_f", [128, NCH, NE], F32)
                wr_hi = sb(ph2, "wr_hi", [128, NCH, NE], BF16)
                wr_lo = sb(ph2, "wr_lo", [128, NCH, NE], BF16)
                R_wr = Region()
                ds_wr = fw.dsem("ds_wr")
                fw.dma("sp", wr_f[:], kview(r_w), ds_wr, writes=[R_wr])
                op("dve", lambda e: e.tensor_copy(wr_hi[:], wr_f[:]), reads=[R_wr], writes=[R_wr])
                op("dve", lambda e: e.tensor_tensor(out=wr_f[:], in0=wr_f[:], in1=wr_hi[:], op=ALU.subtract), reads=[R_wr], writes=[R_wr])
                op("dve", lambda e: e.tensor_copy(wr_lo[:], wr_f[:]), reads=[R_wr], writes=[R_wr])
                fT32 = sb(ph2, "fT32", [128, 512], F32)
                R_f32 = Region()
                loT = [sb(ph2, f"loT{i}", [128, NCH, 512], BF16) for i in range(2)]
                R_lo = [Region(), Region()]
                lg = sb(ph2, "lg", [128, 16, NE], F32)
                R_lg = Region()
                mx = sb(ph2, "mx", [128, 8], F32)
                nv1 = sb(ph2, "nv1", [128, 1], F32)
                msk = sb(ph2, "msk", [128, NE], F32)
                eg = sb(ph2, "eg", [128, NE], F32)
                den = sb(ph2, "den", [128, 1], F32)
                R_s = Region()
                io2 = sb(ph2, "io2", [NE, NE, 128], I32)
                op("pool", lambda e: e.iota(io2[:], [[-1, NE], [0, 128]], base=0, channel_multiplier=1), writes=[R_sel])
                op("dve", lambda e: e.tensor_scalar(sel[:], io2[:], 0.0, None, ALU.is_equal), reads=[R_sel], writes=[R_sel])
                io3 = sb(ph2, "io3", [128, CAP], I32)
                op("pool", lambda e: e.iota(io3[:], [[1, CAP]], base=0, channel_multiplier=0), writes=[R_io])
                op("dve", lambda e: e.tensor_copy(iotaC[:], io3[:]), reads=[R_io], writes=[R_io])
                op("pool", lambda e: e.iota(io3[:, 0:NSC], [[128, NSC]], base=0, channel_multiplier=1), reads=[R_io], writes=[R_io])
                op("dve", lambda e: e.tensor_copy(slotid[:], io3[:, 0:NSC]), reads=[R_io], writes=[R_io])

                def norm_out(g, m, rstd_t, R_rs):
                    b = g % 2
                    op("dve", lambda e: e.scalar_tensor_tensor(out=fT32[:], in0=hT[:, m, gc(g)], scalar=gam[:, 4 * 8 + m:4 * 8 + m + 1],
                                                               in1=rstd_t[:], op0=ALU.mult, op1=ALU.mult),
                       reads=[R_h[m][g], R_rs], writes=[R_f32])
                    op("dve", lambda e: e.tensor_copy(aT[:, m, gc(g)], fT32[:]), reads=[R_f32], writes=[R_a[m][g]])
                    op("dve", lambda e: e.tensor_tensor(out=loT[b][:, m, :], in0=fT32[:], in1=aT[:, m, gc(g)], op=ALU.subtract),
                       reads=[R_f32, R_a[m][g]], writes=[R_lo[b]])

                def route(g):
                    b = g % 2
                    for t in range(4):
                        tt = 4 * g + t
                        tc_ = slice(t * 128, (t + 1) * 128)
                        bank = 4 + (tt % 2)
                        n = 0
                        for kc in range(NCH):
                            for (l, r_, rr) in ((aT[:, kc, tt * 128:(tt + 1) * 128], wr_hi, R_a[kc][g]),
                                                (loT[b][:, kc, tc_], wr_hi, R_lo[b]),
                                                (aT[:, kc, tt * 128:(tt + 1) * 128], wr_lo, R_a[kc][g])):
                                mm(ps[:, bank, 0:NE], l, r_[:, kc, :], n == 0, n == 3 * NCH - 1, [rr, R_wr], [psr[bank]])
                                n += 1
                        op("dve", lambda e: e.tensor_tensor(out=lg[:, tt, :], in0=ps[:, bank, 0:NE], in1=rb[:], op=ALU.add),
                           reads=[psr[bank], R_const], writes=[R_lg])
                        op("dve", lambda e: e.max(mx[:], lg[:, tt, :]), reads=[R_lg], writes=[R_s])
                        op("dve", lambda e: e.tensor_scalar(nv1[:], mx[:, 0:1], -1.0, None, ALU.mult), reads=[R_s], writes=[R_s])
                        op("dve", lambda e: e.tensor_scalar(msk[:], lg[:, tt, :], mx[:, 1:2], None, ALU.is_ge), reads=[R_lg, R_s], writes=[R_s])
                        op("dve", lambda e: e.tensor_copy(Mf[:, tt, :], msk[:]), reads=[R_s], writes=[R_pos])
                        op("act", lambda e: e.activation(out=eg[:], in_=lg[:, tt, :], func=AF.Exp, bias=nv1[:, 0:1], scale=1.0),
                           reads=[R_lg, R_s], writes=[R_s])
                        op("dve", lambda e: e.tensor_tensor(out=eg[:], in0=eg[:], in1=msk[:], op=ALU.mult), reads=[R_s], writes=[R_s])
                        op("dve", lambda e: e.tensor_reduce(out=den[:], in_=eg[:], axis=mybir.AxisListType.X, op=ALU.add), reads=[R_s], writes=[R_s])
                        op("dve", lambda e: e.reciprocal(out=den[:], in_=den[:]), reads=[R_s], writes=[R_s])
                        op("dve", lambda e: e.tensor_scalar(G[:, tt, :], eg[:], den[:, 0:1], None, ALU.mult), reads=[R_s], writes=[R_G])
                        for mq in range(2):
                            fb = 2 * (tt % 2) + mq
                            for mi in range(4):
                                m = mq * 4 + mi
                                mm(ps[:, fb, mi * 128:(mi + 1) * 128], aT[:, m, tt * 128:(tt + 1) * 128], negI_b[:], True, True,
                                   [R_a[m][g], R_const], [psr[fb]], track=(mi == 3))
                            op("dve", lambda e: e.tensor_scalar(f_tok[:, tt, mq * 512:(mq + 1) * 512], ps[:, fb, :], -1.0, None, ALU.mult),
                               reads=[psr[fb]], writes=[R_ft[tt]])

                rmsnorm(4, ph2, out_fn=norm_out, post=route)
                for g in range(NG):
                    bank = g % 2
                    for t in range(4):
                        op("pe", lambda e: e.transpose(ps[0:NE, bank, t * 128:(t + 1) * 128], G[:, 4 * g + t, :], ident_f[:]),
                           reads=[R_G, R_const], writes=[psr[bank]], track=(t == 3))
                    op("dve", lambda e: e.tensor_copy(GT[:, gc(g)], ps[0:NE, bank, :]), reads=[psr[bank]], writes=[R_GT])
                Mk = sb(ph2, "Mk", [128, 16 * NE], BF16)
                tot = sb(ph2, "tot", [128, 16, NE], F32)
                offs = sb(ph2, "offs", [128, 16, NE], F32)
                stri_b = sb(ph2, "stri_b", [128, 128], BF16)
                io4 = sb(ph2, "io4", [128, 128], I32)
                op("pool", lambda e: e.iota(io4[:], [[1, 128]], base=0, channel_multiplier=-1), writes=[R_io])
                op("dve", lambda e: e.tensor_scalar(stri_b[:], io4[:], 0.0, None, ALU.is_gt), reads=[R_io], writes=[R_io])
                op("dve", lambda e: e.tensor_copy(Mk[:], Mf[:].rearrange("p a b -> p (a b)")), reads=[R_pos], writes=[R_pos])
                mm(ps[:, 4, 0:128], stri_b[:], Mk[:], True, True, [R_io, R_pos], [psr[4]])
                mm(ps[:, 5, 0:128], ones_b[:], Mk[:], True, True, [R_const, R_pos], [psr[5]])
                op("dve", lambda e: e.tensor_copy(tot[:].rearrange("p a b -> p (a b)"), ps[:, 5, 0:128]), reads=[psr[5]], writes=[R_pos])
                op("dve", lambda e: e.memset(offs[:], 0.0), writes=[R_pos])
                for k_ in list(range(1, 8)) + list(range(9, 16)):
                    op("dve", lambda e: e.tensor_tensor(out=offs[:, k_, :], in0=offs[:, k_ - 1, :], in1=tot[:, k_ - 1, :], op=ALU.add),
                       reads=[R_pos], writes=[R_pos])
                op("dve", lambda e: e.tensor_tensor(out=pos[:].rearrange("p a b -> p (a b)"), in0=ps[:, 4, 0:128],
                                                    in1=offs[:].rearrange("p a b -> p (a b)"), op=ALU.add),
                   reads=[psr[4], R_pos], writes=[R_pos])
                for g in range(NG):
                    bank = 6 + g % 2
                    for t in range(4):
                        op("pe", lambda e: e.transpose(ps[0:NE, bank, t * 128:(t + 1) * 128], pos[:, 4 * g + t, :], ident_f[:]),
                           reads=[R_pos, R_const], writes=[psr[bank]], track=(t == 3))
                    op("dve", lambda e: e.tensor_copy(posT[:, 0, gc(g)], ps[0:NE, bank, :]), reads=[psr[bank]], writes=[R_posT])
                    op("dve", lambda e: e.tensor_tensor(out=posT[:, 1, gc(g)], in0=ps[0:NE, bank, :], in1=posT[:, 0, gc(g)], op=ALU.subtract),
                       reads=[psr[bank], R_posT], writes=[R_posT])
                fw.barrier()
            with contextlib.ExitStack() as ph2:
                NKF = FE // 128
                wgs = Stream(ph2, "wgm", [128, NCH, 256], BF16, 3)
                wus = Stream(ph2, "wum", [128, NCH, 256], BF16, 3)
                wds = Stream(ph2, "wdm", [128, 4, 512], BF16, 3)
                Se = sb(ph2, "Se", [128, 8, CAP], BF16)
                R_se = [Region() for _ in range(8)]
                Xg = sb(ph2, "Xg", [128, NCH, CAP], BF16)
                R_xg = [Region() for _ in range(NCH)]
                actT = sb(ph2, "actTm", [128, NKF, CAP], BF16)
                R_act = [Region() for _ in range(NKF)]
                Ysm = [sb(ph2, f"Ysm{i}", [128, NSC, 512], BF16) for i in range(2)]
                R_y = [Region(), Region()]
                GS = sb(ph2, "GS", [128, NSC, 1024], BF16)
                R_gs = [Region(), Region()]
                Gb = sb(ph2, "Gb", [128, 1024], BF16)
                R_gb = [Region(), Region()]
                sgt = [sb(ph2, f"sgm{i}", [128, CAP], BF16) for i in range(2)]
                R_sg = [Region(), Region()]
                kk = dict(b=0, s=0, d=0, y=0, e=0)
                eqt = [sb(ph2, f"eqt{i}", [128, 512], BF16) for i in range(2)]
                R_eq = [Region(), Region()]
                items = [(half, ex) for half in range(2) for ex in range(NE)]
                pre = {}

                def prefetch_gu(it):
                    half, ex = it
                    wg_v = kview(w_gu_m[ex, :, :])
                    pre[it] = (wload(wgs, lambda t: t[:], wg_v[:, :, 0:256]), wload(wus, lambda t: t[:], wg_v[:, :, FE:FE + 256]))

                def build_Se(it):
                    half, ex = it
                    for ti in range(8):
                        tt = half * 8 + ti
                        op("dve", lambda e: e.tensor_scalar(Se[:, ti, :], iotaC[:], pos[:, tt, ex:ex + 1], Mf[:, tt, ex:ex + 1],
                                                            ALU.is_equal, ALU.mult),
                           reads=[R_io, R_pos], writes=[R_se[ti]])

                def gather(it):
                    half, ex = it
                    for m in range(NCH):
                        bank = m % 4
                        for ti in range(8):
                            tt = half * 8 + ti
                            mm(ps[:, bank, 0:CAP], f_tok[:, tt, m * 128:(m + 1) * 128], Se[:, ti, :], ti == 0, ti == 7,
                               [R_ft[tt], R_se[ti]], [psr[bank]])
                        if m % 2 == 0:
                            op("act", lambda e: e.activation(out=Xg[:, m, :], in_=ps[:, bank, 0:CAP], func=AF.Copy), reads=[psr[bank]], writes=[R_xg[m]])
                        else:
                            op("dve", lambda e: e.tensor_copy(Xg[:, m, :], ps[:, bank, 0:CAP]), reads=[psr[bank]], writes=[R_xg[m]])

                def build_GS(it):
                    half, ex = it
                    for gg in range(2):
                        g = half * 2 + gg
                        mm(ps[:, 4 + gg, :], sel[:, ex, :], posT[:, 0, gc(g)], True, False, [R_sel, R_posT], [psr[4 + gg]], track=False)
                        mm(ps[:, 4 + gg, :], sel[:, ex, :], posT[:, 1, gc(g)], False, True, [R_sel, R_posT], [psr[4 + gg]])
                        mm(ps[:, 6 + gg, :], sel[:, ex, :], GT[:, gc(g)], True, True, [R_sel, R_GT], [psr[6 + gg]])
                        op("act", lambda e: e.activation(out=Gb[:, gg * 512:(gg + 1) * 512], in_=ps[:, 6 + gg, :], func=AF.Copy),
                           reads=[psr[6 + gg]], writes=[R_gb[gg]])
                        for sc in range(NSC):
                            e_i = kk["e"] % 2
                            kk["e"] += 1
                            op("dve", lambda e: e.tensor_scalar(eqt[e_i][:], ps[:, 4 + gg, :], slotid[:, sc:sc + 1], None, ALU.is_equal),
                               reads=[psr[4 + gg], R_io], writes=[R_eq[e_i]])
                            op("dve", lambda e: e.tensor_tensor(out=GS[:, sc, gg * 512:(gg + 1) * 512], in0=eqt[e_i][:],
                                                                 in1=Gb[:, gg * 512:(gg + 1) * 512], op=ALU.mult),
                               reads=[R_eq[e_i], R_gb[gg]], writes=[R_gs[gg]])

                def gu(it):
                    half, ex = it
                    wg_v = kview(w_gu_m[ex, :, :])
                    nxt = pre.pop(it)
                    for mb in range(FE // 256):
                        (wg, wgr), (wu, wur) = nxt
                        if mb + 1 < FE // 256:
                            c0 = (mb + 1) * 256
                            nxt = (wload(wgs, lambda t: t[:], wg_v[:, :, c0:c0 + 256]),
                                   wload(wus, lambda t: t[:], wg_v[:, :, FE + c0:FE + c0 + 256]))
                        for mi in range(2):
                            m = mb * 2 + mi
                            gb = (kk["b"] % 2) * 2
                            ub = gb + 1
                            kk["b"] += 1
                            for kc in range(NCH):
                                mm(ps[:, gb, 0:CAP], wg[:, kc, mi * 128:(mi + 1) * 128], Xg[:, kc, :], kc == 0, kc == NCH - 1,
                                   [wgr, R_xg[kc]], [psr[gb]])
                            for kc in range(NCH):
                                mm(ps[:, ub, 0:CAP], wu[:, kc, mi * 128:(mi + 1) * 128], Xg[:, kc, :], kc == 0, kc == NCH - 1,
                                   [wur, R_xg[kc]], [psr[ub]])
                            s_ = kk["s"] % 2
                            kk["s"] += 1
                            op("act", lambda e: e.activation(out=sgt[s_][:], in_=ps[:, gb, 0:CAP], func=AF.Silu), reads=[psr[gb]], writes=[R_sg[s_]])
                            op("dve", lambda e: e.tensor_tensor(out=actT[:, m, :], in0=ps[:, ub, 0:CAP], in1=sgt[s_][:], op=ALU.mult),
                               reads=[psr[ub], R_sg[s_]], writes=[R_act[m]])

                def down(it, oh, banks):
                    half, ex = it
                    wd_v = kview(w_dn_m[ex, :, :])
                    for k4 in range(NKF // 4):
                        wd, wdr = wload(wds, lambda t: t[:], wd_v[:, 4 * k4:4 * k4 + 4, oh * 512:(oh + 1) * 512])
                        for kq in range(4):
                            kc = 4 * k4 + kq
                            for sc in range(NSC):
                                mm(ps[:, banks[sc], :], actT[:, kc, sc * 128:(sc + 1) * 128], wd[:, kq, :], kc == 0, kc == NKF - 1,
                                   [R_act[kc], wdr], [psr[banks[sc]]], track=(kc == NKF - 1 or (kq == 3 and sc == NSC - 1)))

                def evac_y(oh, banks):
                    for sc in range(NSC):
                        if sc % 2 == 0:
                            op("act", lambda e: e.activation(out=Ysm[oh][:, sc, :], in_=ps[:, banks[sc], :], func=AF.Copy), reads=[psr[banks[sc]]], writes=[R_y[oh]])
                        else:
                            op("dve", lambda e: e.tensor_copy(Ysm[oh][:, sc, :], ps[:, banks[sc], :]), reads=[psr[banks[sc]]], writes=[R_y[oh]])

                def scatter(it, oh, banks):
                    half, ex = it
                    for o4 in range(4):
                        o = oh * 4 + o4
                        for gg in range(2):
                            g = half * 2 + gg
                            bank = banks[kk["d"] % len(banks)]
                            kk["d"] += 1
                            for sc in range(NSC):
                                mm(ps[:, bank, :], Ysm[oh][:, sc, o4 * 128:(o4 + 1) * 128], GS[:, sc, gg * 512:(gg + 1) * 512],
                                   sc == 0, sc == NSC - 1, [R_y[oh], R_gs[gg]], [psr[bank]])
                            op("dve", lambda e: e.tensor_tensor(out=hT[:, o, gc(g)], in0=ps[:, bank, :], in1=hT[:, o, gc(g)], op=ALU.add),
                               reads=[psr[bank], R_h[o][g]], writes=[R_h[o][g]])

                prefetch_gu(items[0])
                build_Se(items[0])
                gather(items[0])
                for i, it in enumerate(items):
                    nx = items[i + 1] if i + 1 < len(items) else None
                    if nx is not None:
                        build_Se(nx)
                    gu(it)
                    if nx is not None:
                        prefetch_gu(nx)
                    build_GS(it)
                    down(it, 0, [4, 5, 6])
                    evac_y(0, [4, 5, 6])
                    if nx is not None:
                        gather(nx)
                    down(it, 1, [7, 0, 1])
                    scatter(it, 0, [2, 3])
                    evac_y(1, [7, 0, 1])
                    scatter(it, 1, [4, 5])
                fw.barrier()
        if dump("moe"):
            return nc

        ple(1)
        if dump("ple1"):
            return nc

        with contextlib.ExitStack() as ph:
            nrm = [sb(ph, f"nrm{i}", [128, NCH, 512], F32) for i in range(2)]
            R_n = [Region(), Region()]
            ot = [sb(ph, f"ot{i}", [128, D], F32) for i in range(2)]
            R_o = [Region(), Region()]
            ds_o = [fw.dsem("ds_o0"), fw.dsem("ds_o1")]
            cnt = [0]
            last_tok = []

            def norm_out(g, m, rstd_t, R_rs):
                b = g % 2
                op("dve", lambda e: e.scalar_tensor_tensor(out=nrm[b][:, m, :], in0=hT[:, m, gc(g)], scalar=gam[:, 5 * 8 + m:5 * 8 + m + 1],
                                                           in1=rstd_t[:], op0=ALU.mult, op1=ALU.mult),
                   reads=[R_h[m][g], R_rs], writes=[R_n[b]])

            def store(g):
                b = g % 2
                for t in range(4):
                    o_i = cnt[0] % 2
                    cnt[0] += 1
                    for hf in range(2):
                        bank = 2 * o_i + hf
                        for mi in range(4):
                            m = hf * 4 + mi
                            op("pe", lambda e: e.transpose(ps[:, bank, mi * 128:(mi + 1) * 128], nrm[b][:, m, t * 128:(t + 1) * 128], ident_f[:]),
                               reads=[R_n[b], R_const], writes=[psr[bank]], track=(mi == 3))
                        if hf == 0:
                            op("act", lambda e: e.activation(out=ot[o_i][:, 0:512], in_=ps[:, bank, :], func=AF.Copy), reads=[psr[bank]], writes=[R_o[o_i]])
                        else:
                            op("dve", lambda e: e.tensor_copy(ot[o_i][:, 512:1024], ps[:, bank, :]), reads=[psr[bank]], writes=[R_o[o_i]])
                    tok = fw.dma("sp", out[g * 512 + t * 128: g * 512 + (t + 1) * 128, :], ot[o_i][:], ds_o[o_i], reads=[R_o[o_i]])
                    last_tok.append(tok)

            rmsnorm(5, ph, out_fn=norm_out, post=store)
            for tok in last_tok[-2:]:
                fw._wait("sp", tok)
            fw.barrier()
    return nc


_W_KEYS = [("w_in", "w_in_a", 0), ("w_o_a", "w_o_a", 0), ("w_kv", "w_kv", None), ("w_q_b", "w_q_b", 0), ("w_o_b", "w_o_b", 0),
           ("w_gu_d", "w_gu_dense", 0), ("w_dn_d", "w_down_dense", 0), ("r_w", "router_w", 0), ("w_gu_m", "w_gu_moe", 0),
           ("w_dn_m", "w_down_moe", 0), ("w_pp", "w_ple_proj", None), ("w_pg", "w_ple_gate", None)]


def make_in_maps(inputs):
    f = lambda a: np.ascontiguousarray(np.asarray(a, dtype=np.float32))
    shared = {}
    for dst, src, idx in _W_KEYS:
        a = f(inputs[src])
        shared[dst] = f(a[idx]) if idx is not None else a
    norms = [f(inputs["attn_norm"])[0], f(inputs["ffn_norm"])[0], f(inputs["kv_norm"]), f(inputs["attn_norm"])[1],
             f(inputs["ffn_norm"])[1], f(inputs["final_norm"])]
    gam = np.stack([n.reshape(8, 128).T for n in norms], axis=1).reshape(128, 48)
    shared["gam"] = f(gam)
    shared["bf"] = f(inputs["b_f"]).reshape(16, 1)
    shared["rb"] = f(np.broadcast_to(f(inputs["router_b"]).reshape(1, 8), (128, 8)))
    x = f(inputs["x"])
    p = f(inputs["p"])
    maps = []
    for c in range(8):
        m = dict(shared)
        m["x"] = f(x[c])
        m["p"] = f(p[:, c])
        maps.append(m)
    return maps


def kernel(**inputs):
    nc = build()
    maps = make_in_maps(inputs)
    res = run_bass_kernel_spmd(nc, maps, core_ids=list(range(8)))
    return np.stack([np.asarray(res.results[c]["out"], dtype=np.float32) for c in range(8)], axis=0)
```
